# Optimizing a Trainium2 kernel written in Bass

```python
import math
import jax
import jax.numpy as jnp
from jax import lax
import numpy as np

D_MODEL = 1024
BATCH = 32
SEQ = 2048
DEPTH = 4

BRANCH_WIDTH = 512
N_BRANCH = 3
CHUNK = 128
GLA_CHUNK = 64
SSM_INNER = BRANCH_WIDTH
SSM_HEAD_DIM = 64
SSM_HEADS = SSM_INNER // SSM_HEAD_DIM
SSM_GROUPS = 2
SSM_STATE = 64
SSM_CONV = 4
SSM_CONV_DIM = SSM_INNER + 2 * SSM_GROUPS * SSM_STATE
RET_HEADS = 8
RET_HEAD_DIM = BRANCH_WIDTH // RET_HEADS
RET_DIM = RET_HEADS * RET_HEAD_DIM
ROPE_BASE = 10000.0
GLA_HEADS = 4
GLA_KEY = BRANCH_WIDTH // 2
GLA_VAL = BRANCH_WIDTH
GLA_KEY_HEAD = GLA_KEY // GLA_HEADS
GLA_VAL_HEAD = GLA_VAL // GLA_HEADS
GLA_RANK = 16
GLA_GATE_NORMALIZER = 16.0
D_FF = 2816
FFN_CONV = 3
IN_SIZES = (SSM_INNER, SSM_CONV_DIM, SSM_HEADS,
            RET_DIM, RET_DIM, RET_DIM, RET_DIM,
            GLA_KEY, GLA_KEY, GLA_VAL, GLA_VAL, GLA_RANK,
            N_BRANCH * D_MODEL)
N_IN = 1288 + 2048 + 1552 + 3072
RMS_EPS = 1e-6
GN_EPS = 1e-5

kernel_name = "hybrid_ssd_retention_gla_convffn"


def rms_norm(x, g):
    xf = x.astype(jnp.float32)
    y = xf * lax.rsqrt(jnp.mean(xf * xf, axis=-1, keepdims=True) + RMS_EPS)
    return y.astype(x.dtype) * g


def grouped_norm(x, g, groups, center):
    bsz, seq, w = x.shape
    xf = x.astype(jnp.float32).reshape(bsz, seq, groups, w // groups)
    if center:
        xf = xf - jnp.mean(xf, axis=-1, keepdims=True)
    y = xf * lax.rsqrt(jnp.mean(xf * xf, axis=-1, keepdims=True) + GN_EPS)
    return y.reshape(bsz, seq, w).astype(x.dtype) * g


def causal_dwconv(x, w, b):
    k, c = w.shape
    y = lax.conv_general_dilated(x, w[:, None, :], window_strides=(1,), padding=[(k - 1, 0)],
                                 dimension_numbers=('NWC', 'WIO', 'NWC'), feature_group_count=c)
    return y + b


def rotary(x, positions):
    half = x.shape[-1] // 2
    inv_freq = ROPE_BASE ** (-jnp.arange(half, dtype=jnp.float32) / half)
    ang = positions.astype(jnp.float32)[:, None] * inv_freq[None, :]
    cos = jnp.cos(ang)[None, :, None, :]
    sin = jnp.sin(ang)[None, :, None, :]
    x1 = x[..., :half].astype(jnp.float32)
    x2 = x[..., half:].astype(jnp.float32)
    return jnp.concatenate([x1 * cos - x2 * sin, x1 * sin + x2 * cos], axis=-1).astype(x.dtype)


def chunked_scalar_decay(q, k, v, log_a, chunk):
    bsz, seq, nh, dn = q.shape
    dp = v.shape[-1]
    nc = seq // chunk
    dt = q.dtype
    q = q.reshape(bsz, nc, chunk, nh, dn)
    k = k.reshape(bsz, nc, chunk, nh, dn)
    v = v.reshape(bsz, nc, chunk, nh, dp)
    cum = jnp.cumsum(log_a.astype(jnp.float32).reshape(bsz, nc, chunk, nh), axis=2)
    cum_t = jnp.swapaxes(cum, 2, 3)
    causal = jnp.tril(jnp.ones((chunk, chunk), dtype=bool))
    seg = cum_t[..., :, None] - cum_t[..., None, :]
    decay = jnp.exp(jnp.where(causal, seg, -jnp.inf)).astype(dt)
    scores = jnp.einsum('bclhn,bcshn->bchls', q, k) * decay
    y_intra = jnp.einsum('bchls,bcshp->bclhp', scores, v)
    last = cum_t[..., -1:]
    w_state = jnp.exp(last - cum_t).astype(dt)
    chunk_kv = jnp.einsum('bcshn,bchs,bcshp->bchnp', k, w_state, v)
    chunk_decay = jnp.exp(last[..., 0]).astype(chunk_kv.dtype)

    def step(state, inp):
        kv_c, dec_c = inp
        return dec_c[..., None, None] * state + kv_c, state

    init = jnp.zeros((bsz, nh, dn, dp), dtype=chunk_kv.dtype)
    _, s_prev = lax.scan(step, init, (jnp.moveaxis(chunk_kv, 1, 0), jnp.moveaxis(chunk_decay, 1, 0)))
    s_prev = jnp.moveaxis(s_prev, 0, 1)
    q_dec = q * jnp.exp(cum)[..., None].astype(dt)
    y_inter = jnp.einsum('bclhn,bchnp->bclhp', q_dec, s_prev)
    return (y_intra + y_inter).reshape(bsz, seq, nh, dp)


def chunked_gla(q, k, v, log_alpha, chunk):
    bsz, seq, nh, dn = q.shape
    dp = v.shape[-1]
    nc = seq // chunk
    dt = q.dtype
    q = q.reshape(bsz, nc, chunk, nh, dn)
    k = k.reshape(bsz, nc, chunk, nh, dn)
    v = v.reshape(bsz, nc, chunk, nh, dp)
    cum = jnp.cumsum(log_alpha.astype(jnp.float32).reshape(bsz, nc, chunk, nh, dn), axis=2)
    last = cum[:, :, -1:]
    q_dec = (q * jnp.exp(cum)).astype(dt)
    k_inv = (k * jnp.exp(-cum)).astype(dt)
    k_state = (k * jnp.exp(last - cum)).astype(dt)
    causal = jnp.tril(jnp.ones((chunk, chunk), dtype=bool))
    scores = jnp.where(causal, jnp.einsum('bclhn,bcshn->bchls', q_dec, k_inv), 0.0).astype(dt)
    y_intra = jnp.einsum('bchls,bcshp->bclhp', scores, v)
    chunk_kv = jnp.einsum('bcshn,bcshp->bchnp', k_state, v)
    chunk_decay = jnp.exp(last[:, :, 0]).astype(chunk_kv.dtype)

    def step(state, inp):
        kv_c, dec_c = inp
        return dec_c[..., None] * state + kv_c, state

    init = jnp.zeros((bsz, nh, dn, dp), dtype=chunk_kv.dtype)
    _, s_prev = lax.scan(step, init, (jnp.moveaxis(chunk_kv, 1, 0), jnp.moveaxis(chunk_decay, 1, 0)))
    s_prev = jnp.moveaxis(s_prev, 0, 1)
    y_inter = jnp.einsum('bclhn,bchnp->bclhp', q_dec, s_prev)
    return (y_intra + y_inter).reshape(bsz, seq, nh, dp)


def hybrid_mixer(h, w_in, ssm_conv_w, ssm_conv_b, ssm_dt_bias, ssm_a_log, ssm_d, ssm_norm_g,
                 ret_norm_g, gla_w_alpha2, gla_b_alpha, gla_norm_g, w_branch, b_gate, w_out):
    bsz, seq, _ = h.shape
    proj = h @ w_in
    idx = np.cumsum(np.array(IN_SIZES))[:-1].tolist()
    (z, xbc, dt_raw, r_q, r_k, r_v, r_g, g_q, g_k, g_v, g_r, g_lr, gate_logits) = jnp.split(proj, idx, axis=-1)

    xbc = jax.nn.silu(causal_dwconv(xbc, ssm_conv_w, ssm_conv_b))
    xs, bm, cm = jnp.split(xbc, [SSM_INNER, SSM_INNER + SSM_GROUPS * SSM_STATE], axis=-1)
    xs = xs.reshape(bsz, seq, SSM_HEADS, SSM_HEAD_DIM)
    heads_per_group = SSM_HEADS // SSM_GROUPS
    bh = jnp.repeat(bm.reshape(bsz, seq, SSM_GROUPS, SSM_STATE), heads_per_group, axis=2)
    ch = jnp.repeat(cm.reshape(bsz, seq, SSM_GROUPS, SSM_STATE), heads_per_group, axis=2)
    dt = jax.nn.softplus(dt_raw.astype(jnp.float32) + ssm_dt_bias.astype(jnp.float32))
    a = -jnp.exp(ssm_a_log.astype(jnp.float32))
    y_ssm = chunked_scalar_decay(ch, bh, xs * dt[..., None].astype(xs.dtype), dt * a, CHUNK)
    y_ssm = (y_ssm + ssm_d[:, None] * xs).reshape(bsz, seq, SSM_INNER)
    y_ssm = grouped_norm(y_ssm * jax.nn.silu(z), ssm_norm_g, SSM_GROUPS, center=False)

    positions = jnp.arange(seq)
    rq = rotary(r_q.reshape(bsz, seq, RET_HEADS, RET_HEAD_DIM), positions)
    rk = rotary(r_k.reshape(bsz, seq, RET_HEADS, RET_HEAD_DIM), positions) * (RET_HEAD_DIM ** -0.5)
    rv = r_v.reshape(bsz, seq, RET_HEADS, RET_HEAD_DIM)
    log_gamma = jnp.log(1.0 - jnp.exp2(-5.0 - jnp.arange(RET_HEADS, dtype=jnp.float32)))
    y_ret = chunked_scalar_decay(rq, rk, rv, jnp.broadcast_to(log_gamma, (bsz, seq, RET_HEADS)), CHUNK)
    y_ret = grouped_norm(y_ret.reshape(bsz, seq, RET_DIM), ret_norm_g, RET_HEADS, center=True)
    y_ret = jax.nn.silu(r_g) * y_ret

    gq = g_q.reshape(bsz, seq, GLA_HEADS, GLA_KEY_HEAD) * (GLA_KEY_HEAD ** -0.5)
    gk = g_k.reshape(bsz, seq, GLA_HEADS, GLA_KEY_HEAD)
    gv = g_v.reshape(bsz, seq, GLA_HEADS, GLA_VAL_HEAD)
    alpha_logits = (g_lr @ gla_w_alpha2 + gla_b_alpha).astype(jnp.float32)
    log_alpha = (jax.nn.log_sigmoid(alpha_logits) / GLA_GATE_NORMALIZER).reshape(bsz, seq, GLA_HEADS, GLA_KEY_HEAD)
    y_gla = chunked_gla(gq, gk, gv, log_alpha, GLA_CHUNK).reshape(bsz, seq, GLA_VAL)
    y_gla = jax.nn.silu(g_r) * grouped_norm(y_gla, gla_norm_g, GLA_HEADS, center=False)

    gates = jax.nn.sigmoid(gate_logits.reshape(bsz, seq, N_BRANCH, D_MODEL) + b_gate)
    merged = (gates[:, :, 0] * (y_ssm @ w_branch[0])
              + gates[:, :, 1] * (y_ret @ w_branch[1])
              + gates[:, :, 2] * (y_gla @ w_branch[2]))
    return merged @ w_out


def conv_ffn(h, w_up, conv_w, conv_b, w_down):
    u = causal_dwconv(h @ w_up, conv_w, conv_b)
    gate, val = jnp.split(u, 2, axis=-1)
    return (jax.nn.silu(gate) * val) @ w_down


def setup_inputs(seed: int = 0) -> dict:
    key = jax.random.key(seed)
    ks = jax.random.split(key, 24)
    L, D = DEPTH, D_MODEL
    nrm = lambda k, shape, scale: jax.random.normal(k, shape, jnp.float32) * scale
    gain = lambda k, shape: 1.0 + 0.02 * jax.random.normal(k, shape, jnp.float32)
    dt0 = jnp.exp(jax.random.uniform(ks[5], (L, SSM_HEADS), jnp.float32, math.log(1e-3), math.log(1e-1)))
    return {
        "x": jax.random.normal(ks[0], (BATCH, SEQ, D), jnp.float32),
        "norm_mix_g": gain(ks[1], (L, D)),
        "w_in": nrm(ks[2], (L, D, N_IN), D ** -0.5),
        "ssm_conv_w": nrm(ks[3], (L, SSM_CONV, SSM_CONV_DIM), SSM_CONV ** -0.5),
        "ssm_conv_b": nrm(ks[4], (L, SSM_CONV_DIM), 0.02),
        "ssm_dt_bias": dt0 + jnp.log(-jnp.expm1(-dt0)),
        "ssm_a_log": jnp.log(jax.random.uniform(ks[6], (L, SSM_HEADS), jnp.float32, 1.0, 16.0)),
        "ssm_d": gain(ks[7], (L, SSM_HEADS)),
        "ssm_norm_g": gain(ks[8], (L, SSM_INNER)),
        "ret_norm_g": gain(ks[9], (L, RET_DIM)),
        "gla_w_alpha2": nrm(ks[10], (L, GLA_RANK, GLA_KEY), GLA_RANK ** -0.5),
        "gla_b_alpha": nrm(ks[11], (L, GLA_KEY), 0.02),
        "gla_norm_g": gain(ks[12], (L, GLA_VAL)),
        "w_branch": nrm(ks[13], (L, N_BRANCH, BRANCH_WIDTH, D), BRANCH_WIDTH ** -0.5),
        "b_gate": nrm(ks[14], (L, N_BRANCH, D), 0.02),
        "w_out": nrm(ks[15], (L, D, D), D ** -0.5),
        "norm_ffn_g": gain(ks[16], (L, D)),
        "w_up": nrm(ks[17], (L, D, 2 * D_FF), D ** -0.5),
        "ffn_conv_w": nrm(ks[18], (L, FFN_CONV, 2 * D_FF), FFN_CONV ** -0.5),
        "ffn_conv_b": nrm(ks[19], (L, 2 * D_FF), 0.02),
        "w_down": nrm(ks[20], (L, D_FF, D), D_FF ** -0.5),
        "norm_f_g": gain(ks[21], (D,)),
    }


def reference(x, norm_mix_g, w_in, ssm_conv_w, ssm_conv_b, ssm_dt_bias, ssm_a_log, ssm_d, ssm_norm_g,
              ret_norm_g, gla_w_alpha2, gla_b_alpha, gla_norm_g, w_branch, b_gate, w_out,
              norm_ffn_g, w_up, ffn_conv_w, ffn_conv_b, w_down, norm_f_g):
    for i in range(DEPTH):
        h = rms_norm(x, norm_mix_g[i])
        x = x + hybrid_mixer(h, w_in[i], ssm_conv_w[i], ssm_conv_b[i], ssm_dt_bias[i], ssm_a_log[i],
                             ssm_d[i], ssm_norm_g[i], ret_norm_g[i], gla_w_alpha2[i], gla_b_alpha[i],
                             gla_norm_g[i], w_branch[i], b_gate[i], w_out[i])
        h = rms_norm(x, norm_ffn_g[i])
        x = x + conv_ffn(h, w_up[i], ffn_conv_w[i], ffn_conv_b[i], w_down[i])
    return rms_norm(x, norm_f_g)
```

```python
import math
from contextlib import ExitStack

import numpy as np
import ml_dtypes
import concourse.bass as bass
import concourse.mybir as mybir
from concourse.bass_utils import run_bass_kernel_spmd

F32 = mybir.dt.float32
BF16 = mybir.dt.bfloat16
AF = mybir.ActivationFunctionType
ALU = mybir.AluOpType
AX = mybir.AxisListType

D = 1024
NIN = 7960
DFF = 2816
NBLK = 40
RMS_EPS = 1e-6
GN_EPS = 1e-5


class Buf:
    __slots__ = ("last_w", "reads")

    def __init__(self):
        self.last_w = None
        self.reads = {}


class Eng:
    def __init__(self, name, sem, inc):
        self.name = name
        self.sem = sem
        self.inc = inc
        self.count = 0
        self.seen = {}
        self.stream = []
        self.pending = False


class Sched:
    NLANES = 12

    def __init__(self, nc, sems):
        self.nc = nc
        self.E = {}
        for i, n in enumerate(["pe", "act", "dve", "pool"]):
            self.E[n] = Eng(n, sems[i], 1)
        self.sp = Eng("sp", None, 0)
        self.lanes = [Eng("lane%d" % i, sems[4 + i], 16) for i in range(self.NLANES)]
        self.lane_rr = 0
        self.n_inst = 0

    def _deps(self, reads, writes):
        deps = {}
        for b in reads:
            if b.last_w is not None:
                e, c = b.last_w
                if deps.get(e, 0) < c:
                    deps[e] = c
        for b in writes:
            if b.last_w is not None:
                e, c = b.last_w
                if deps.get(e, 0) < c:
                    deps[e] = c
            for e, c in b.reads.items():
                if deps.get(e, 0) < c:
                    deps[e] = c
        return deps

    def _emit_waits(self, E, deps, skip_self=False):
        for e, c in deps.items():
            if e is E and skip_self:
                continue
            if E.seen.get(e, 0) >= c:
                continue
            E.seen[e] = c
            E.stream.append(("wait", e.sem, c))

    def op(self, eng, fn, reads=(), writes=(), inc=True):
        E = self.E[eng]
        deps = self._deps(reads, writes)
        self._emit_waits(E, deps, skip_self=(eng == "pe"))
        stamp = E.count + E.inc
        if inc:
            E.count = stamp
            E.pending = False
        else:
            E.pending = True
        E.stream.append(("inst", fn, inc))
        for b in reads:
            b.reads[E] = stamp
        for b in writes:
            b.last_w = (E, stamp)
            b.reads = {}
        self.n_inst += 1

    def dma(self, out, in_, reads=(), writes=()):
        L = self.lanes[self.lane_rr]
        self.lane_rr = (self.lane_rr + 1) % self.NLANES
        deps = self._deps(reads, writes)
        if L.count > 0:
            deps[L] = max(deps.get(L, 0), L.count)
        self._emit_waits(self.sp, deps)
        L.count += 16
        stamp = L.count
        self.sp.stream.append(("dma", out, in_, L.sem))
        for b in reads:
            b.reads[L] = stamp
        for b in writes:
            b.last_w = (L, stamp)
            b.reads = {}
        self.n_inst += 1

    def barrier(self):
        allE = list(self.E.values()) + self.lanes
        for E in list(self.E.values()) + [self.sp]:
            deps = {e: e.count for e in allE if e.count > 0 and e is not E}
            self._emit_waits(E, deps)

    def finish(self):
        deps = {e: e.count for e in list(self.E.values()) + self.lanes if e.count > 0}
        self._emit_waits(self.sp, deps)

    def replay(self, block):
        def run(E, eng):
            for item in E.stream:
                if item[0] == "wait":
                    eng.wait_ge(item[1], item[2])
                elif item[0] == "inst":
                    ins = item[1](eng)
                    if item[2]:
                        ins.then_inc(E.sem, 1)
                else:
                    eng.dma_start(out=item[1], in_=item[2]).then_inc(item[3], 16)

        for E in self.E.values():
            assert not E.pending, E.name

        @block.tensor
        def _(e):
            run(self.E["pe"], e)

        @block.scalar
        def _(e):
            run(self.E["act"], e)

        @block.vector
        def _(e):
            run(self.E["dve"], e)

        @block.gpsimd
        def _(e):
            run(self.E["pool"], e)

        @block.sync
        def _(e):
            run(self.sp, e)


def _const_pack(seqlen):
    nchs = seqlen // 128
    s = np.arange(128)[:, None].astype(np.float64)
    t = np.arange(128)[None, :].astype(np.float64)
    le = (s <= t)
    cols = {}
    cols["triI"] = le.astype(np.float64)
    cols["triS"] = (s > t).astype(np.float64)
    cols["ones"] = np.ones((128, 128))
    cols["triI16"] = -le.astype(np.float64) / 16.0
    cols["triS16"] = -(s > t).astype(np.float64) / 16.0
    cols["m16"] = -np.ones((128, 2)) / 16.0
    cols["causal4"] = np.tile(le.astype(np.float64), (1, 4))
    lg = np.log(1.0 - np.exp2(-5.0 - np.arange(8, dtype=np.float64)))
    dret = np.zeros((128, 8, 128))
    for h in range(8):
        dret[:, h, :] = np.where(le, np.exp(lg[h] * (t - s)), 0.0) * 0.125
    cols["dret"] = dret.reshape(128, 1024)
    qd = np.zeros((128, 4, 128))
    decs = np.zeros((128, 4))
    for j in range(4):
        for hh in range(2):
            h = 2 * j + hh
            qd[hh * 64:(hh + 1) * 64, j, :] = np.exp(lg[h] * (np.arange(128) + 1.0))[None, :] * 0.125
            decs[hh * 64:(hh + 1) * 64, j] = np.exp(lg[h] * 128.0)
    cols["qdec"] = qd.reshape(128, 512)
    cols["decs"] = decs
    wt = np.zeros((128, 8))
    for h in range(8):
        wt[:, h] = np.exp(lg[h] * (127.0 - np.arange(128)))
    cols["wtab"] = wt
    inv = 10000.0 ** (-np.arange(32, dtype=np.float32) / 32.0)
    pos = np.arange(seqlen, dtype=np.float32)
    ang = (pos[:, None] * inv[None, :]).astype(np.float32)
    cos = np.cos(ang).astype(np.float64)
    sin = np.sin(ang).astype(np.float64)
    cos2 = np.concatenate([cos, cos], axis=1).reshape(nchs, 128, 64).transpose(1, 0, 2)
    sin2 = np.concatenate([-sin, sin], axis=1).reshape(nchs, 128, 64).transpose(1, 0, 2)
    cols["cos2"] = cos2.reshape(128, nchs * 64)
    cols["sin2"] = sin2.reshape(128, nchs * 64)
    sel = np.zeros((128, 2, 4, 128))
    for g in range(2):
        for hl in range(4):
            sel[4 * g + hl, g, hl, :] = 1.0
    cols["sel"] = sel.reshape(128, 1024)
    cols["identf"] = np.eye(128)
    off = {}
    o = 0
    arrs = []
    for k, v in cols.items():
        off[k] = (o, v.shape[1])
        o += v.shape[1]
        arrs.append(v)
    cp = np.ascontiguousarray(np.concatenate(arrs, axis=1).astype(np.float32))
    cb = {}
    cb["identb"] = np.eye(128)
    cb["negmask4"] = np.tile(np.where(le, 0.0, -30000.0), (1, 4))
    cb["cmean"] = np.full((128, 128), 1.0 / 1024.0)
    offb = {}
    o = 0
    arrs = []
    for k, v in cb.items():
        offb[k] = (o, v.shape[1])
        o += v.shape[1]
        arrs.append(v)
    cbp = np.ascontiguousarray(np.concatenate(arrs, axis=1).astype(ml_dtypes.bfloat16))
    return cp, off, cbp, offb


PP_FIELDS = [("gmix", 8), ("gffn", 8), ("gbr", 12), ("cw", 24), ("cb", 6), ("bg", 24),
             ("fw", 132), ("fb", 44), ("dtb", 8), ("alog", 8), ("dsk", 8)]
PP_L = sum(n for _, n in PP_FIELDS)


def _pp_off(L):
    off = {}
    o = 0
    for k, n in PP_FIELDS:
        off[k] = o
        o += n
    return off, PP_L * L + 8


def _param_pack(L, p):
    off, tot = _pp_off(L)
    pp = np.zeros((128, tot), np.float32)

    def fm(v, nk):
        return np.asarray(v, np.float32).reshape(nk, 128).T

    for l in range(L):
        b = l * PP_L
        pp[:, b + off["gmix"]: b + off["gmix"] + 8] = fm(p["norm_mix_g"][l], 8)
        pp[:, b + off["gffn"]: b + off["gffn"] + 8] = fm(p["norm_ffn_g"][l], 8)
        for i, nm in enumerate(["ssm_norm_g", "ret_norm_g", "gla_norm_g"]):
            pp[:, b + off["gbr"] + 4 * i: b + off["gbr"] + 4 * i + 4] = fm(p[nm][l], 4)
        for j in range(4):
            pp[:, b + off["cw"] + 6 * j: b + off["cw"] + 6 * j + 6] = fm(p["ssm_conv_w"][l, j], 6)
        pp[:, b + off["cb"]: b + off["cb"] + 6] = fm(p["ssm_conv_b"][l], 6)
        for i in range(3):
            pp[:, b + off["bg"] + 8 * i: b + off["bg"] + 8 * i + 8] = fm(p["b_gate"][l, i], 8)
        for j in range(3):
            pp[:, b + off["fw"] + 44 * j: b + off["fw"] + 44 * j + 44] = fm(p["ffn_conv_w"][l, j], 44)
        pp[:, b + off["fb"]: b + off["fb"] + 44] = fm(p["ffn_conv_b"][l], 44)
        pp[:, b + off["dtb"]: b + off["dtb"] + 8] = np.broadcast_to(p["ssm_dt_bias"][l], (128, 8))
        pp[:, b + off["alog"]: b + off["alog"] + 8] = np.broadcast_to(p["ssm_a_log"][l], (128, 8))
        pp[:, b + off["dsk"]: b + off["dsk"] + 8] = np.broadcast_to(p["ssm_d"][l], (128, 8))
    pp[:, PP_L * L: PP_L * L + 8] = fm(p["norm_f_g"], 8)
    return pp


C_Z, C_XBC, C_DT, C_RQ, C_RK, C_RV, C_RG = 0, 512, 1280, 1288, 1800, 2312, 2824
C_GQ, C_GK, C_GV, C_GR, C_GLR, C_GATE = 3336, 3592, 3848, 4360, 4872, 4888


def _block_defs():
    blks = []
    blks.append(("w_in", [(C_Z, 512)], 8, "gmix"))
    blks.append(("w_in", [(C_XBC, 512)], 8, "gmix"))
    blks.append(("w_in", [(C_XBC + 512, 256), (C_DT, 8), (C_GLR, 16)], 8, "gmix"))
    blks.append(("w_in", [(C_RQ, 512)], 8, "gmix"))
    blks.append(("w_in", [(C_RK, 512)], 8, "gmix"))
    blks.append(("w_in", [(C_RV, 512)], 8, "gmix"))
    blks.append(("w_in", [(C_RG, 512)], 8, "gmix"))
    blks.append(("w_in", [(C_GQ, 512)], 8, "gmix"))
    blks.append(("w_in", [(C_GV, 512)], 8, "gmix"))
    blks.append(("w_in", [(C_GR, 512)], 8, "gmix"))
    for i in range(3):
        blks.append(("w_in", [(C_GATE + i * 1024, 512)], 8, "gmix"))
        blks.append(("w_in", [(C_GATE + i * 1024 + 512, 512)], 8, "gmix"))
        blks.append(("w_branch%d" % i, [(0, 1024)], 4, "gbr%d" % i))
    blks.append(("w_out", [(0, 512)], 8, "half"))
    blks.append(("w_out", [(512, 512)], 8, "half"))
    for b in range(11):
        blks.append(("w_up", [(2 * b * 128, 256), (DFF + 2 * b * 128, 256)], 8, "gffn"))
    for oc in range(8):
        blks.append(("w_down", [(oc * 128, 128)], 22, None))
    assert len(blks) == NBLK
    return blks


def build_nc(L, NSEQ, SEQLEN, T=256, debug=None, stage=2):
    NCH = T // 128
    NT = SEQLEN // T
    nc = bass.Bass("TRN2", target_bir_lowering=False)
    cp_np, coff, cbp_np, cboff = _const_pack(SEQLEN)
    ppoff, pptot = _pp_off(L)

    def dram(name, shape, dt=F32, kind="ExternalInput"):
        return nc.dram_tensor(name, list(shape), dt, kind=kind).ap()

    x_d = dram("x", [NSEQ, SEQLEN, D])
    w_in_d = dram("w_in", [L, D, NIN])
    w_br_d = dram("w_branch", [L, 3, 512, D])
    w_out_d = dram("w_out", [L, D, D])
    w_up_d = dram("w_up", [L, D, 2 * DFF])
    w_dn_d = dram("w_down", [L, DFF, D])
    wa2_d = dram("gla_w_alpha2", [L, 16, 256])
    ba_d = dram("gla_b_alpha", [L, 256])
    pp_d = dram("pp", [128, pptot])
    cp_d = dram("cp", list(cp_np.shape))
    cbp_d = dram("cbp", list(cbp_np.shape), BF16)
    out_d = dram("out", [NSEQ, SEQLEN, D], kind="ExternalOutput")
    ws_d = dram("wscratch", [L, NBLK, 128, 4096], BF16, kind="Internal")
    dbg_d = None
    if debug:
        dbg_d = dram("dbg", [128, debug[1]], kind="ExternalOutput")

    blkdefs = _block_defs()

    with ExitStack() as es:
        sems = [es.enter_context(nc.semaphore("s%d" % i)) for i in range(4 + Sched.NLANES)]
        S = Sched(nc, sems)
        _cnt = [0]

        def sb(shape, dt=F32):
            _cnt[0] += 1
            return es.enter_context(nc.sbuf_tensor("t%d" % _cnt[0], list(shape), dt))

        def act(out, in_, func, r, w, bias=0.0, scale=1.0, accum=None):
            if accum is None:
                S.op("act", lambda e: e.activation(out=out, in_=in_, func=func, bias=bias, scale=scale), r, w)
            else:
                S.op("act", lambda e: e.activation(out=out, in_=in_, func=func, bias=bias, scale=scale,
                                                   accum_out=accum), r, w)

        def tt(eng, out, a, b, op, r, w):
            S.op(eng, lambda e: e.tensor_tensor(out=out, in0=a, in1=b, op=op), r, w)

        def ts(eng, out, a, s1, s2, op0, op1, r, w):
            if s2 is None:
                S.op(eng, lambda e: e.tensor_scalar(out=out, in0=a, scalar1=s1, scalar2=None, op0=op0), r, w)
            else:
                S.op(eng, lambda e: e.tensor_scalar(out=out, in0=a, scalar1=s1, scalar2=s2, op0=op0, op1=op1), r, w)

        def stt(eng, out, a, s, b, op0, op1, r, w):
            S.op("dve", lambda e: e.scalar_tensor_tensor(out=out, in0=a, scalar=s, in1=b, op0=op0, op1=op1), r, w)

        def cpy(eng, out, in_, r, w):
            if eng == "act":
                S.op("act", lambda e: e.copy(out=out, in_=in_), r, w)
            else:
                S.op(eng, lambda e: e.tensor_copy(out=out, in_=in_), r, w)

        def mm(out, lhsT, rhs, start, stop, r, w, inc=None):
            if inc is None:
                inc = stop
            S.op("pe", lambda e: e.matmul(out, lhsT=lhsT, rhs=rhs, start=start, stop=stop), r, w, inc=inc)

        def trp(out, in_, ident, r, w, inc=True):
            S.op("pe", lambda e: e.transpose(out, in_, ident), r, w, inc=inc)

        def bc(ap, shape):
            return ap.to_broadcast(list(shape))

        CP = sb(cp_np.shape)
        CPb = Buf()
        CB = sb(cbp_np.shape, BF16)
        CBb = Buf()
        PP = sb([128, pptot])
        PPb = Buf()
        WA2 = sb([16, L, 256])
        BA = sb([1, L * 256])
        S.dma(CP[:], cp_d[:, :], writes=[CPb])
        S.dma(CB[:], cbp_d[:, :], writes=[CBb])
        S.dma(PP[:], pp_d[:, :], writes=[PPb])
        S.dma(WA2[:], wa2_d.rearrange("l r c -> r l c"), writes=[PPb])
        S.dma(BA[:], ba_d.rearrange("l c -> (l c)").unsqueeze(0), writes=[PPb])

        def C(name, lo=0, hi=None, p0=0, p1=128):
            o, n = coff[name]
            if hi is None:
                hi = n
            return CP[p0:p1, o + lo:o + hi]

        def CBc(name, lo=0, hi=None):
            o, n = cboff[name]
            if hi is None:
                hi = n
            return CB[:, o + lo:o + hi]

        def P(l, name, lo, hi):
            o = l * PP_L + ppoff[name]
            return PP[:, o + lo:o + hi]

        AN = sb([128, L, 8])
        BGH = sb([128, L, 24])
        for l in range(L):
            act(AN[:, l, :], P(l, "alog", 0, 8), AF.Exp, [PPb], [PPb])
            ts("dve", AN[:, l, :], AN[:, l, :], -1.0, None, ALU.mult, None, [PPb], [PPb])
            ts("dve", BGH[:, l, :], P(l, "bg", 0, 24), 0.5, None, ALU.mult, None, [PPb], [PPb])

        banks = [es.enter_context(nc.psum_tensor("pb%d" % i, [128, 512], F32)) for i in range(8)]
        bankb = [Buf() for _ in range(8)]
        _bk = [0]

        def bank():
            i = _bk[0]
            _bk[0] = (i + 1) % 8
            return banks[i], bankb[i]

        with ExitStack() as es2:
            stg = [es2.enter_context(nc.sbuf_tensor("stg%d" % i, [128, 4096], F32)) for i in range(2)]
            stgb = [Buf() for _ in range(2)]
            cvt = [es2.enter_context(nc.sbuf_tensor("cvt%d" % i, [128, 4096], BF16)) for i in range(2)]
            cvtb = [Buf() for _ in range(2)]
            wsb = Buf()
            k = 0
            engs = ["dve", "pool", "act"]
            for l in range(L):
                for bi, (mat, segs, KC, sk) in enumerate(blkdefs):
                    s_, c_ = stg[k % 2], cvt[k % 2]
                    sB, cB = stgb[k % 2], cvtb[k % 2]
                    ncols = sum(n for _, n in segs)
                    if mat == "w_in":
                        src = w_in_d[l]
                    elif mat.startswith("w_branch"):
                        src = w_br_d[l, int(mat[-1])]
                    elif mat == "w_out":
                        src = w_out_d[l]
                    elif mat == "w_up":
                        src = w_up_d[l]
                    else:
                        src = w_dn_d[l]
                    srcv = src.rearrange("(kc p) n -> p kc n", p=128)
                    sv = s_[:, 0:KC * ncols].rearrange("p (kc n) -> p kc n", kc=KC)
                    cv = c_[:, 0:KC * ncols].rearrange("p (kc n) -> p kc n", kc=KC)
                    o = 0
                    for (c0, n) in segs:
                        S.dma(sv[:, :, o:o + n], srcv[:, :, c0:c0 + n], writes=[sB])
                        o += n
                    if sk is None or sk == "half":
                        e = engs[k % 2]
                        if sk is None:
                            cpy(e, c_[:, 0:KC * ncols], s_[:, 0:KC * ncols], [sB], [cB])
                        else:
                            ts(e, c_[:, 0:KC * ncols], s_[:, 0:KC * ncols], 0.5, None, ALU.mult, None, [sB], [cB])
                    else:
                        for kc in range(KC):
                            if sk == "gmix":
                                g = P(l, "gmix", kc, kc + 1)
                            elif sk == "gffn":
                                g = P(l, "gffn", kc, kc + 1)
                            else:
                                i = int(sk[-1])
                                g = P(l, "gbr", 4 * i + kc, 4 * i + kc + 1)
                            e = engs[kc % 3]
                            if e == "act":
                                act(cv[:, kc, :], sv[:, kc, :], AF.Copy, [sB, PPb], [cB], scale=g)
                            else:
                                ts(e, cv[:, kc, :], sv[:, kc, :], g, None, ALU.mult, None, [sB, PPb], [cB])
                    S.dma(ws_d[l, bi, :, 0:KC * ncols], c_[:, 0:KC * ncols], reads=[cB], writes=[wsb])
                    k += 1
            S.barrier()

        NSLOT = 4
        ring = [sb([128, 4096], BF16) for _ in range(NSLOT)]
        ringb = [Buf() for _ in range(NSLOT)]

        units = [(s, t, l) for s in range(NSEQ) for t in range(NT) for l in range(L)]
        stream = [(l, bi) for (_, _, l) in units for bi in range([0, 21, NBLK][stage])]
        _ws = {"next_load": 0, "next_use": 0}

        def _issue_load():
            i = _ws["next_load"]
            if i >= len(stream):
                return
            l, bi = stream[i]
            _, segs, KC, _ = blkdefs[bi]
            n = KC * sum(nn for _, nn in segs)
            S.dma(ring[i % NSLOT][:, 0:n], ws_d[l, bi, :, 0:n], reads=[wsb], writes=[ringb[i % NSLOT]])
            _ws["next_load"] = i + 1

        def wnext(l, bi):
            i = _ws["next_use"]
            assert stream[i] == (l, bi), (stream[i], l, bi)
            while _ws["next_load"] < min(len(stream), i + NSLOT - 2):
                _issue_load()
            _ws["next_use"] = i + 1
            _, segs, KC, _ = blkdefs[bi]
            ncols = sum(nn for _, nn in segs)
            v = ring[i % NSLOT][:, 0:KC * ncols].rearrange("p (kc n) -> p kc n", kc=KC)
            return v, ringb[i % NSLOT]

        X = sb([128, 8, T])
        Xb = Buf()
        hT = sb([128, 8, T], BF16)
        hTb = Buf()
        sq = [sb([128, T], BF16) for _ in range(3)]
        sqb = [Buf() for _ in range(3)]
        rstd = sb([128, T])
        rstdb = Buf()
        xin = sb([128, D])
        xinb = Buf()
        xout = xin
        xoutb = xinb

        sz = sb([128, NCH, 512], BF16); szb = [Buf() for _ in range(NCH)]
        xbuf = [sb([128, 3 + T]) for _ in range(2)]; xbufb = [Buf() for _ in range(2)]
        cacc = [sb([128, T]) for _ in range(2)]; caccb = [Buf() for _ in range(2)]
        xact = sb([128, 6, T], BF16); xactb = [Buf() for _ in range(6)]
        dtraw = sb([128, NCH, 8]); dtrawb = Buf()
        glrT = sb([16, T]); glrTb = Buf()
        q_tok = sb([128, NCH, 512], BF16); q_tokb = [Buf() for _ in range(NCH)]
        k_tok = sb([128, NCH, 512], BF16); k_tokb = [Buf() for _ in range(NCH)]
        v_tok = sb([128, NCH, 512], BF16); v_tokb = [Buf() for _ in range(NCH)]
        srg = sb([128, NCH, 512], BF16); srgb = [Buf() for _ in range(NCH)]
        ropeA = [sb([128, 512])] * 2; ropeAb = [Buf()] * 2
        ropeB = [sb([128, 512])] * 2; ropeBb = [Buf()] * 2
        gqT = sb([128, 2, T], BF16); gqTb = Buf()
        gkT = sb([128, 2, T], BF16); gkTb = Buf()
        gk_tok = sb([128, NCH, 256], BF16); gk_tokb = [Buf() for _ in range(NCH)]
        gv_tok = sb([128, NCH, 512], BF16); gv_tokb = [Buf() for _ in range(NCH)]
        sgr = sb([128, NCH, 512], BF16); sgrb = [Buf() for _ in range(NCH)]
        yT = [sb([128, 4, T], BF16) for _ in range(3)]; yTb = [Buf() for _ in range(3)]
        mrg = sb([128, 8, T]); mrgb = [Buf() for _ in range(8)]
        mT = sb([128, 8, T], BF16); mTb = Buf()
        gth = [sb([128, T])] * 2; gthb = [Buf()] * 2
        gtmp = [sb([128, T])] * 2; gtmpb = [Buf()] * 2
        fa = sb([128, 22, T], BF16); fab = [Buf() for _ in range(22)]
        ub = [sb([128, 2 + T]) for _ in range(2)] * 2; ubb = [Buf() for _ in range(2)] * 2
        facc = [sb([128, T]) for _ in range(4)]; faccb = [Buf() for _ in range(4)]
        fsg = [sb([128, T]) for _ in range(2)]; fsgb = [Buf() for _ in range(2)]
        Sssd = [sb([128, 256]) for _ in range(L)]; Sssdb = [Buf() for _ in range(L)]
        Sssd16 = [sb([128, 256], BF16) for _ in range(L)]; Sssd16b = [Buf() for _ in range(L)]
        Sret = [sb([128, 256]) for _ in range(L)]; Sretb = [Buf() for _ in range(L)]
        Sret16 = [sb([128, 256], BF16) for _ in range(L)]; Sret16b = [Buf() for _ in range(L)]
        Sgla = [sb([128, 256]) for _ in range(L)]; Sglab = [Buf() for _ in range(L)]
        Sgla16 = [sb([128, 256], BF16) for _ in range(L)]; Sgla16b = [Buf() for _ in range(L)]
        halo_s = [sb([128, 6, 3]) for _ in range(L)]; halo_sb = [Buf() for _ in range(L)]
        halo_f = [sb([128, 44, 2]) for _ in range(L)]; halo_fb = [Buf() for _ in range(L)]
        dtv = sb([128, 8]); la = sb([128, 8]); smallb = Buf()
        ex3 = sb([128, 24]); ex3b = Buf()
        decsel = sb([128, 4]); decselb = Buf()
        ncr = sb([8, 128]); ncrb = Buf()
        Bm = sb([128, 8, 128]); Bmb = Buf()
        Esb = [sb([128, 512], BF16) for _ in range(2)]; Esbb = [Buf() for _ in range(2)]
        Psb = [sb([128, 512], BF16) for _ in range(2)]; Psbb = [Buf() for _ in range(2)]
        xs_tok = sb([128, 512], BF16); xs_tokb = Buf()
        vv = sb([128, 512], BF16); vvb = Buf()
        vw = sb([128, 512], BF16); vwb = Buf()
        B_tok = sb([128, 128], BF16); B_tokb = Buf()
        ytmp = [sb([128, 512]) for _ in range(2)]; ytmpb = [Buf() for _ in range(2)]
        ybf = sb([128, 512], BF16); ybfb = Buf()
        st8 = sb([128, 64]); st8b = Buf()
        kTs = sb([128, 4, 128], BF16); kTsb = Buf()
        kw = sb([128, 512], BF16); kwb = Buf()
        gL = sb([128, 256]); gLb = Buf()
        gE = sb([128, 256]); gEb = Buf()
        epos = sb([128, 2, 128]); eposb = Buf()
        eneg = sb([128, 2, 128]); enegb = Buf()
        gki = sb([128, 2, 128], BF16); gkib = Buf()
        gwst = sb([128, 256]); gwstb = Buf()
        gks = sb([128, 256], BF16); gksb = Buf()
        gdec = sb([128, 2]); gdecb = Buf()
        Cm = [sb([128, 128], BF16) for _ in range(2)]; Cmb = [Buf() for _ in range(2)]
        qm = [sb([128, 4, 128], BF16) for _ in range(2)]; qmb = [Buf() for _ in range(2)]
        qdm = [sb([128, 4, 128], BF16) for _ in range(2)]; qdmb = [Buf() for _ in range(2)]
        gqdm = [sb([128, 2, 128], BF16) for _ in range(2)]; gqdmb = [Buf() for _ in range(2)]
        for i_ in range(2):
            S.op("pool", lambda e, i_=i_: e.memset(Cm[i_][:], 0.0), [], [Cmb[i_]])
            S.op("pool", lambda e, i_=i_: e.memset(qm[i_][:], 0.0), [], [qmb[i_]])
            S.op("pool", lambda e, i_=i_: e.memset(qdm[i_][:], 0.0), [], [qdmb[i_]])
            S.op("pool", lambda e, i_=i_: e.memset(gqdm[i_][:], 0.0), [], [gqdmb[i_]])

        dbg_list = []

        def dump(name, ap_, b, width):
            if debug and debug[0] == name:
                dbg_list.append((ap_, b, width))

        def rmsnorm():
            bk, bb = bank()
            for kc in range(8):
                s_, sB = sq[kc % 3], sqb[kc % 3]
                act(s_[:], X[:, kc, :], AF.Square, [Xb], [sB])
                mm(bk[:, 0:T], CBc("cmean"), s_[:], kc == 0, kc == 7, [sB, CBb], [bb], inc=True)
            act(rstd[:], bk[:, 0:T], AF.Ln, [bb], [rstdb], bias=RMS_EPS)
            act(rstd[:], rstd[:], AF.Exp, [rstdb], [rstdb], scale=-0.5)
            for kc in range(8):
                tt("pool" if kc % 2 else "dve", hT[:, kc, :], X[:, kc, :], rstd[:], ALU.mult, [Xb, rstdb], [hTb])

        def proj_feat(W, Wb, c0, n):
            bk, bb = bank()
            for kc in range(8):
                mm(bk[0:n, 0:T], W[:, kc, c0:c0 + n], hT[:, kc, :], kc == 0, kc == 7, [Wb, hTb], [bb])
            return bk, bb

        def proj_tok(W, Wb, c0, n, c):
            bk, bb = bank()
            for kc in range(8):
                mm(bk[:, 0:n], hT[:, kc, c * 128:(c + 1) * 128], W[:, kc, c0:c0 + n], kc == 0, kc == 7, [Wb, hTb], [bb])
            return bk, bb

        import os as _os
        SUB = int(_os.environ.get("SUB", "99"))
        SSUB = int(_os.environ.get("SSUB", "99"))
        RSUB = int(_os.environ.get("RSUB", "99"))

        def _drain(l, frm):
            for bi in range(frm, 21):
                wnext(l, bi)

        def mixer(l, first, pc0):
            rmsnorm()
            W, Wb = wnext(l, 0)
            for c in range(NCH):
                bk, bb = proj_tok(W, Wb, 0, 512, c)
                act(sz[:, c, :], bk[:, :], AF.Silu, [bb], [szb[c]])
            if SUB <= 0:
                return _drain(l, 1)
            W1, W1b = wnext(l, 1)
            W2, W2b = wnext(l, 2)
            for j in range(6):
                if j < 4:
                    bk, bb = proj_feat(W1, W1b, j * 128, 128)
                else:
                    bk, bb = proj_feat(W2, W2b, (j - 4) * 128, 128)
                xb_, xbB = xbuf[j % 2], xbufb[j % 2]
                ac_, acB = cacc[j % 2], caccb[j % 2]
                if first:
                    S.op("pool", lambda e, xb_=xb_: e.memset(xb_[:, 0:3], 0.0), [], [xbB])
                else:
                    cpy("pool", xb_[:, 0:3], halo_s[l][:, j, :], [halo_sb[l]], [xbB])
                act(xb_[:, 3:3 + T], bk[:, 0:T], AF.Copy, [bb], [xbB])
                act(ac_[:], bk[:, 0:T], AF.Identity, [bb, PPb], [acB],
                    bias=P(l, "cb", j, j + 1), scale=P(l, "cw", 18 + j, 19 + j))
                stt("dve", ac_[:], xb_[:, 2:2 + T], P(l, "cw", 12 + j, 13 + j), ac_[:], ALU.mult, ALU.add, [xbB, acB, PPb], [acB])
                stt("pool", ac_[:], xb_[:, 1:1 + T], P(l, "cw", 6 + j, 7 + j), ac_[:], ALU.mult, ALU.add, [xbB, acB, PPb], [acB])
                stt("dve", ac_[:], xb_[:, 0:T], P(l, "cw", j, j + 1), ac_[:], ALU.mult, ALU.add, [xbB, acB, PPb], [acB])
                cpy("pool", halo_s[l][:, j, :], xb_[:, T:T + 3], [xbB], [halo_sb[l]])
                act(xact[:, j, :], ac_[:], AF.Silu, [acB], [xactb[j]])
            for c in range(NCH):
                bk, bb = proj_tok(W2, W2b, 256, 8, c)
                tt("dve", dtraw[:, c, :], bk[:, 0:8], P(l, "dtb", 0, 8), ALU.add, [bb, PPb], [dtrawb])
            bk, bb = proj_feat(W2, W2b, 264, 16)
            cpy("act", glrT[:, :], bk[0:16, 0:T], [bb], [glrTb])
            if SUB <= 1:
                return _drain(l, 3)
            for bi, dst, dstb in ((3, q_tok, q_tokb), (4, k_tok, k_tokb)):
                W, Wb = wnext(l, bi)
                for c in range(NCH):
                    bk, bb = proj_tok(W, Wb, 0, 512, c)
                    pc = pc0 + c
                    rA, rAb = ropeA[c % 2], ropeAb[c % 2]
                    rB, rBb = ropeB[c % 2], ropeBb[c % 2]
                    bk3 = bk[:, :].rearrange("p (h d) -> p h d", d=64)
                    rA3 = rA[:, :].rearrange("p (h d) -> p h d", d=64)
                    rB3 = rB[:, :].rearrange("p (h d) -> p h d", d=64)
                    cosb = bc(C("cos2", pc * 64, pc * 64 + 64).unsqueeze(1), [128, 8, 64])
                    sn1 = bc(C("sin2", pc * 64, pc * 64 + 32).unsqueeze(1), [128, 8, 32])
                    sn2 = bc(C("sin2", pc * 64 + 32, pc * 64 + 64).unsqueeze(1), [128, 8, 32])
                    tt("dve", rA3, bk3, cosb, ALU.mult, [bb, CPb], [rAb])
                    tt("dve", rB3[:, :, 0:32], bk3[:, :, 32:64], sn1, ALU.mult, [bb, CPb], [rBb])
                    tt("dve", rB3[:, :, 32:64], bk3[:, :, 0:32], sn2, ALU.mult, [bb, CPb], [rBb])
                    tt("pool", dst[:, c, :], rA[:, :], rB[:, :], ALU.add, [rAb, rBb], [dstb[c]])
            W, Wb = wnext(l, 5)
            for c in range(NCH):
                bk, bb = proj_tok(W, Wb, 0, 512, c)
                cpy("act", v_tok[:, c, :], bk[:, :], [bb], [v_tokb[c]])
            W, Wb = wnext(l, 6)
            for c in range(NCH):
                bk, bb = proj_tok(W, Wb, 0, 512, c)
                act(srg[:, c, :], bk[:, :], AF.Silu, [bb], [srgb[c]])
            if SUB <= 2:
                return _drain(l, 7)
            W, Wb = wnext(l, 7)
            for j in range(2):
                bk, bb = proj_feat(W, Wb, j * 128, 128)
                cpy("act", gqT[:, j, :], bk[:, 0:T], [bb], [gqTb])
                bk, bb = proj_feat(W, Wb, 256 + j * 128, 128)
                cpy("act", gkT[:, j, :], bk[:, 0:T], [bb], [gkTb])
            for c in range(NCH):
                bk, bb = proj_tok(W, Wb, 256, 256, c)
                cpy("dve", gk_tok[:, c, :], bk[:, 0:256], [bb], [gk_tokb[c]])
            W, Wb = wnext(l, 8)
            for c in range(NCH):
                bk, bb = proj_tok(W, Wb, 0, 512, c)
                cpy("act", gv_tok[:, c, :], bk[:, :], [bb], [gv_tokb[c]])
            W, Wb = wnext(l, 9)
            for c in range(NCH):
                bk, bb = proj_tok(W, Wb, 0, 512, c)
                act(sgr[:, c, :], bk[:, :], AF.Silu, [bb], [sgrb[c]])
            if SUB <= 3:
                return _drain(l, 10)
            for c in range(NCH):
                ssd_chunk(l, c, first and c == 0)
                if SUB >= 5:
                    ret_chunk(l, c, first and c == 0)
                if SUB >= 6:
                    gla_chunk(l, c, first and c == 0)
            if SUB <= 6:
                return _drain(l, 10)
            for i in range(3):
                Wg0, Wg0b = wnext(l, 10 + 3 * i)
                Wg1, Wg1b = wnext(l, 11 + 3 * i)
                Wbr, Wbrb = wnext(l, 12 + 3 * i)
                for oc in range(8):
                    Wg, Wgb = (Wg0, Wg0b) if oc < 4 else (Wg1, Wg1b)
                    bk, bb = proj_feat(Wg, Wgb, (oc % 4) * 128, 128)
                    th, thb = gth[oc % 2], gthb[oc % 2]
                    act(th[:], bk[:, 0:T], AF.Tanh, [bb, PPb], [thb], bias=BGH[:, l, 8 * i + oc:8 * i + oc + 1], scale=0.5)
                    bk2, bb2 = bank()
                    for kc in range(4):
                        mm(bk2[:, 0:T], Wbr[:, kc, oc * 128:(oc + 1) * 128], yT[i][:, kc, :], kc == 0, kc == 3,
                           [Wbrb, yTb[i]], [bb2])
                    if i == 0:
                        stt("dve", mrg[:, oc, :], th[:], 1.0, bk2[:, 0:T], ALU.add, ALU.mult, [thb, bb2], [mrgb[oc]])
                    else:
                        g_, gB = gtmp[oc % 2], gtmpb[oc % 2]
                        stt("dve", g_[:], th[:], 1.0, bk2[:, 0:T], ALU.add, ALU.mult, [thb, bb2], [gB])
                        if i == 1:
                            tt("pool", mrg[:, oc, :], mrg[:, oc, :], g_[:], ALU.add, [gB, mrgb[oc]], [mrgb[oc]])
                        else:
                            tt("pool", mT[:, oc, :], mrg[:, oc, :], g_[:], ALU.add, [gB, mrgb[oc]], [mTb])
            for half in range(2):
                W, Wb = wnext(l, 19 + half)
                for o4 in range(4):
                    oc = half * 4 + o4
                    bk, bb = bank()
                    for kc in range(8):
                        mm(bk[:, 0:T], W[:, kc, o4 * 128:(o4 + 1) * 128], mT[:, kc, :], kc == 0, kc == 7, [Wb, mTb], [bb])
                    tt("dve", X[:, oc, :], X[:, oc, :], bk[:, 0:T], ALU.add, [Xb, bb], [Xb])

        def ffn(l, first):
            rmsnorm()
            for b in range(11):
                W, Wb = wnext(l, 21 + b)
                for jj in range(2):
                    j = 2 * b + jj
                    accs = []
                    for gv in range(2):
                        ci = j + 22 * gv
                        bk, bb = proj_feat(W, Wb, gv * 256 + jj * 128, 128)
                        u_, uB = ub[(2 * jj + gv) % 4], ubb[(2 * jj + gv) % 4]
                        a_, aB = facc[(2 * jj + gv) % 4], faccb[(2 * jj + gv) % 4]
                        if first:
                            S.op("pool", lambda e, u_=u_: e.memset(u_[:, 0:2], 0.0), [], [uB])
                        else:
                            cpy("pool", u_[:, 0:2], halo_f[l][:, ci, :], [halo_fb[l]], [uB])
                        act(u_[:, 2:2 + T], bk[:, 0:T], AF.Copy, [bb], [uB])
                        act(a_[:], bk[:, 0:T], AF.Identity, [bb, PPb], [aB],
                            bias=P(l, "fb", ci, ci + 1), scale=P(l, "fw", 88 + ci, 89 + ci))
                        stt("dve", a_[:], u_[:, 1:1 + T], P(l, "fw", 44 + ci, 45 + ci), a_[:], ALU.mult, ALU.add, [uB, aB, PPb], [aB])
                        stt("pool", a_[:], u_[:, 0:T], P(l, "fw", ci, ci + 1), a_[:], ALU.mult, ALU.add, [uB, aB, PPb], [aB])
                        cpy("pool", halo_f[l][:, ci, :], u_[:, T:T + 2], [uB], [halo_fb[l]])
                        accs.append((a_, aB))
                    g_, gB = fsg[jj], fsgb[jj]
                    act(g_[:], accs[0][0][:], AF.Silu, [accs[0][1]], [gB])
                    tt("dve", fa[:, j, :], g_[:], accs[1][0][:], ALU.mult, [gB, accs[1][1]], [fab[j]])
            for oc in range(8):
                W, Wb = wnext(l, 32 + oc)
                bk, bb = bank()
                for j in range(22):
                    mm(bk[:, 0:T], W[:, j, 0:128], fa[:, j, :], j == 0, j == 21, [Wb, fab[j]], [bb])
                tt("dve", X[:, oc, :], X[:, oc, :], bk[:, 0:T], ALU.add, [Xb, bb], [Xb])

        def ssd_chunk(l, c, zero_state):
            cs = slice(c * 128, (c + 1) * 128)
            act(dtv[:], dtraw[:, c, :], AF.Exp, [dtrawb], [smallb])
            act(dtv[:], dtv[:], AF.Ln, [smallb], [smallb], bias=1.0)
            tt("dve", la[:], dtv[:], AN[:, l, :], ALU.mult, [smallb, PPb], [smallb])
            bk, bb = bank()
            mm(bk[:, 0:8], C("triI"), la[:], True, True, [CPb, smallb], [bb], inc=False)
            mm(bk[:, 8:16], C("triS"), la[:], True, True, [CPb, smallb], [bb], inc=False)
            mm(bk[:, 16:24], C("ones"), la[:], True, True, [CPb, smallb], [bb], inc=False)
            mm(bk[0:8, 128:256], la[:], C("triI"), True, True, [CPb, smallb], [bb])
            act(ex3[:], bk[:, 0:24], AF.Exp, [bb], [ex3b])
            act(decsel[0:64, :], bk[0:64, 16:20], AF.Exp, [bb], [decselb])
            act(decsel[64:128, :], bk[64:128, 20:24], AF.Exp, [bb], [decselb])
            act(ncr[:], bk[0:8, 128:256], AF.Copy, [bb], [ncrb], scale=-1.0)
            if SSUB <= 1:
                return
            tt("pool", Bm[:], bc(C("triI").unsqueeze(1), [128, 8, 128]), bc(la[:, :].unsqueeze(2), [128, 8, 128]),
               ALU.mult, [CPb, smallb], [Bmb])
            if SSUB <= 2:
                return
            sk, skb = bank()
            for g in range(2):
                cpy("pool", Cm[g][g * 64:(g + 1) * 64, :], xact[g * 64:(g + 1) * 64, 5, cs], [xactb[5]], [Cmb[g]])
            for g in range(2):
                mm(sk[:, g * 128:(g + 1) * 128], xact[:, 4, cs], Cm[g][:, :],
                   True, True, [xactb[4], Cmb[g]], [skb], inc=(g == 1))
            if SSUB <= 3:
                return
            tk, tkb = bank()
            tkv = tk[:, 0:256].bitcast(BF16)
            for j in range(4):
                trp(tkv[:, j * 128:(j + 1) * 128], xact[:, j, cs], CBc("identb"), [xactb[j], CBb], [tkb], inc=(j == 3))
            cpy("act", xs_tok[:], tkv, [tkb], [xs_tokb])
            tt("dve", vv[:, :].rearrange("p (h d) -> p h d", d=64), xs_tok[:, :].rearrange("p (h d) -> p h d", d=64),
               bc(dtv[:, :].unsqueeze(2), [128, 8, 64]), ALU.mult, [xs_tokb, smallb], [vvb])
            tt("pool", vw[:, :].rearrange("p (h d) -> p h d", d=64), vv[:, :].rearrange("p (h d) -> p h d", d=64),
               bc(ex3[:, 8:16].unsqueeze(2), [128, 8, 64]), ALU.mult, [vvb, ex3b], [vwb])
            if SSUB <= 4:
                return
            tb, tbb = bank()
            tbv = tb[:, 0:64].bitcast(BF16)
            trp(tbv, xact[:, 4, cs], CBc("identb"), [xactb[4], CBb], [tbb])
            cpy("act", B_tok[:], tbv, [tbb], [B_tokb])
            if SSUB <= 5:
                return
            for g in range(2):
                dk, dkb = bank()
                mm(dk[:, :], C("ones"), Bm[:, 4 * g:4 * g + 4, :].rearrange("p h t -> p (h t)"), True, False, [CPb, Bmb], [dkb], inc=False)
                mm(dk[:, :], ncr[:], C("sel", g * 512, (g + 1) * 512, 0, 8), False, False, [ncrb, CPb], [dkb], inc=False)
                mm(dk[:, :], CBc("identb"), CBc("negmask4"), False, True, [CBb], [dkb])
                act(Esb[g][:], dk[:, :], AF.Exp, [dkb], [Esbb[g]])
                tt("dve", Psb[g][:, :].rearrange("p (h t) -> p h t", h=4), Esb[g][:, :].rearrange("p (h t) -> p h t", h=4),
                   bc(sk[:, g * 128:(g + 1) * 128].unsqueeze(1), [128, 4, 128]), ALU.mult, [Esbb[g], skb], [Psbb[g]])
            if SSUB <= 6:
                return
            ya, yab = bank()
            for h in range(8):
                g, hl = h // 4, h % 4
                mm(ya[:, h * 64:(h + 1) * 64], Psb[g][:, hl * 128:(hl + 1) * 128], vv[:, h * 64:(h + 1) * 64], True, True,
                   [Psbb[g], vvb], [yab], inc=(h == 7))
            y1, y1b = ytmp[0], ytmpb[0]
            if not zero_state:
                yb_, ybb_ = bank()
                for g in range(2):
                    mm(yb_[:, g * 256:(g + 1) * 256], Cm[g][:, :], Sssd16[l][:, :], True, True,
                       [Cmb[g], Sssd16b[l]], [ybb_], inc=(g == 1))
                tt("dve", y1[:, :].rearrange("p (h d) -> p h d", d=64), yb_[:, :].rearrange("p (h d) -> p h d", d=64),
                   bc(ex3[:, 0:8].unsqueeze(2), [128, 8, 64]), ALU.mult, [ybb_, ex3b], [y1b])
                tt("dve", y1[:], y1[:], ya[:, :], ALU.add, [y1b, yab], [y1b])
            else:
                cpy("dve", y1[:], ya[:, :], [yab], [y1b])
            y2, y2b = ytmp[1], ytmpb[1]
            tt("pool", y2[:, :].rearrange("p (h d) -> p h d", d=64), xs_tok[:, :].rearrange("p (h d) -> p h d", d=64),
               bc(P(l, "dsk", 0, 8).unsqueeze(2), [128, 8, 64]), ALU.mult, [xs_tokb, PPb], [y2b])
            tt("pool", y1[:], y1[:], y2[:], ALU.add, [y1b, y2b], [y1b])
            tt("pool", y1[:], y1[:], sz[:, c, :], ALU.mult, [y1b, szb[c]], [y1b])
            if SSUB <= 7:
                return
            su, sub_ = bank()
            for g in range(2):
                mm(su[g * 64:(g + 1) * 64, 0:256], B_tok[:, g * 64:(g + 1) * 64], vw[:, g * 256:(g + 1) * 256], True, True,
                   [B_tokb, vwb], [sub_], inc=(g == 1))
            if zero_state:
                cpy("dve", Sssd[l][:], su[:, 0:256], [sub_], [Sssdb[l]])
            else:
                tt("pool", Sssd[l][:, :].rearrange("p (h d) -> p h d", d=64), Sssd[l][:, :].rearrange("p (h d) -> p h d", d=64),
                   bc(decsel[:, :].unsqueeze(2), [128, 4, 64]), ALU.mult, [Sssdb[l], decselb], [Sssdb[l]])
                tt("dve", Sssd[l][:], Sssd[l][:], su[:, 0:256], ALU.add, [Sssdb[l], sub_], [Sssdb[l]])
            cpy("act", Sssd16[l][:], Sssd[l][:], [Sssdb[l]], [Sssd16b[l]])
            if SSUB <= 8:
                return
            act(y2[:, 0:256], y1[:, 0:256], AF.Square, [y1b], [y2b, st8b], accum=st8[:, 0:1])
            act(y2[:, 256:512], y1[:, 256:512], AF.Square, [y1b], [y2b, st8b], accum=st8[:, 1:2])
            act(st8[:, 2:4], st8[:, 0:2], AF.Ln, [st8b], [st8b], bias=GN_EPS, scale=1.0 / 256.0)
            act(st8[:, 2:4], st8[:, 2:4], AF.Exp, [st8b], [st8b], scale=-0.5)
            act(ybf[:, 0:256], y1[:, 0:256], AF.Copy, [y1b, st8b], [ybfb], scale=st8[:, 2:3])
            act(ybf[:, 256:512], y1[:, 256:512], AF.Copy, [y1b, st8b], [ybfb], scale=st8[:, 3:4])
            dump("yssm", y1, y1b, 512)
            finish_y(0, c)

        def finish_y(i, c):
            tk, tkb = bank()
            tkv = tk[:, 0:256].bitcast(BF16)
            for j in range(4):
                trp(tkv[:, j * 128:(j + 1) * 128], ybf[:, j * 128:(j + 1) * 128], CBc("identb"), [ybfb, CBb], [tkb], inc=(j == 3))
            cpy("act", yT[i][:, :, c * 128:(c + 1) * 128], tkv.rearrange("p (j t) -> p j t", j=4), [tkb], [yTb[i]])

        def ret_chunk(l, c, zero_state):
            tq, tqb = bank()
            tqv = tq[:, 0:256].bitcast(BF16)
            for j in range(4):
                trp(tqv[:, j * 128:(j + 1) * 128], q_tok[:, c, j * 128:(j + 1) * 128], CBc("identb"), [q_tokb[c], CBb], [tqb], inc=(j == 3))
            for hh in range(2):
                r0, r1 = hh * 64, hh * 64 + 64
                cpy("act", qm[hh][r0:r1, :, :].rearrange("p j t -> p (j t)"), tqv[r0:r1, :], [tqb], [qmb[hh]])
                tt("dve", qdm[hh][r0:r1, :, :].rearrange("p j t -> p (j t)"), qm[hh][r0:r1, :, :].rearrange("p j t -> p (j t)"),
                   C("qdec", 0, None, r0, r1), ALU.mult, [qmb[hh], CPb], [qdmb[hh]])
            tk_, tkb_ = bank()
            tkv = tk_[:, 0:256].bitcast(BF16)
            for j in range(4):
                trp(tkv[:, j * 128:(j + 1) * 128], k_tok[:, c, j * 128:(j + 1) * 128], CBc("identb"), [k_tokb[c], CBb], [tkb_], inc=(j == 3))
            cpy("act", kTs[:, :, :].rearrange("p j t -> p (j t)"), tkv, [tkb_], [kTsb])
            if RSUB <= 1:
                return
            tt("pool", kw[:, :].rearrange("p (h d) -> p h d", d=64), k_tok[:, c, :].rearrange("p (h d) -> p h d", d=64),
               bc(C("wtab").unsqueeze(2), [128, 8, 64]), ALU.mult, [k_tokb[c], CPb], [kwb])
            if RSUB <= 2:
                return
            for g in range(2):
                sk, skb = bank()
                for hl in range(4):
                    h = 4 * g + hl
                    ps = slice((h % 2) * 64, (h % 2) * 64 + 64)
                    mm(sk[:, hl * 128:(hl + 1) * 128], kTs[:, h // 2, :], qm[h % 2][:, h // 2, :], True, True, [kTsb, qmb[h % 2]], [skb], inc=(hl == 3))
                tt("dve", Psb[g][:], sk[:, :], C("dret", g * 512, (g + 1) * 512), ALU.mult, [skb, CPb], [Psbb[g]])
            ya, yab = bank()
            for h in range(8):
                g, hl = h // 4, h % 4
                ps = slice((h % 2) * 64, (h % 2) * 64 + 64)
                mm(ya[:, h * 64:(h + 1) * 64], Psb[g][:, hl * 128:(hl + 1) * 128], v_tok[:, c, h * 64:(h + 1) * 64], True, zero_state,
                   [Psbb[g], v_tokb[c]], [yab], inc=(zero_state and h == 7))
                if not zero_state:
                    mm(ya[:, h * 64:(h + 1) * 64], qdm[h % 2][:, h // 2, :], Sret16[l][:, (h // 2) * 64:(h // 2) * 64 + 64], False, True,
                       [qdmb[h % 2], Sret16b[l]], [yab], inc=(h == 7))
            if RSUB <= 3:
                return
            su, sub_ = bank()
            for h in range(8):
                ps = slice((h % 2) * 64, (h % 2) * 64 + 64)
                mm(su[ps, (h // 2) * 64:(h // 2) * 64 + 64], kw[:, h * 64:(h + 1) * 64], v_tok[:, c, h * 64:(h + 1) * 64], True, True,
                   [kwb, v_tokb[c]], [sub_], inc=(h == 7))
            if zero_state:
                cpy("dve", Sret[l][:], su[:, 0:256], [sub_], [Sretb[l]])
            else:
                tt("pool", Sret[l][:, :].rearrange("p (j d) -> p j d", d=64), Sret[l][:, :].rearrange("p (j d) -> p j d", d=64),
                   bc(C("decs").unsqueeze(2), [128, 4, 64]), ALU.mult, [Sretb[l], CPb], [Sretb[l]])
                tt("dve", Sret[l][:], Sret[l][:], su[:, 0:256], ALU.add, [Sretb[l], sub_], [Sretb[l]])
            cpy("act", Sret16[l][:], Sret[l][:], [Sretb[l]], [Sret16b[l]])
            if RSUB <= 4:
                return
            y1, y1b = ytmp[0], ytmpb[0]
            y2, y2b = ytmp[1], ytmpb[1]
            for h in range(8):
                act(y2[:, h * 64:(h + 1) * 64], ya[:, h * 64:(h + 1) * 64], AF.Identity, [yab], [y2b, st8b], accum=st8[:, h:h + 1])
                act(y2[:, h * 64:(h + 1) * 64], ya[:, h * 64:(h + 1) * 64], AF.Square, [yab], [y2b, st8b], accum=st8[:, 8 + h:9 + h])
            ts("dve", st8[:, 16:24], st8[:, 0:8], 1.0 / 64.0, None, ALU.mult, None, [st8b], [st8b])
            tt("dve", st8[:, 24:32], st8[:, 16:24], st8[:, 16:24], ALU.mult, [st8b], [st8b])
            stt("dve", st8[:, 32:40], st8[:, 8:16], 1.0 / 64.0, st8[:, 24:32], ALU.mult, ALU.subtract, [st8b], [st8b])
            act(st8[:, 32:40], st8[:, 32:40], AF.Ln, [st8b], [st8b], bias=GN_EPS)
            act(st8[:, 32:40], st8[:, 32:40], AF.Exp, [st8b], [st8b], scale=-0.5)
            tt("dve", y1[:, :].rearrange("p (h d) -> p h d", d=64), ya[:, :].rearrange("p (h d) -> p h d", d=64),
               bc(st8[:, 16:24].unsqueeze(2), [128, 8, 64]), ALU.subtract, [yab, st8b], [y1b])
            tt("pool", y1[:, :].rearrange("p (h d) -> p h d", d=64), y1[:, :].rearrange("p (h d) -> p h d", d=64),
               bc(st8[:, 32:40].unsqueeze(2), [128, 8, 64]), ALU.mult, [y1b, st8b], [y1b])
            tt("pool", ybf[:], y1[:], srg[:, c, :], ALU.mult, [y1b, srgb[c]], [ybfb])
            dump("yret", y1, y1b, 512)
            finish_y(1, c)

        def gla_chunk(l, c, zero_state):
            cs = slice(c * 128, (c + 1) * 128)
            al, alb = bank()
            mm(al[:, 0:256], glrT[:, cs], WA2[:, l, :], True, False, [glrTb, PPb], [alb], inc=False)
            mm(al[:, 0:256], C("ones", 0, 128, 0, 1), BA[:, l * 256:(l + 1) * 256], False, True, [CPb, PPb], [alb])
            act(gE[:], al[:, 0:256], AF.Exp, [alb], [gEb], scale=-1.0)
            act(gL[:], gE[:], AF.Ln, [gEb], [gLb], bias=1.0)
            ck, ckb = bank()
            for j in range(2):
                mm(ck[:, j * 128:(j + 1) * 128], gL[:, j * 128:(j + 1) * 128], C("triI16"), True, True, [gLb, CPb], [ckb], inc=False)
            mm(ck[:, 256:512], C("triS16"), gL[:], True, True, [gLb, CPb], [ckb])
            act(epos[:, :, :].rearrange("p j t -> p (j t)"), ck[:, 0:256], AF.Exp, [ckb], [eposb], bias=math.log(0.125))
            act(eneg[:, :, :].rearrange("p j t -> p (j t)"), ck[:, 0:256], AF.Exp, [ckb], [enegb], scale=-1.0)
            act(gwst[:], ck[:, 256:512], AF.Exp, [ckb], [gwstb])
            tk_, tkb_ = bank()
            for j in range(2):
                mm(tk_[:, 2 * j:2 * j + 2], gL[:, j * 128:(j + 1) * 128], C("m16"), True, True, [gLb, CPb], [tkb_], inc=(j == 1))
            act(gdec[:], tk_[:, 0:4].rearrange("p (j two) -> p j two", two=2)[:, :, 0], AF.Exp, [tkb_], [gdecb])
            for hh in range(2):
                r0, r1 = hh * 64, hh * 64 + 64
                tt("pool", gqdm[hh][r0:r1, :, :], gqT[r0:r1, :, cs], epos[r0:r1, :, :], ALU.mult, [gqTb, eposb], [gqdmb[hh]])
            tt("pool", gki[:], gkT[:, :, cs], eneg[:], ALU.mult, [gkTb, enegb], [gkib])
            tt("dve", gks[:], gk_tok[:, c, :], gwst[:], ALU.mult, [gk_tokb[c], gwstb], [gksb])
            sk, skb = bank()
            for h in range(4):
                ps = slice((h % 2) * 64, (h % 2) * 64 + 64)
                mm(sk[:, h * 128:(h + 1) * 128], gki[:, h // 2, :], gqdm[h % 2][:, h // 2, :], True, True, [gkib, gqdmb[h % 2]], [skb], inc=(h == 3))
            tt("dve", Psb[0][:], sk[:, :], C("causal4"), ALU.mult, [skb, CPb], [Psbb[0]])
            ya, yab = bank()
            for h in range(4):
                ps = slice((h % 2) * 64, (h % 2) * 64 + 64)
                mm(ya[:, h * 128:(h + 1) * 128], Psb[0][:, h * 128:(h + 1) * 128], gv_tok[:, c, h * 128:(h + 1) * 128], True, zero_state,
                   [Psbb[0], gv_tokb[c]], [yab], inc=(zero_state and h == 3))
                if not zero_state:
                    mm(ya[:, h * 128:(h + 1) * 128], gqdm[h % 2][:, h // 2, :], Sgla16[l][:, (h // 2) * 128:(h // 2) * 128 + 128], False, True,
                       [gqdmb[h % 2], Sgla16b[l]], [yab], inc=(h == 3))
            su, sub_ = bank()
            for h in range(4):
                ps = slice((h % 2) * 64, (h % 2) * 64 + 64)
                mm(su[ps, (h // 2) * 128:(h // 2) * 128 + 128], gks[:, h * 64:(h + 1) * 64], gv_tok[:, c, h * 128:(h + 1) * 128], True, True,
                   [gksb, gv_tokb[c]], [sub_], inc=(h == 3))
            if zero_state:
                cpy("dve", Sgla[l][:], su[:, 0:256], [sub_], [Sglab[l]])
            else:
                for j in range(2):
                    stt("dve", Sgla[l][:, j * 128:(j + 1) * 128], Sgla[l][:, j * 128:(j + 1) * 128], gdec[:, j:j + 1],
                        su[:, j * 128:(j + 1) * 128], ALU.mult, ALU.add, [Sglab[l], gdecb, sub_], [Sglab[l]])
            cpy("act", Sgla16[l][:], Sgla[l][:], [Sglab[l]], [Sgla16b[l]])
            y1, y1b = ytmp[0], ytmpb[0]
            y2, y2b = ytmp[1], ytmpb[1]
            for g in range(4):
                act(y2[:, g * 128:(g + 1) * 128], ya[:, g * 128:(g + 1) * 128], AF.Square, [yab], [y2b, st8b], accum=st8[:, 40 + g:41 + g])
            act(st8[:, 44:48], st8[:, 40:44], AF.Ln, [st8b], [st8b], bias=GN_EPS, scale=1.0 / 128.0)
            act(st8[:, 44:48], st8[:, 44:48], AF.Exp, [st8b], [st8b], scale=-0.5)
            tt("dve", y1[:, :].rearrange("p (h d) -> p h d", d=128), ya[:, :].rearrange("p (h d) -> p h d", d=128),
               bc(st8[:, 44:48].unsqueeze(2), [128, 4, 128]), ALU.mult, [yab, st8b], [y1b])
            tt("pool", ybf[:], y1[:], sgr[:, c, :], ALU.mult, [y1b, sgrb[c]], [ybfb])
            dump("ygla", y1, y1b, 512)
            finish_y(2, c)

        outb = Buf()
        for s in range(NSEQ):
            for t in range(NT):
                t0 = t * T
                for c in range(NCH):
                    S.dma(xin[:], x_d[s, t0 + c * 128:t0 + (c + 1) * 128, :], writes=[xinb])
                    for half in range(2):
                        bk, bb = bank()
                        for k4 in range(4):
                            kc = half * 4 + k4
                            trp(bk[:, k4 * 128:(k4 + 1) * 128], xin[:, kc * 128:(kc + 1) * 128], C("identf"), [xinb, CPb], [bb], inc=(k4 == 3))
                        cpy("act", X[:, half * 4:half * 4 + 4, c * 128:(c + 1) * 128], bk[:, :].rearrange("p (k t) -> p k t", k=4), [bb], [Xb])
                for l in range(L):
                    if stage >= 1:
                        mixer(l, t == 0, (t0 // 128))
                    if stage >= 2:
                        ffn(l, t == 0)
                bk, bb = bank()
                for kc in range(8):
                    s_, sB = sq[kc % 3], sqb[kc % 3]
                    act(s_[:], X[:, kc, :], AF.Square, [Xb], [sB])
                    mm(bk[:, 0:T], CBc("cmean"), s_[:], kc == 0, kc == 7, [sB, CBb], [bb], inc=True)
                act(rstd[:], bk[:, 0:T], AF.Ln, [bb], [rstdb], bias=RMS_EPS)
                act(rstd[:], rstd[:], AF.Exp, [rstdb], [rstdb], scale=-0.5)
                for kc in range(8):
                    stt("dve", X[:, kc, :], X[:, kc, :], PP[:, PP_L * L + kc:PP_L * L + kc + 1], rstd[:], ALU.mult, ALU.mult,
                        [Xb, rstdb, PPb], [Xb])
                for c in range(NCH):
                    for half in range(2):
                        bk, bb = bank()
                        for k4 in range(4):
                            kc = half * 4 + k4
                            trp(bk[:, k4 * 128:(k4 + 1) * 128], X[:, kc, c * 128:(c + 1) * 128], C("identf"), [Xb, CPb], [bb], inc=(k4 == 3))
                        cpy("act", xout[:, half * 512:(half + 1) * 512], bk[:, :], [bb], [xoutb])
                    S.dma(out_d[s, t0 + c * 128:t0 + (c + 1) * 128, :], xout[:], reads=[xoutb], writes=[outb])
        if debug and dbg_list:
            o = 0
            for ap_, b, width in dbg_list:
                S.dma(dbg_d[:, o:o + width], ap_[:, 0:width], reads=[b], writes=[outb])
                o += width
        S.finish()
        with nc.Block() as block:
            S.replay(block)
    print('n_inst', S.n_inst, {k: len(v.stream) for k, v in S.E.items()})
    return nc, cp_np, cbp_np


def _run(inputs, L, NSEQ, SEQLEN, ncores, debug=None, stage=2):
    nc, cp_np, cbp_np = build_nc(L, NSEQ, SEQLEN, debug=debug, stage=stage)
    p = {k: np.asarray(v) for k, v in inputs.items()}
    pp = _param_pack(L, p)
    f = lambda a: np.ascontiguousarray(np.asarray(a, np.float32))
    shared = {
        "w_in": f(p["w_in"]), "w_branch": f(p["w_branch"]), "w_out": f(p["w_out"]), "w_up": f(p["w_up"]),
        "w_down": f(p["w_down"]), "gla_w_alpha2": f(p["gla_w_alpha2"]), "gla_b_alpha": f(p["gla_b_alpha"]),
        "pp": pp, "cp": cp_np, "cbp": cbp_np,
    }
    x = f(p["x"])
    in_maps = []
    for i in range(ncores):
        m = dict(shared)
        m["x"] = np.ascontiguousarray(x[i * NSEQ:(i + 1) * NSEQ])
        in_maps.append(m)
    res = run_bass_kernel_spmd(nc, in_maps, core_ids=list(range(ncores)))
    out = np.concatenate([r["out"] for r in res.results], axis=0)
    if debug:
        return out, [r["dbg"] for r in res.results]
    return out


def kernel(**inputs):
    x = inputs["x"]
    B, SEQLEN, _ = x.shape
    L = inputs["w_in"].shape[0]
    ncores = 8
    return _run(inputs, L, B // ncores, SEQLEN, ncores).astype(np.float32)
```

```python
import math
from contextlib import ExitStack

import numpy as np
import ml_dtypes
import concourse.bass as bass
import concourse.mybir as mybir
from concourse.bass_utils import run_bass_kernel_spmd

F32 = mybir.dt.float32
BF16 = mybir.dt.bfloat16
AF = mybir.ActivationFunctionType
ALU = mybir.AluOpType
AX = mybir.AxisListType

D = 1024
NIN = 7960
DFF = 2816
NBLK = 40
RMS_EPS = 1e-6
GN_EPS = 1e-5


class Buf:
    __slots__ = ("last_w", "reads")

    def __init__(self):
        self.last_w = None
        self.reads = {}


class Eng:
    def __init__(self, name, sem, inc):
        self.name = name
        self.sem = sem
        self.inc = inc
        self.count = 0
        self.seen = {}
        self.stream = []
        self.pending = False


class Sched:
    NLANES = 12

    def __init__(self, nc, sems):
        self.nc = nc
        self.E = {}
        for i, n in enumerate(["pe", "act", "dve", "pool"]):
            self.E[n] = Eng(n, sems[i], 1)
        self.sp = Eng("sp", None, 0)
        self.lanes = [Eng("lane%d" % i, sems[4 + i], 16) for i in range(self.NLANES)]
        self.lane_rr = 0
        self.n_inst = 0

    def _deps(self, reads, writes):
        deps = {}
        for b in reads:
            if b.last_w is not None:
                e, c = b.last_w
                if deps.get(e, 0) < c:
                    deps[e] = c
        for b in writes:
            if b.last_w is not None:
                e, c = b.last_w
                if deps.get(e, 0) < c:
                    deps[e] = c
            for e, c in b.reads.items():
                if deps.get(e, 0) < c:
                    deps[e] = c
        return deps

    def _emit_waits(self, E, deps, skip_self=False):
        for e, c in deps.items():
            if e is E and skip_self:
                continue
            if E.seen.get(e, 0) >= c:
                continue
            E.seen[e] = c
            E.stream.append(("wait", e.sem, c))

    def op(self, eng, fn, reads=(), writes=(), inc=True):
        E = self.E[eng]
        deps = self._deps(reads, writes)
        self._emit_waits(E, deps, skip_self=(eng == "pe"))
        stamp = E.count + E.inc
        if inc:
            E.count = stamp
            E.pending = False
        else:
            E.pending = True
        E.stream.append(("inst", fn, inc))
        for b in reads:
            b.reads[E] = stamp
        for b in writes:
            b.last_w = (E, stamp)
            b.reads = {}
        self.n_inst += 1

    def dma(self, out, in_, reads=(), writes=()):
        L = self.lanes[self.lane_rr]
        self.lane_rr = (self.lane_rr + 1) % self.NLANES
        deps = self._deps(reads, writes)
        if L.count > 0:
            deps[L] = max(deps.get(L, 0), L.count)
        self._emit_waits(self.sp, deps)
        L.count += 16
        stamp = L.count
        self.sp.stream.append(("dma", out, in_, L.sem))
        for b in reads:
            b.reads[L] = stamp
        for b in writes:
            b.last_w = (L, stamp)
            b.reads = {}
        self.n_inst += 1

    def barrier(self):
        allE = list(self.E.values()) + self.lanes
        for E in list(self.E.values()) + [self.sp]:
            deps = {e: e.count for e in allE if e.count > 0 and e is not E}
            self._emit_waits(E, deps)

    def finish(self):
        deps = {e: e.count for e in list(self.E.values()) + self.lanes if e.count > 0}
        self._emit_waits(self.sp, deps)

    def replay(self, block):
        def run(E, eng):
            for item in E.stream:
                if item[0] == "wait":
                    eng.wait_ge(item[1], item[2])
                elif item[0] == "inst":
                    ins = item[1](eng)
                    if item[2]:
                        ins.then_inc(E.sem, 1)
                else:
                    eng.dma_start(out=item[1], in_=item[2]).then_inc(item[3], 16)

        for E in self.E.values():
            assert not E.pending, E.name

        @block.tensor
        def _(e):
            run(self.E["pe"], e)

        @block.scalar
        def _(e):
            run(self.E["act"], e)

        @block.vector
        def _(e):
            run(self.E["dve"], e)

        @block.gpsimd
        def _(e):
            run(self.E["pool"], e)

        @block.sync
        def _(e):
            run(self.sp, e)


def _const_pack(seqlen):
    nchs = seqlen // 128
    s = np.arange(128)[:, None].astype(np.float64)
    t = np.arange(128)[None, :].astype(np.float64)
    le = (s <= t)
    cols = {}
    cols["triI"] = le.astype(np.float64)
    cols["triS"] = (s > t).astype(np.float64)
    cols["ones"] = np.ones((128, 128))
    cols["triI16"] = -le.astype(np.float64) / 16.0
    cols["triS16"] = -(s > t).astype(np.float64) / 16.0
    cols["m16"] = -np.ones((128, 2)) / 16.0
    cols["causal4"] = np.tile(le.astype(np.float64), (1, 4))
    lg = np.log(1.0 - np.exp2(-5.0 - np.arange(8, dtype=np.float64)))
    dret = np.zeros((128, 8, 128))
    for h in range(8):
        dret[:, h, :] = np.where(le, np.exp(lg[h] * (t - s)), 0.0) * 0.125
    cols["dret"] = dret.reshape(128, 1024)
    qd = np.zeros((128, 4, 128))
    decs = np.zeros((128, 4))
    for j in range(4):
        for hh in range(2):
            h = 2 * j + hh
            qd[hh * 64:(hh + 1) * 64, j, :] = np.exp(lg[h] * (np.arange(128) + 1.0))[None, :] * 0.125
            decs[hh * 64:(hh + 1) * 64, j] = np.exp(lg[h] * 128.0)
    cols["qdec"] = qd.reshape(128, 512)
    cols["decs"] = decs
    wt = np.zeros((128, 8))
    for h in range(8):
        wt[:, h] = np.exp(lg[h] * (127.0 - np.arange(128)))
    cols["wtab"] = wt
    inv = 10000.0 ** (-np.arange(32, dtype=np.float32) / 32.0)
    pos = np.arange(seqlen, dtype=np.float32)
    ang = (pos[:, None] * inv[None, :]).astype(np.float32)
    cos = np.cos(ang).astype(np.float64)
    sin = np.sin(ang).astype(np.float64)
    cos2 = np.concatenate([cos, cos], axis=1).reshape(nchs, 128, 64).transpose(1, 0, 2)
    sin2 = np.concatenate([-sin, sin], axis=1).reshape(nchs, 128, 64).transpose(1, 0, 2)
    cols["cos2"] = cos2.reshape(128, nchs * 64)
    cols["sin2"] = sin2.reshape(128, nchs * 64)
    sel = np.zeros((128, 2, 4, 128))
    for g in range(2):
        for hl in range(4):
            sel[4 * g + hl, g, hl, :] = 1.0
    cols["sel"] = sel.reshape(128, 1024)
    cols["identf"] = np.eye(128)
    off = {}
    o = 0
    arrs = []
    for k, v in cols.items():
        off[k] = (o, v.shape[1])
        o += v.shape[1]
        arrs.append(v)
    cp = np.ascontiguousarray(np.concatenate(arrs, axis=1).astype(np.float32))
    cb = {}
    cb["identb"] = np.eye(128)
    cb["negmask4"] = np.tile(np.where(le, 0.0, -30000.0), (1, 4))
    cb["cmean"] = np.full((128, 128), 1.0 / 1024.0)
    offb = {}
    o = 0
    arrs = []
    for k, v in cb.items():
        offb[k] = (o, v.shape[1])
        o += v.shape[1]
        arrs.append(v)
    cbp = np.ascontiguousarray(np.concatenate(arrs, axis=1).astype(ml_dtypes.bfloat16))
    return cp, off, cbp, offb


PP_FIELDS = [("gmix", 8), ("gffn", 8), ("gbr", 12), ("cw", 24), ("cb", 6), ("bg", 24),
             ("fw", 132), ("fb", 44), ("dtb", 8), ("alog", 8), ("dsk", 8)]
PP_L = sum(n for _, n in PP_FIELDS)


def _pp_off(L):
    off = {}
    o = 0
    for k, n in PP_FIELDS:
        off[k] = o
        o += n
    return off, PP_L * L + 8


def _param_pack(L, p):
    off, tot = _pp_off(L)
    pp = np.zeros((128, tot), np.float32)

    def fm(v, nk):
        return np.asarray(v, np.float32).reshape(nk, 128).T

    for l in range(L):
        b = l * PP_L
        pp[:, b + off["gmix"]: b + off["gmix"] + 8] = fm(p["norm_mix_g"][l], 8)
        pp[:, b + off["gffn"]: b + off["gffn"] + 8] = fm(p["norm_ffn_g"][l], 8)
        for i, nm in enumerate(["ssm_norm_g", "ret_norm_g", "gla_norm_g"]):
            pp[:, b + off["gbr"] + 4 * i: b + off["gbr"] + 4 * i + 4] = fm(p[nm][l], 4)
        for j in range(4):
            pp[:, b + off["cw"] + 6 * j: b + off["cw"] + 6 * j + 6] = fm(p["ssm_conv_w"][l, j], 6)
        pp[:, b + off["cb"]: b + off["cb"] + 6] = fm(p["ssm_conv_b"][l], 6)
        for i in range(3):
            pp[:, b + off["bg"] + 8 * i: b + off["bg"] + 8 * i + 8] = fm(p["b_gate"][l, i], 8)
        for j in range(3):
            pp[:, b + off["fw"] + 44 * j: b + off["fw"] + 44 * j + 44] = fm(p["ffn_conv_w"][l, j], 44)
        pp[:, b + off["fb"]: b + off["fb"] + 44] = fm(p["ffn_conv_b"][l], 44)
        pp[:, b + off["dtb"]: b + off["dtb"] + 8] = np.broadcast_to(p["ssm_dt_bias"][l], (128, 8))
        pp[:, b + off["alog"]: b + off["alog"] + 8] = np.broadcast_to(p["ssm_a_log"][l], (128, 8))
        pp[:, b + off["dsk"]: b + off["dsk"] + 8] = np.broadcast_to(p["ssm_d"][l], (128, 8))
    pp[:, PP_L * L: PP_L * L + 8] = fm(p["norm_f_g"], 8)
    return pp


C_Z, C_XBC, C_DT, C_RQ, C_RK, C_RV, C_RG = 0, 512, 1280, 1288, 1800, 2312, 2824
C_GQ, C_GK, C_GV, C_GR, C_GLR, C_GATE = 3336, 3592, 3848, 4360, 4872, 4888


def _block_defs():
    blks = []
    blks.append(("w_in", [(C_Z, 512)], 8, "gmix"))
    blks.append(("w_in", [(C_XBC, 512)], 8, "gmix"))
    blks.append(("w_in", [(C_XBC + 512, 256), (C_DT, 8), (C_GLR, 16)], 8, "gmix"))
    blks.append(("w_in", [(C_RQ, 512)], 8, "gmix"))
    blks.append(("w_in", [(C_RK, 512)], 8, "gmix"))
    blks.append(("w_in", [(C_RV, 512)], 8, "gmix"))
    blks.append(("w_in", [(C_RG, 512)], 8, "gmix"))
    blks.append(("w_in", [(C_GQ, 512)], 8, "gmix"))
    blks.append(("w_in", [(C_GV, 512)], 8, "gmix"))
    blks.append(("w_in", [(C_GR, 512)], 8, "gmix"))
    for i in range(3):
        blks.append(("w_in", [(C_GATE + i * 1024, 512)], 8, "gmix"))
        blks.append(("w_in", [(C_GATE + i * 1024 + 512, 512)], 8, "gmix"))
        blks.append(("w_branch%d" % i, [(0, 1024)], 4, "gbr%d" % i))
    blks.append(("w_out", [(0, 512)], 8, "half"))
    blks.append(("w_out", [(512, 512)], 8, "half"))
    for b in range(11):
        blks.append(("w_up", [(2 * b * 128, 256), (DFF + 2 * b * 128, 256)], 8, "gffn"))
    for oc in range(8):
        blks.append(("w_down", [(oc * 128, 128)], 22, None))
    assert len(blks) == NBLK
    return blks


def build_nc(L, NSEQ, SEQLEN, T=256, debug=None, stage=2):
    NCH = T // 128
    NT = SEQLEN // T
    nc = bass.Bass("TRN2", target_bir_lowering=False)
    cp_np, coff, cbp_np, cboff = _const_pack(SEQLEN)
    ppoff, pptot = _pp_off(L)

    def dram(name, shape, dt=F32, kind="ExternalInput"):
        return nc.dram_tensor(name, list(shape), dt, kind=kind).ap()

    x_d = dram("x", [NSEQ, SEQLEN, D])
    w_in_d = dram("w_in", [L, D, NIN])
    w_br_d = dram("w_branch", [L, 3, 512, D])
    w_out_d = dram("w_out", [L, D, D])
    w_up_d = dram("w_up", [L, D, 2 * DFF])
    w_dn_d = dram("w_down", [L, DFF, D])
    wa2_d = dram("gla_w_alpha2", [L, 16, 256])
    ba_d = dram("gla_b_alpha", [L, 256])
    pp_d = dram("pp", [128, pptot])
    cp_d = dram("cp", list(cp_np.shape))
    cbp_d = dram("cbp", list(cbp_np.shape), BF16)
    out_d = dram("out", [NSEQ, SEQLEN, D], kind="ExternalOutput")
    ws_d = dram("wscratch", [L, NBLK, 128, 4096], BF16, kind="Internal")
    dbg_d = None
    if debug:
        dbg_d = dram("dbg", [128, debug[1]], kind="ExternalOutput")

    blkdefs = _block_defs()

    with ExitStack() as es:
        sems = [es.enter_context(nc.semaphore("s%d" % i)) for i in range(4 + Sched.NLANES)]
        S = Sched(nc, sems)
        _cnt = [0]

        def sb(shape, dt=F32):
            _cnt[0] += 1
            return es.enter_context(nc.sbuf_tensor("t%d" % _cnt[0], list(shape), dt))

        def act(out, in_, func, r, w, bias=0.0, scale=1.0, accum=None):
            if accum is None:
                S.op("act", lambda e: e.activation(out=out, in_=in_, func=func, bias=bias, scale=scale), r, w)
            else:
                S.op("act", lambda e: e.activation(out=out, in_=in_, func=func, bias=bias, scale=scale,
                                                   accum_out=accum), r, w)

        def tt(eng, out, a, b, op, r, w):
            S.op(eng, lambda e: e.tensor_tensor(out=out, in0=a, in1=b, op=op), r, w)

        def ts(eng, out, a, s1, s2, op0, op1, r, w):
            if s2 is None:
                S.op(eng, lambda e: e.tensor_scalar(out=out, in0=a, scalar1=s1, scalar2=None, op0=op0), r, w)
            else:
                S.op(eng, lambda e: e.tensor_scalar(out=out, in0=a, scalar1=s1, scalar2=s2, op0=op0, op1=op1), r, w)

        def stt(eng, out, a, s, b, op0, op1, r, w):
            S.op("dve", lambda e: e.scalar_tensor_tensor(out=out, in0=a, scalar=s, in1=b, op0=op0, op1=op1), r, w)

        def cpy(eng, out, in_, r, w):
            if eng == "act":
                S.op("act", lambda e: e.copy(out=out, in_=in_), r, w)
            else:
                S.op(eng, lambda e: e.tensor_copy(out=out, in_=in_), r, w)

        def mm(out, lhsT, rhs, start, stop, r, w, inc=None):
            if inc is None:
                inc = stop
            S.op("pe", lambda e: e.matmul(out, lhsT=lhsT, rhs=rhs, start=start, stop=stop), r, w, inc=inc)

        def trp(out, in_, ident, r, w, inc=True):
            S.op("pe", lambda e: e.transpose(out, in_, ident), r, w, inc=inc)

        def bc(ap, shape):
            return ap.to_broadcast(list(shape))

        CP = sb(cp_np.shape)
        CPb = Buf()
        CB = sb(cbp_np.shape, BF16)
        CBb = Buf()
        PP = sb([128, pptot])
        PPb = Buf()
        WA2 = sb([16, L, 256])
        BA = sb([1, L * 256])
        S.dma(CP[:], cp_d[:, :], writes=[CPb])
        S.dma(CB[:], cbp_d[:, :], writes=[CBb])
        S.dma(PP[:], pp_d[:, :], writes=[PPb])
        S.dma(WA2[:], wa2_d.rearrange("l r c -> r l c"), writes=[PPb])
        S.dma(BA[:], ba_d.rearrange("l c -> (l c)").unsqueeze(0), writes=[PPb])

        def C(name, lo=0, hi=None, p0=0, p1=128):
            o, n = coff[name]
            if hi is None:
                hi = n
            return CP[p0:p1, o + lo:o + hi]

        def CBc(name, lo=0, hi=None):
            o, n = cboff[name]
            if hi is None:
                hi = n
            return CB[:, o + lo:o + hi]

        def P(l, name, lo, hi):
            o = l * PP_L + ppoff[name]
            return PP[:, o + lo:o + hi]

        AN = sb([128, L, 8])
        BGH = sb([128, L, 24])
        for l in range(L):
            act(AN[:, l, :], P(l, "alog", 0, 8), AF.Exp, [PPb], [PPb])
            ts("dve", AN[:, l, :], AN[:, l, :], -1.0, None, ALU.mult, None, [PPb], [PPb])
            ts("dve", BGH[:, l, :], P(l, "bg", 0, 24), 0.5, None, ALU.mult, None, [PPb], [PPb])

        banks = [es.enter_context(nc.psum_tensor("pb%d" % i, [128, 512], F32)) for i in range(8)]
        bankb = [Buf() for _ in range(8)]
        _bk = [0]

        _bctx = [None]

        def bank():
            ctx = _bctx[0]
            if ctx is not None:
                i = ctx["ids"][ctx["k"] % len(ctx["ids"])]
                ctx["k"] += 1
                return banks[i], bankb[i]
            i = _bk[0]
            _bk[0] = (i + 1) % 8
            return banks[i], bankb[i]

        skAb = Buf()

        with ExitStack() as es2:
            stg = [es2.enter_context(nc.sbuf_tensor("stg%d" % i, [128, 4096], F32)) for i in range(2)]
            stgb = [Buf() for _ in range(2)]
            cvt = [es2.enter_context(nc.sbuf_tensor("cvt%d" % i, [128, 4096], BF16)) for i in range(2)]
            cvtb = [Buf() for _ in range(2)]
            wsb = Buf()
            k = 0
            engs = ["dve", "pool", "act"]
            for l in range(L):
                for bi, (mat, segs, KC, sk) in enumerate(blkdefs):
                    s_, c_ = stg[k % 2], cvt[k % 2]
                    sB, cB = stgb[k % 2], cvtb[k % 2]
                    ncols = sum(n for _, n in segs)
                    if mat == "w_in":
                        src = w_in_d[l]
                    elif mat.startswith("w_branch"):
                        src = w_br_d[l, int(mat[-1])]
                    elif mat == "w_out":
                        src = w_out_d[l]
                    elif mat == "w_up":
                        src = w_up_d[l]
                    else:
                        src = w_dn_d[l]
                    srcv = src.rearrange("(kc p) n -> p kc n", p=128)
                    sv = s_[:, 0:KC * ncols].rearrange("p (kc n) -> p kc n", kc=KC)
                    cv = c_[:, 0:KC * ncols].rearrange("p (kc n) -> p kc n", kc=KC)
                    o = 0
                    for (c0, n) in segs:
                        S.dma(sv[:, :, o:o + n], srcv[:, :, c0:c0 + n], writes=[sB])
                        o += n
                    if sk is None or sk == "half":
                        e = engs[k % 2]
                        if sk is None:
                            cpy(e, c_[:, 0:KC * ncols], s_[:, 0:KC * ncols], [sB], [cB])
                        else:
                            ts(e, c_[:, 0:KC * ncols], s_[:, 0:KC * ncols], 0.5, None, ALU.mult, None, [sB], [cB])
                    else:
                        for kc in range(KC):
                            if sk == "gmix":
                                g = P(l, "gmix", kc, kc + 1)
                            elif sk == "gffn":
                                g = P(l, "gffn", kc, kc + 1)
                            else:
                                i = int(sk[-1])
                                g = P(l, "gbr", 4 * i + kc, 4 * i + kc + 1)
                            e = engs[kc % 3]
                            if e == "act":
                                act(cv[:, kc, :], sv[:, kc, :], AF.Copy, [sB, PPb], [cB], scale=g)
                            else:
                                ts(e, cv[:, kc, :], sv[:, kc, :], g, None, ALU.mult, None, [sB, PPb], [cB])
                    S.dma(ws_d[l, bi, :, 0:KC * ncols], c_[:, 0:KC * ncols], reads=[cB], writes=[wsb])
                    k += 1
            S.barrier()

        NSLOT = 4
        ring = [sb([128, 4096], BF16) for _ in range(NSLOT)]
        ringb = [Buf() for _ in range(NSLOT)]

        units = [(s, t, l) for s in range(NSEQ) for t in range(NT) for l in range(L)]
        stream = [(l, bi) for (_, _, l) in units for bi in range([0, 21, NBLK][stage])]
        _ws = {"next_load": 0, "next_use": 0}

        def _issue_load():
            i = _ws["next_load"]
            if i >= len(stream):
                return
            l, bi = stream[i]
            _, segs, KC, _ = blkdefs[bi]
            n = KC * sum(nn for _, nn in segs)
            S.dma(ring[i % NSLOT][:, 0:n], ws_d[l, bi, :, 0:n], reads=[wsb], writes=[ringb[i % NSLOT]])
            _ws["next_load"] = i + 1

        def wnext(l, bi):
            i = _ws["next_use"]
            assert stream[i] == (l, bi), (stream[i], l, bi)
            while _ws["next_load"] < min(len(stream), i + NSLOT - 2):
                _issue_load()
            _ws["next_use"] = i + 1
            _, segs, KC, _ = blkdefs[bi]
            ncols = sum(nn for _, nn in segs)
            v = ring[i % NSLOT][:, 0:KC * ncols].rearrange("p (kc n) -> p kc n", kc=KC)
            return v, ringb[i % NSLOT]

        X = sb([128, 8, T])
        Xb = Buf()
        hT = sb([128, 8, T], BF16)
        hTb = Buf()
        sq = [sb([128, T], BF16) for _ in range(3)]
        sqb = [Buf() for _ in range(3)]
        rstd = sb([128, T])
        rstdb = Buf()
        xin = sb([128, D])
        xinb = Buf()
        xout = xin
        xoutb = xinb

        sz = sb([128, NCH, 512], BF16); szb = [Buf() for _ in range(NCH)]
        xbuf = [sb([128, 3 + T]) for _ in range(2)]; xbufb = [Buf() for _ in range(2)]
        cacc = [sb([128, T]) for _ in range(2)]; caccb = [Buf() for _ in range(2)]
        xact = sb([128, 6, T], BF16); xactb = [Buf() for _ in range(6)]
        dtraw = sb([128, NCH, 8]); dtrawb = Buf()
        glrT = sb([16, T]); glrTb = Buf()
        q_tok = sb([128, NCH, 512], BF16); q_tokb = [Buf() for _ in range(NCH)]
        k_tok = sb([128, NCH, 512], BF16); k_tokb = [Buf() for _ in range(NCH)]
        v_tok = sb([128, NCH, 512], BF16); v_tokb = [Buf() for _ in range(NCH)]
        srg = sb([128, NCH, 512], BF16); srgb = [Buf() for _ in range(NCH)]
        ropeA = [sb([128, 512])] * 2; ropeAb = [Buf()] * 2
        ropeB = [sb([128, 512])] * 2; ropeBb = [Buf()] * 2
        gqT = sb([128, 2, T], BF16); gqTb = Buf()
        gkT = sb([128, 2, T], BF16); gkTb = Buf()
        gk_tok = sb([128, NCH, 256], BF16); gk_tokb = [Buf() for _ in range(NCH)]
        gv_tok = sb([128, NCH, 512], BF16); gv_tokb = [Buf() for _ in range(NCH)]
        sgr = sb([128, NCH, 512], BF16); sgrb = [Buf() for _ in range(NCH)]
        yT = [sb([128, 4, T], BF16) for _ in range(3)]; yTb = [Buf() for _ in range(3)]
        mrg = sb([128, 8, T]); mrgb = [Buf() for _ in range(8)]
        mT = sb([128, 8, T], BF16); mTb = Buf()
        gth = [sb([128, T])] * 2; gthb = [Buf()] * 2
        gtmp = [sb([128, T])] * 2; gtmpb = [Buf()] * 2
        fa = sb([128, 22, T], BF16); fab = [Buf() for _ in range(22)]
        ub = [sb([128, 2 + T]) for _ in range(2)] * 2; ubb = [Buf() for _ in range(2)] * 2
        facc = [sb([128, T]) for _ in range(4)]; faccb = [Buf() for _ in range(4)]
        fsg = [sb([128, T]) for _ in range(2)]; fsgb = [Buf() for _ in range(2)]
        Sssd = [sb([128, 256]) for _ in range(L)]; Sssdb = [Buf() for _ in range(L)]
        Sssd16 = [sb([128, 256], BF16) for _ in range(L)]; Sssd16b = [Buf() for _ in range(L)]
        Sret = [sb([128, 256]) for _ in range(L)]; Sretb = [Buf() for _ in range(L)]
        Sret16 = [sb([128, 256], BF16) for _ in range(L)]; Sret16b = [Buf() for _ in range(L)]
        Sgla = [sb([128, 256]) for _ in range(L)]; Sglab = [Buf() for _ in range(L)]
        Sgla16 = [sb([128, 256], BF16) for _ in range(L)]; Sgla16b = [Buf() for _ in range(L)]
        halo_s = [sb([128, 6, 3]) for _ in range(L)]; halo_sb = [Buf() for _ in range(L)]
        halo_f = [sb([128, 44, 2]) for _ in range(L)]; halo_fb = [Buf() for _ in range(L)]
        dtv = sb([128, 8]); la = sb([128, 8]); smallb = Buf()
        ex3 = sb([128, 24]); ex3b = Buf()
        decsel = sb([128, 4]); decselb = Buf()
        ncr = sb([8, 128]); ncrb = Buf()
        Bm = sb([128, 8, 128]); Bmb = Buf()
        Esb = [sb([128, 512], BF16) for _ in range(2)]; Esbb = [Buf() for _ in range(2)]
        Psb = [sb([128, 512], BF16) for _ in range(2)]; Psbb = [Buf() for _ in range(2)]
        xs_tok = sb([128, 512], BF16); xs_tokb = Buf()
        vv = sb([128, 512], BF16); vvb = Buf()
        vw = sb([128, 512], BF16); vwb = Buf()
        B_tok = sb([128, 128], BF16); B_tokb = Buf()
        ytmp = [sb([128, 512]) for _ in range(2)]; ytmpb = [Buf() for _ in range(2)]
        ybf = sb([128, 512], BF16); ybfb = Buf()
        st8 = sb([128, 64]); st8b = Buf()
        kTs = sb([128, 4, 128], BF16); kTsb = Buf()
        kw = sb([128, 512], BF16); kwb = Buf()
        gL = sb([128, 256]); gLb = Buf()
        gE = sb([128, 256]); gEb = Buf()
        epos = sb([128, 2, 128]); eposb = Buf()
        eneg = sb([128, 2, 128]); enegb = Buf()
        gki = sb([128, 2, 128], BF16); gkib = Buf()
        gwst = sb([128, 256]); gwstb = Buf()
        gks = sb([128, 256], BF16); gksb = Buf()
        gdec = sb([128, 2]); gdecb = Buf()
        Cm = [sb([128, 128], BF16) for _ in range(2)]; Cmb = [Buf() for _ in range(2)]
        qm = [sb([128, 4, 128], BF16) for _ in range(2)]; qmb = [Buf() for _ in range(2)]
        qdm = [sb([128, 4, 128], BF16) for _ in range(2)]; qdmb = [Buf() for _ in range(2)]
        gqdm = [sb([128, 2, 128], BF16) for _ in range(2)]; gqdmb = [Buf() for _ in range(2)]
        for i_ in range(2):
            S.op("pool", lambda e, i_=i_: e.memset(Cm[i_][:], 0.0), [], [Cmb[i_]])
            S.op("pool", lambda e, i_=i_: e.memset(qm[i_][:], 0.0), [], [qmb[i_]])
            S.op("pool", lambda e, i_=i_: e.memset(qdm[i_][:], 0.0), [], [qdmb[i_]])
            S.op("pool", lambda e, i_=i_: e.memset(gqdm[i_][:], 0.0), [], [gqdmb[i_]])

        assert T == 256
        faflat = fa[:, :, :].rearrange("p j t -> p (j t)")
        y1r = faflat[:, 0:4 * T].bitcast(F32); y1rB = fab[0:4]
        ybf_r = faflat[:, 4 * T:6 * T]; ybf_rB = fab[4:6]
        Psb_r = [faflat[:, 6 * T:8 * T], faflat[:, 8 * T:10 * T]]; Psb_rB = [fab[6:8], fab[8:10]]
        y1g = faflat[:, 10 * T:14 * T].bitcast(F32); y1gB = fab[10:14]
        ybf_g = faflat[:, 14 * T:16 * T]; ybf_gB = fab[14:16]
        Psb_g = faflat[:, 16 * T:18 * T]; Psb_gB = fab[16:18]
        st8r = Buf(); st8g = Buf()

        dbg_list = []

        def dump(name, ap_, b, width):
            if debug and debug[0] == name:
                dbg_list.append((ap_, b, width))

        def rmsnorm():
            bk, bb = bank()
            for kc in range(8):
                s_, sB = sq[kc % 3], sqb[kc % 3]
                act(s_[:], X[:, kc, :], AF.Square, [Xb], [sB])
                mm(bk[:, 0:T], CBc("cmean"), s_[:], kc == 0, kc == 7, [sB, CBb], [bb], inc=True)
            act(rstd[:], bk[:, 0:T], AF.Ln, [bb], [rstdb], bias=RMS_EPS)
            act(rstd[:], rstd[:], AF.Exp, [rstdb], [rstdb], scale=-0.5)
            for kc in range(8):
                tt("pool" if kc % 2 else "dve", hT[:, kc, :], X[:, kc, :], rstd[:], ALU.mult, [Xb, rstdb], [hTb])

        def proj_feat(W, Wb, c0, n):
            bk, bb = bank()
            for kc in range(8):
                mm(bk[0:n, 0:T], W[:, kc, c0:c0 + n], hT[:, kc, :], kc == 0, kc == 7, [Wb, hTb], [bb])
            return bk, bb

        def proj_tok(W, Wb, c0, n, c):
            bk, bb = bank()
            for kc in range(8):
                mm(bk[:, 0:n], hT[:, kc, c * 128:(c + 1) * 128], W[:, kc, c0:c0 + n], kc == 0, kc == 7, [Wb, hTb], [bb])
            return bk, bb

        import os as _os
        SUB = int(_os.environ.get("SUB", "99"))
        SSUB = int(_os.environ.get("SSUB", "99"))
        RSUB = int(_os.environ.get("RSUB", "99"))

        def _drain(l, frm):
            for bi in range(frm, 21):
                wnext(l, bi)

        def mixer(l, first, pc0):
            rmsnorm()
            W, Wb = wnext(l, 0)
            for c in range(NCH):
                bk, bb = proj_tok(W, Wb, 0, 512, c)
                act(sz[:, c, :], bk[:, :], AF.Silu, [bb], [szb[c]])
            if SUB <= 0:
                return _drain(l, 1)
            W1, W1b = wnext(l, 1)
            W2, W2b = wnext(l, 2)
            for j in range(6):
                if j < 4:
                    bk, bb = proj_feat(W1, W1b, j * 128, 128)
                else:
                    bk, bb = proj_feat(W2, W2b, (j - 4) * 128, 128)
                xb_, xbB = xbuf[j % 2], xbufb[j % 2]
                ac_, acB = cacc[j % 2], caccb[j % 2]
                if first:
                    S.op("pool", lambda e, xb_=xb_: e.memset(xb_[:, 0:3], 0.0), [], [xbB])
                else:
                    cpy("pool", xb_[:, 0:3], halo_s[l][:, j, :], [halo_sb[l]], [xbB])
                act(xb_[:, 3:3 + T], bk[:, 0:T], AF.Copy, [bb], [xbB])
                act(ac_[:], bk[:, 0:T], AF.Identity, [bb, PPb], [acB],
                    bias=P(l, "cb", j, j + 1), scale=P(l, "cw", 18 + j, 19 + j))
                stt("dve", ac_[:], xb_[:, 2:2 + T], P(l, "cw", 12 + j, 13 + j), ac_[:], ALU.mult, ALU.add, [xbB, acB, PPb], [acB])
                stt("pool", ac_[:], xb_[:, 1:1 + T], P(l, "cw", 6 + j, 7 + j), ac_[:], ALU.mult, ALU.add, [xbB, acB, PPb], [acB])
                stt("dve", ac_[:], xb_[:, 0:T], P(l, "cw", j, j + 1), ac_[:], ALU.mult, ALU.add, [xbB, acB, PPb], [acB])
                cpy("pool", halo_s[l][:, j, :], xb_[:, T:T + 3], [xbB], [halo_sb[l]])
                act(xact[:, j, :], ac_[:], AF.Silu, [acB], [xactb[j]])
            for c in range(NCH):
                bk, bb = proj_tok(W2, W2b, 256, 8, c)
                tt("dve", dtraw[:, c, :], bk[:, 0:8], P(l, "dtb", 0, 8), ALU.add, [bb, PPb], [dtrawb])
            bk, bb = proj_feat(W2, W2b, 264, 16)
            cpy("act", glrT[:, :], bk[0:16, 0:T], [bb], [glrTb])
            if SUB <= 1:
                return _drain(l, 3)
            for bi, dst, dstb in ((3, q_tok, q_tokb), (4, k_tok, k_tokb)):
                W, Wb = wnext(l, bi)
                for c in range(NCH):
                    bk, bb = proj_tok(W, Wb, 0, 512, c)
                    pc = pc0 + c
                    rA, rAb = ropeA[c % 2], ropeAb[c % 2]
                    rB, rBb = ropeB[c % 2], ropeBb[c % 2]
                    bk3 = bk[:, :].rearrange("p (h d) -> p h d", d=64)
                    rA3 = rA[:, :].rearrange("p (h d) -> p h d", d=64)
                    rB3 = rB[:, :].rearrange("p (h d) -> p h d", d=64)
                    cosb = bc(C("cos2", pc * 64, pc * 64 + 64).unsqueeze(1), [128, 8, 64])
                    sn1 = bc(C("sin2", pc * 64, pc * 64 + 32).unsqueeze(1), [128, 8, 32])
                    sn2 = bc(C("sin2", pc * 64 + 32, pc * 64 + 64).unsqueeze(1), [128, 8, 32])
                    tt("dve", rA3, bk3, cosb, ALU.mult, [bb, CPb], [rAb])
                    tt("dve", rB3[:, :, 0:32], bk3[:, :, 32:64], sn1, ALU.mult, [bb, CPb], [rBb])
                    tt("dve", rB3[:, :, 32:64], bk3[:, :, 0:32], sn2, ALU.mult, [bb, CPb], [rBb])
                    tt("pool", dst[:, c, :], rA[:, :], rB[:, :], ALU.add, [rAb, rBb], [dstb[c]])
            W, Wb = wnext(l, 5)
            for c in range(NCH):
                bk, bb = proj_tok(W, Wb, 0, 512, c)
                cpy("act", v_tok[:, c, :], bk[:, :], [bb], [v_tokb[c]])
            W, Wb = wnext(l, 6)
            for c in range(NCH):
                bk, bb = proj_tok(W, Wb, 0, 512, c)
                act(srg[:, c, :], bk[:, :], AF.Silu, [bb], [srgb[c]])
            if SUB <= 2:
                return _drain(l, 7)
            W, Wb = wnext(l, 7)
            for j in range(2):
                bk, bb = proj_feat(W, Wb, j * 128, 128)
                cpy("act", gqT[:, j, :], bk[:, 0:T], [bb], [gqTb])
                bk, bb = proj_feat(W, Wb, 256 + j * 128, 128)
                cpy("act", gkT[:, j, :], bk[:, 0:T], [bb], [gkTb])
            for c in range(NCH):
                bk, bb = proj_tok(W, Wb, 256, 256, c)
                cpy("dve", gk_tok[:, c, :], bk[:, 0:256], [bb], [gk_tokb[c]])
            W, Wb = wnext(l, 8)
            for c in range(NCH):
                bk, bb = proj_tok(W, Wb, 0, 512, c)
                cpy("act", gv_tok[:, c, :], bk[:, :], [bb], [gv_tokb[c]])
            W, Wb = wnext(l, 9)
            for c in range(NCH):
                bk, bb = proj_tok(W, Wb, 0, 512, c)
                act(sgr[:, c, :], bk[:, :], AF.Silu, [bb], [sgrb[c]])
            if SUB <= 3:
                return _drain(l, 10)
            for c in range(NCH):
                gens = [(ssd_chunk(l, c, first and c == 0), None),
                        (ret_chunk(l, c, first and c == 0), {"ids": [3, 4], "k": 0}),
                        (gla_chunk(l, c, first and c == 0), {"ids": [5, 6], "k": 0})]
                while gens:
                    for item in list(gens):
                        _bctx[0] = item[1]
                        try:
                            next(item[0])
                        except StopIteration:
                            gens.remove(item)
                _bctx[0] = None
            if SUB <= 6:
                return _drain(l, 10)
            for i in range(3):
                Wg0, Wg0b = wnext(l, 10 + 3 * i)
                Wg1, Wg1b = wnext(l, 11 + 3 * i)
                Wbr, Wbrb = wnext(l, 12 + 3 * i)
                for oc in range(8):
                    Wg, Wgb = (Wg0, Wg0b) if oc < 4 else (Wg1, Wg1b)
                    bk, bb = proj_feat(Wg, Wgb, (oc % 4) * 128, 128)
                    th, thb = gth[oc % 2], gthb[oc % 2]
                    act(th[:], bk[:, 0:T], AF.Tanh, [bb, PPb], [thb], bias=BGH[:, l, 8 * i + oc:8 * i + oc + 1], scale=0.5)
                    bk2, bb2 = bank()
                    for kc in range(4):
                        mm(bk2[:, 0:T], Wbr[:, kc, oc * 128:(oc + 1) * 128], yT[i][:, kc, :], kc == 0, kc == 3,
                           [Wbrb, yTb[i]], [bb2])
                    if i == 0:
                        stt("dve", mrg[:, oc, :], th[:], 1.0, bk2[:, 0:T], ALU.add, ALU.mult, [thb, bb2], [mrgb[oc]])
                    else:
                        g_, gB = gtmp[oc % 2], gtmpb[oc % 2]
                        stt("dve", g_[:], th[:], 1.0, bk2[:, 0:T], ALU.add, ALU.mult, [thb, bb2], [gB])
                        if i == 1:
                            tt("pool", mrg[:, oc, :], mrg[:, oc, :], g_[:], ALU.add, [gB, mrgb[oc]], [mrgb[oc]])
                        else:
                            tt("pool", mT[:, oc, :], mrg[:, oc, :], g_[:], ALU.add, [gB, mrgb[oc]], [mTb])
            for half in range(2):
                W, Wb = wnext(l, 19 + half)
                for o4 in range(4):
                    oc = half * 4 + o4
                    bk, bb = bank()
                    for kc in range(8):
                        mm(bk[:, 0:T], W[:, kc, o4 * 128:(o4 + 1) * 128], mT[:, kc, :], kc == 0, kc == 7, [Wb, mTb], [bb])
                    tt("dve", X[:, oc, :], X[:, oc, :], bk[:, 0:T], ALU.add, [Xb, bb], [Xb])

        def ffn(l, first):
            rmsnorm()
            for b in range(11):
                W, Wb = wnext(l, 21 + b)
                for jj in range(2):
                    j = 2 * b + jj
                    accs = []
                    for gv in range(2):
                        ci = j + 22 * gv
                        bk, bb = proj_feat(W, Wb, gv * 256 + jj * 128, 128)
                        u_, uB = ub[(2 * jj + gv) % 4], ubb[(2 * jj + gv) % 4]
                        a_, aB = facc[(2 * jj + gv) % 4], faccb[(2 * jj + gv) % 4]
                        if first:
                            S.op("pool", lambda e, u_=u_: e.memset(u_[:, 0:2], 0.0), [], [uB])
                        else:
                            cpy("pool", u_[:, 0:2], halo_f[l][:, ci, :], [halo_fb[l]], [uB])
                        act(u_[:, 2:2 + T], bk[:, 0:T], AF.Copy, [bb], [uB])
                        act(a_[:], bk[:, 0:T], AF.Identity, [bb, PPb], [aB],
                            bias=P(l, "fb", ci, ci + 1), scale=P(l, "fw", 88 + ci, 89 + ci))
                        stt("dve", a_[:], u_[:, 1:1 + T], P(l, "fw", 44 + ci, 45 + ci), a_[:], ALU.mult, ALU.add, [uB, aB, PPb], [aB])
                        stt("pool", a_[:], u_[:, 0:T], P(l, "fw", ci, ci + 1), a_[:], ALU.mult, ALU.add, [uB, aB, PPb], [aB])
                        cpy("pool", halo_f[l][:, ci, :], u_[:, T:T + 2], [uB], [halo_fb[l]])
                        accs.append((a_, aB))
                    g_, gB = fsg[jj], fsgb[jj]
                    act(g_[:], accs[0][0][:], AF.Silu, [accs[0][1]], [gB])
                    tt("dve", fa[:, j, :], g_[:], accs[1][0][:], ALU.mult, [gB, accs[1][1]], [fab[j]])
            for oc in range(8):
                W, Wb = wnext(l, 32 + oc)
                bk, bb = bank()
                for j in range(22):
                    mm(bk[:, 0:T], W[:, j, 0:128], fa[:, j, :], j == 0, j == 21, [Wb, fab[j]], [bb])
                tt("dve", X[:, oc, :], X[:, oc, :], bk[:, 0:T], ALU.add, [Xb, bb], [Xb])

        def ssd_chunk(l, c, zero_state):
            cs = slice(c * 128, (c + 1) * 128)
            act(dtv[:], dtraw[:, c, :], AF.Exp, [dtrawb], [smallb])
            act(dtv[:], dtv[:], AF.Ln, [smallb], [smallb], bias=1.0)
            tt("dve", la[:], dtv[:], AN[:, l, :], ALU.mult, [smallb, PPb], [smallb])
            bk, bb = banks[0], bankb[0]
            mm(bk[:, 0:8], C("triI"), la[:], True, True, [CPb, smallb], [bb], inc=False)
            mm(bk[:, 8:16], C("triS"), la[:], True, True, [CPb, smallb], [bb], inc=False)
            mm(bk[:, 16:24], C("ones"), la[:], True, True, [CPb, smallb], [bb], inc=False)
            mm(bk[0:8, 128:256], la[:], C("triI"), True, True, [CPb, smallb], [bb])
            act(ex3[:], bk[:, 0:24], AF.Exp, [bb], [ex3b])
            act(decsel[0:64, :], bk[0:64, 16:20], AF.Exp, [bb], [decselb])
            act(decsel[64:128, :], bk[64:128, 20:24], AF.Exp, [bb], [decselb])
            act(ncr[:], bk[0:8, 128:256], AF.Copy, [bb], [ncrb], scale=-1.0)
            yield
            tt("pool", Bm[:], bc(C("triI").unsqueeze(1), [128, 8, 128]), bc(la[:, :].unsqueeze(2), [128, 8, 128]),
               ALU.mult, [CPb, smallb], [Bmb])
            sk, skb = banks[7][:, 0:256], bankb[7]
            for g in range(2):
                cpy("pool", Cm[g][g * 64:(g + 1) * 64, :], xact[g * 64:(g + 1) * 64, 5, cs], [xactb[5]], [Cmb[g]])
            yield
            for g in range(2):
                mm(sk[:, g * 128:(g + 1) * 128], xact[:, 4, cs], Cm[g][:, :],
                   True, True, [xactb[4], Cmb[g]], [skb], inc=(g == 1))
            tk, tkb = banks[1], bankb[1]
            tkv = tk[:, 0:256].bitcast(BF16)
            for j in range(4):
                trp(tkv[:, j * 128:(j + 1) * 128], xact[:, j, cs], CBc("identb"), [xactb[j], CBb], [tkb], inc=(j == 3))
            tb, tbb = banks[2], bankb[2]
            tbv = tb[:, 0:64].bitcast(BF16)
            trp(tbv, xact[:, 4, cs], CBc("identb"), [xactb[4], CBb], [tbb])
            yield
            cpy("act", xs_tok[:], tkv, [tkb], [xs_tokb])
            cpy("act", B_tok[:], tbv, [tbb], [B_tokb])
            yield
            tt("dve", vv[:, :].rearrange("p (h d) -> p h d", d=64), xs_tok[:, :].rearrange("p (h d) -> p h d", d=64),
               bc(dtv[:, :].unsqueeze(2), [128, 8, 64]), ALU.mult, [xs_tokb, smallb], [vvb])
            tt("pool", vw[:, :].rearrange("p (h d) -> p h d", d=64), vv[:, :].rearrange("p (h d) -> p h d", d=64),
               bc(ex3[:, 8:16].unsqueeze(2), [128, 8, 64]), ALU.mult, [vvb, ex3b], [vwb])
            yield
            for g in range(2):
                dk, dkb = banks[1 + g], bankb[1 + g]
                mm(dk[:, :], C("ones"), Bm[:, 4 * g:4 * g + 4, :].rearrange("p h t -> p (h t)"), True, False, [CPb, Bmb], [dkb], inc=False)
                mm(dk[:, :], ncr[:], C("sel", g * 512, (g + 1) * 512, 0, 8), False, False, [ncrb, CPb], [dkb], inc=False)
                mm(dk[:, :], CBc("identb"), CBc("negmask4"), False, True, [CBb], [dkb])
                yield
                act(Esb[g][:], dk[:, :], AF.Exp, [dkb], [Esbb[g]])
                tt("dve", Psb[g][:, :].rearrange("p (h t) -> p h t", h=4), Esb[g][:, :].rearrange("p (h t) -> p h t", h=4),
                   bc(sk[:, g * 128:(g + 1) * 128].unsqueeze(1), [128, 4, 128]), ALU.mult, [Esbb[g], skb], [Psbb[g]])
                yield
            ya, yab = banks[1], bankb[1]
            for h in range(8):
                g, hl = h // 4, h % 4
                mm(ya[:, h * 64:(h + 1) * 64], Psb[g][:, hl * 128:(hl + 1) * 128], vv[:, h * 64:(h + 1) * 64], True, True,
                   [Psbb[g], vvb], [yab], inc=(h == 7))
            y1, y1b = ytmp[0], ytmpb[0]
            if not zero_state:
                yb_, ybb_ = banks[2], bankb[2]
                for g in range(2):
                    mm(yb_[:, g * 256:(g + 1) * 256], Cm[g][:, :], Sssd16[l][:, :], True, True,
                       [Cmb[g], Sssd16b[l]], [ybb_], inc=(g == 1))
                yield
                tt("dve", y1[:, :].rearrange("p (h d) -> p h d", d=64), yb_[:, :].rearrange("p (h d) -> p h d", d=64),
                   bc(ex3[:, 0:8].unsqueeze(2), [128, 8, 64]), ALU.mult, [ybb_, ex3b], [y1b])
                tt("dve", y1[:], y1[:], ya[:, :], ALU.add, [y1b, yab], [y1b])
            else:
                yield
                cpy("dve", y1[:], ya[:, :], [yab], [y1b])
            yield
            y2, y2b = ytmp[1], ytmpb[1]
            tt("pool", y2[:, :].rearrange("p (h d) -> p h d", d=64), xs_tok[:, :].rearrange("p (h d) -> p h d", d=64),
               bc(P(l, "dsk", 0, 8).unsqueeze(2), [128, 8, 64]), ALU.mult, [xs_tokb, PPb], [y2b])
            tt("pool", y1[:], y1[:], y2[:], ALU.add, [y1b, y2b], [y1b])
            tt("pool", y1[:], y1[:], sz[:, c, :], ALU.mult, [y1b, szb[c]], [y1b])
            yield
            su, sub_ = banks[0], bankb[0]
            for g in range(2):
                mm(su[g * 64:(g + 1) * 64, 0:256], B_tok[:, g * 64:(g + 1) * 64], vw[:, g * 256:(g + 1) * 256], True, True,
                   [B_tokb, vwb], [sub_], inc=(g == 1))
            yield
            if zero_state:
                cpy("dve", Sssd[l][:], su[:, 0:256], [sub_], [Sssdb[l]])
            else:
                tt("pool", Sssd[l][:, :].rearrange("p (h d) -> p h d", d=64), Sssd[l][:, :].rearrange("p (h d) -> p h d", d=64),
                   bc(decsel[:, :].unsqueeze(2), [128, 4, 64]), ALU.mult, [Sssdb[l], decselb], [Sssdb[l]])
                tt("dve", Sssd[l][:], Sssd[l][:], su[:, 0:256], ALU.add, [Sssdb[l], sub_], [Sssdb[l]])
            cpy("act", Sssd16[l][:], Sssd[l][:], [Sssdb[l]], [Sssd16b[l]])
            yield
            act(y2[:, 0:256], y1[:, 0:256], AF.Square, [y1b], [y2b, st8b], accum=st8[:, 0:1])
            act(y2[:, 256:512], y1[:, 256:512], AF.Square, [y1b], [y2b, st8b], accum=st8[:, 1:2])
            act(st8[:, 2:4], st8[:, 0:2], AF.Ln, [st8b], [st8b], bias=GN_EPS, scale=1.0 / 256.0)
            act(st8[:, 2:4], st8[:, 2:4], AF.Exp, [st8b], [st8b], scale=-0.5)
            yield
            act(ybf[:, 0:256], y1[:, 0:256], AF.Copy, [y1b, st8b], [ybfb], scale=st8[:, 2:3])
            act(ybf[:, 256:512], y1[:, 256:512], AF.Copy, [y1b, st8b], [ybfb], scale=st8[:, 3:4])
            yield
            yield from finish_y(0, c, ybf, [ybfb], (banks[1], bankb[1]))

        def finish_y(i, c, yb_ap, yb_bufs, bk_=None):
            tk, tkb = bk_ if bk_ is not None else bank()
            tkv = tk[:, 0:256].bitcast(BF16)
            for j in range(4):
                trp(tkv[:, j * 128:(j + 1) * 128], yb_ap[:, j * 128:(j + 1) * 128], CBc("identb"), list(yb_bufs) + [CBb], [tkb], inc=(j == 3))
            yield
            cpy("act", yT[i][:, :, c * 128:(c + 1) * 128], tkv.rearrange("p (j t) -> p j t", j=4), [tkb], [yTb[i]])

        def ret_chunk(l, c, zero_state):
            tq, tqb = bank()
            tqv = tq[:, 0:256].bitcast(BF16)
            for j in range(4):
                trp(tqv[:, j * 128:(j + 1) * 128], q_tok[:, c, j * 128:(j + 1) * 128], CBc("identb"), [q_tokb[c], CBb], [tqb], inc=(j == 3))
            tk_, tkb_ = bank()
            tkv = tk_[:, 0:256].bitcast(BF16)
            for j in range(4):
                trp(tkv[:, j * 128:(j + 1) * 128], k_tok[:, c, j * 128:(j + 1) * 128], CBc("identb"), [k_tokb[c], CBb], [tkb_], inc=(j == 3))
            yield
            for hh in range(2):
                r0, r1 = hh * 64, hh * 64 + 64
                cpy("act", qm[hh][r0:r1, :, :].rearrange("p j t -> p (j t)"), tqv[r0:r1, :], [tqb], [qmb[hh]])
                tt("dve", qdm[hh][r0:r1, :, :].rearrange("p j t -> p (j t)"), qm[hh][r0:r1, :, :].rearrange("p j t -> p (j t)"),
                   C("qdec", 0, None, r0, r1), ALU.mult, [qmb[hh], CPb], [qdmb[hh]])
            cpy("act", kTs[:, :, :].rearrange("p j t -> p (j t)"), tkv, [tkb_], [kTsb])
            yield
            tt("pool", kw[:, :].rearrange("p (h d) -> p h d", d=64), k_tok[:, c, :].rearrange("p (h d) -> p h d", d=64),
               bc(C("wtab").unsqueeze(2), [128, 8, 64]), ALU.mult, [k_tokb[c], CPb], [kwb])
            for g in range(2):
                sk, skb = bank()
                for hl in range(4):
                    h = 4 * g + hl
                    ps = slice((h % 2) * 64, (h % 2) * 64 + 64)
                    mm(sk[:, hl * 128:(hl + 1) * 128], kTs[:, h // 2, :], qm[h % 2][:, h // 2, :], True, True, [kTsb, qmb[h % 2]], [skb], inc=(hl == 3))
                yield
                tt("dve", Psb_r[g][:, :], sk[:, :], C("dret", g * 512, (g + 1) * 512), ALU.mult, [skb, CPb], Psb_rB[g])
            yield
            ya, yab = bank()
            for h in range(8):
                g, hl = h // 4, h % 4
                ps = slice((h % 2) * 64, (h % 2) * 64 + 64)
                mm(ya[:, h * 64:(h + 1) * 64], Psb_r[g][:, hl * 128:(hl + 1) * 128], v_tok[:, c, h * 64:(h + 1) * 64], True, zero_state,
                   Psb_rB[g] + [v_tokb[c]], [yab], inc=(zero_state and h == 7))
                if not zero_state:
                    mm(ya[:, h * 64:(h + 1) * 64], qdm[h % 2][:, h // 2, :], Sret16[l][:, (h // 2) * 64:(h // 2) * 64 + 64], False, True,
                       [qdmb[h % 2], Sret16b[l]], [yab], inc=(h == 7))
            yield
            su, sub_ = bank()
            for h in range(8):
                ps = slice((h % 2) * 64, (h % 2) * 64 + 64)
                mm(su[ps, (h // 2) * 64:(h // 2) * 64 + 64], kw[:, h * 64:(h + 1) * 64], v_tok[:, c, h * 64:(h + 1) * 64], True, True,
                   [kwb, v_tokb[c]], [sub_], inc=(h == 7))
            yield
            if zero_state:
                cpy("dve", Sret[l][:], su[:, 0:256], [sub_], [Sretb[l]])
            else:
                tt("pool", Sret[l][:, :].rearrange("p (j d) -> p j d", d=64), Sret[l][:, :].rearrange("p (j d) -> p j d", d=64),
                   bc(C("decs").unsqueeze(2), [128, 4, 64]), ALU.mult, [Sretb[l], CPb], [Sretb[l]])
                tt("dve", Sret[l][:], Sret[l][:], su[:, 0:256], ALU.add, [Sretb[l], sub_], [Sretb[l]])
            cpy("act", Sret16[l][:], Sret[l][:], [Sretb[l]], [Sret16b[l]])
            yield
            y1 = y1r
            for h in range(8):
                act(y1[:, h * 64:(h + 1) * 64], ya[:, h * 64:(h + 1) * 64], AF.Identity, [yab], y1rB + [st8r], accum=st8[:, 8 + h:9 + h])
                act(y1[:, h * 64:(h + 1) * 64], ya[:, h * 64:(h + 1) * 64], AF.Square, [yab], y1rB + [st8r], accum=st8[:, 16 + h:17 + h])
                if h % 2:
                    yield
            ts("dve", st8[:, 24:32], st8[:, 8:16], 1.0 / 64.0, None, ALU.mult, None, [st8r], [st8r])
            tt("dve", st8[:, 32:40], st8[:, 24:32], st8[:, 24:32], ALU.mult, [st8r], [st8r])
            stt("dve", st8[:, 48:56], st8[:, 16:24], 1.0 / 64.0, st8[:, 32:40], ALU.mult, ALU.subtract, [st8r], [st8r])
            yield
            act(st8[:, 48:56], st8[:, 48:56], AF.Ln, [st8r], [st8r], bias=GN_EPS)
            act(st8[:, 48:56], st8[:, 48:56], AF.Exp, [st8r], [st8r], scale=-0.5)
            yield
            tt("dve", y1[:, :].rearrange("p (h d) -> p h d", d=64), ya[:, :].rearrange("p (h d) -> p h d", d=64),
               bc(st8[:, 24:32].unsqueeze(2), [128, 8, 64]), ALU.subtract, [yab, st8r], y1rB)
            yield
            tt("pool", y1[:, :].rearrange("p (h d) -> p h d", d=64), y1[:, :].rearrange("p (h d) -> p h d", d=64),
               bc(st8[:, 48:56].unsqueeze(2), [128, 8, 64]), ALU.mult, y1rB + [st8r], y1rB)
            tt("pool", ybf_r[:, :], y1[:, :], srg[:, c, :], ALU.mult, y1rB + [srgb[c]], ybf_rB)
            yield
            yield from finish_y(1, c, ybf_r, ybf_rB)

        def gla_chunk(l, c, zero_state):
            cs = slice(c * 128, (c + 1) * 128)
            al, alb = bank()
            mm(al[:, 0:256], glrT[:, cs], WA2[:, l, :], True, False, [glrTb, PPb], [alb], inc=False)
            mm(al[:, 0:256], C("ones", 0, 128, 0, 1), BA[:, l * 256:(l + 1) * 256], False, True, [CPb, PPb], [alb])
            yield
            act(gE[:], al[:, 0:256], AF.Exp, [alb], [gEb], scale=-1.0)
            act(gL[:], gE[:], AF.Ln, [gEb], [gLb], bias=1.0)
            yield
            ck, ckb = bank()
            for j in range(2):
                mm(ck[:, j * 128:(j + 1) * 128], gL[:, j * 128:(j + 1) * 128], C("triI16"), True, True, [gLb, CPb], [ckb], inc=False)
            mm(ck[:, 256:512], C("triS16"), gL[:], True, True, [gLb, CPb], [ckb])
            yield
            act(epos[:, :, :].rearrange("p j t -> p (j t)"), ck[:, 0:256], AF.Exp, [ckb], [eposb], bias=math.log(0.125))
            act(eneg[:, :, :].rearrange("p j t -> p (j t)"), ck[:, 0:256], AF.Exp, [ckb], [enegb], scale=-1.0)
            act(gwst[:], ck[:, 256:512], AF.Exp, [ckb], [gwstb])
            tk_, tkb_ = bank()
            for j in range(2):
                mm(tk_[:, 2 * j:2 * j + 2], gL[:, j * 128:(j + 1) * 128], C("m16"), True, True, [gLb, CPb], [tkb_], inc=(j == 1))
            yield
            act(gdec[:], tk_[:, 0:4].rearrange("p (j two) -> p j two", two=2)[:, :, 0], AF.Exp, [tkb_], [gdecb])
            for hh in range(2):
                r0, r1 = hh * 64, hh * 64 + 64
                tt("pool", gqdm[hh][r0:r1, :, :], gqT[r0:r1, :, cs], epos[r0:r1, :, :], ALU.mult, [gqTb, eposb], [gqdmb[hh]])
            tt("pool", gki[:], gkT[:, :, cs], eneg[:], ALU.mult, [gkTb, enegb], [gkib])
            tt("dve", gks[:], gk_tok[:, c, :], gwst[:], ALU.mult, [gk_tokb[c], gwstb], [gksb])
            yield
            sk, skb = bank()
            for h in range(4):
                ps = slice((h % 2) * 64, (h % 2) * 64 + 64)
                mm(sk[:, h * 128:(h + 1) * 128], gki[:, h // 2, :], gqdm[h % 2][:, h // 2, :], True, True, [gkib, gqdmb[h % 2]], [skb], inc=(h == 3))
            yield
            tt("dve", Psb_g[:, :], sk[:, :], C("causal4"), ALU.mult, [skb, CPb], Psb_gB)
            yield
            ya, yab = bank()
            for h in range(4):
                ps = slice((h % 2) * 64, (h % 2) * 64 + 64)
                mm(ya[:, h * 128:(h + 1) * 128], Psb_g[:, h * 128:(h + 1) * 128], gv_tok[:, c, h * 128:(h + 1) * 128], True, zero_state,
                   Psb_gB + [gv_tokb[c]], [yab], inc=(zero_state and h == 3))
                if not zero_state:
                    mm(ya[:, h * 128:(h + 1) * 128], gqdm[h % 2][:, h // 2, :], Sgla16[l][:, (h // 2) * 128:(h // 2) * 128 + 128], False, True,
                       [gqdmb[h % 2], Sgla16b[l]], [yab], inc=(h == 3))
            yield
            su, sub_ = bank()
            for h in range(4):
                ps = slice((h % 2) * 64, (h % 2) * 64 + 64)
                mm(su[ps, (h // 2) * 128:(h // 2) * 128 + 128], gks[:, h * 64:(h + 1) * 64], gv_tok[:, c, h * 128:(h + 1) * 128], True, True,
                   [gksb, gv_tokb[c]], [sub_], inc=(h == 3))
            yield
            if zero_state:
                cpy("dve", Sgla[l][:], su[:, 0:256], [sub_], [Sglab[l]])
            else:
                for j in range(2):
                    stt("dve", Sgla[l][:, j * 128:(j + 1) * 128], Sgla[l][:, j * 128:(j + 1) * 128], gdec[:, j:j + 1],
                        su[:, j * 128:(j + 1) * 128], ALU.mult, ALU.add, [Sglab[l], gdecb, sub_], [Sglab[l]])
            cpy("act", Sgla16[l][:], Sgla[l][:], [Sglab[l]], [Sgla16b[l]])
            yield
            y1 = y1g
            for g in range(4):
                act(y1[:, g * 128:(g + 1) * 128], ya[:, g * 128:(g + 1) * 128], AF.Square, [yab], y1gB + [st8g], accum=st8[:, 40 + g:41 + g])
            yield
            act(st8[:, 44:48], st8[:, 40:44], AF.Ln, [st8g], [st8g], bias=GN_EPS, scale=1.0 / 128.0)
            act(st8[:, 44:48], st8[:, 44:48], AF.Exp, [st8g], [st8g], scale=-0.5)
            yield
            tt("dve", y1[:, :].rearrange("p (h d) -> p h d", d=128), ya[:, :].rearrange("p (h d) -> p h d", d=128),
               bc(st8[:, 44:48].unsqueeze(2), [128, 4, 128]), ALU.mult, [yab, st8g], y1gB)
            yield
            tt("pool", ybf_g[:, :], y1[:, :], sgr[:, c, :], ALU.mult, y1gB + [sgrb[c]], ybf_gB)
            yield
            yield from finish_y(2, c, ybf_g, ybf_gB)

        outb = Buf()
        for s in range(NSEQ):
            for t in range(NT):
                t0 = t * T
                for c in range(NCH):
                    S.dma(xin[:], x_d[s, t0 + c * 128:t0 + (c + 1) * 128, :], writes=[xinb])
                    for half in range(2):
                        bk, bb = bank()
                        for k4 in range(4):
                            kc = half * 4 + k4
                            trp(bk[:, k4 * 128:(k4 + 1) * 128], xin[:, kc * 128:(kc + 1) * 128], C("identf"), [xinb, CPb], [bb], inc=(k4 == 3))
                        cpy("act", X[:, half * 4:half * 4 + 4, c * 128:(c + 1) * 128], bk[:, :].rearrange("p (k t) -> p k t", k=4), [bb], [Xb])
                for l in range(L):
                    if stage >= 1:
                        mixer(l, t == 0, (t0 // 128))
                    if stage >= 2:
                        ffn(l, t == 0)
                bk, bb = bank()
                for kc in range(8):
                    s_, sB = sq[kc % 3], sqb[kc % 3]
                    act(s_[:], X[:, kc, :], AF.Square, [Xb], [sB])
                    mm(bk[:, 0:T], CBc("cmean"), s_[:], kc == 0, kc == 7, [sB, CBb], [bb], inc=True)
                act(rstd[:], bk[:, 0:T], AF.Ln, [bb], [rstdb], bias=RMS_EPS)
                act(rstd[:], rstd[:], AF.Exp, [rstdb], [rstdb], scale=-0.5)
                for kc in range(8):
                    stt("dve", X[:, kc, :], X[:, kc, :], PP[:, PP_L * L + kc:PP_L * L + kc + 1], rstd[:], ALU.mult, ALU.mult,
                        [Xb, rstdb, PPb], [Xb])
                for c in range(NCH):
                    for half in range(2):
                        bk, bb = bank()
                        for k4 in range(4):
                            kc = half * 4 + k4
                            trp(bk[:, k4 * 128:(k4 + 1) * 128], X[:, kc, c * 128:(c + 1) * 128], C("identf"), [Xb, CPb], [bb], inc=(k4 == 3))
                        cpy("act", xout[:, half * 512:(half + 1) * 512], bk[:, :], [bb], [xoutb])
                    S.dma(out_d[s, t0 + c * 128:t0 + (c + 1) * 128, :], xout[:], reads=[xoutb], writes=[outb])
        if debug and dbg_list:
            o = 0
            for ap_, b, width in dbg_list:
                S.dma(dbg_d[:, o:o + width], ap_[:, 0:width], reads=[b], writes=[outb])
                o += width
        S.finish()
        with nc.Block() as block:
            S.replay(block)
    print('n_inst', S.n_inst, {k: len(v.stream) for k, v in S.E.items()})
    return nc, cp_np, cbp_np


def _run(inputs, L, NSEQ, SEQLEN, ncores, debug=None, stage=2):
    nc, cp_np, cbp_np = build_nc(L, NSEQ, SEQLEN, debug=debug, stage=stage)
    p = {k: np.asarray(v) for k, v in inputs.items()}
    pp = _param_pack(L, p)
    f = lambda a: np.ascontiguousarray(np.asarray(a, np.float32))
    shared = {
        "w_in": f(p["w_in"]), "w_branch": f(p["w_branch"]), "w_out": f(p["w_out"]), "w_up": f(p["w_up"]),
        "w_down": f(p["w_down"]), "gla_w_alpha2": f(p["gla_w_alpha2"]), "gla_b_alpha": f(p["gla_b_alpha"]),
        "pp": pp, "cp": cp_np, "cbp": cbp_np,
    }
    x = f(p["x"])
    in_maps = []
    for i in range(ncores):
        m = dict(shared)
        m["x"] = np.ascontiguousarray(x[i * NSEQ:(i + 1) * NSEQ])
        in_maps.append(m)
    res = run_bass_kernel_spmd(nc, in_maps, core_ids=list(range(ncores)))
    out = np.concatenate([r["out"] for r in res.results], axis=0)
    if debug:
        return out, [r["dbg"] for r in res.results]
    return out


def kernel(**inputs):
    x = inputs["x"]
    B, SEQLEN, _ = x.shape
    L = inputs["w_in"].shape[0]
    ncores = 8
    return _run(inputs, L, B // ncores, SEQLEN, ncores).astype(np.float32)
```

```python
import math
from contextlib import ExitStack

import numpy as np
import ml_dtypes
import concourse.bass as bass
import concourse.mybir as mybir
from concourse.bass_utils import run_bass_kernel_spmd

F32 = mybir.dt.float32
BF16 = mybir.dt.bfloat16
AF = mybir.ActivationFunctionType
ALU = mybir.AluOpType
AX = mybir.AxisListType

D = 1024
NIN = 7960
DFF = 2816
NBLK = 40
RMS_EPS = 1e-6
GN_EPS = 1e-5


class Buf:
    __slots__ = ("last_w", "reads")

    def __init__(self):
        self.last_w = None
        self.reads = {}


class Eng:
    def __init__(self, name, sem, inc):
        self.name = name
        self.sem = sem
        self.inc = inc
        self.count = 0
        self.seen = {}
        self.stream = []
        self.pending = False
        self.needed = set()


class Sched:
    NLANES = 12

    def __init__(self, nc, sems):
        self.nc = nc
        self.E = {}
        for i, n in enumerate(["pe", "act", "dve", "pool"]):
            self.E[n] = Eng(n, sems[i], 1)
        self.sp = Eng("sp", None, 0)
        self.lanes = [Eng("lane%d" % i, sems[4 + i], 16) for i in range(self.NLANES)]
        self.lane_rr = 0
        self.n_inst = 0

    def _deps(self, reads, writes):
        deps = {}
        for b in reads:
            if b.last_w is not None:
                e, c = b.last_w
                if deps.get(e, 0) < c:
                    deps[e] = c
        for b in writes:
            if b.last_w is not None:
                e, c = b.last_w
                if deps.get(e, 0) < c:
                    deps[e] = c
            for e, c in b.reads.items():
                if deps.get(e, 0) < c:
                    deps[e] = c
        return deps

    def _emit_waits(self, E, deps, skip_self=False):
        for e, c in deps.items():
            if e is E and skip_self:
                continue
            if E.seen.get(e, 0) >= c:
                continue
            E.seen[e] = c
            e.needed.add(c)
            E.stream.append(("wait", e, c))

    def op(self, eng, fn, reads=(), writes=(), inc=True):
        E = self.E[eng]
        deps = self._deps(reads, writes)
        self._emit_waits(E, deps, skip_self=(eng == "pe"))
        stamp = E.count + E.inc
        if inc:
            E.count = stamp
            E.pending = False
        else:
            E.pending = True
        E.stream.append(("inst", fn, inc, stamp))
        for b in reads:
            b.reads[E] = stamp
        for b in writes:
            b.last_w = (E, stamp)
            b.reads = {}
        self.n_inst += 1

    def dma(self, out, in_, reads=(), writes=()):
        L = self.lanes[self.lane_rr]
        self.lane_rr = (self.lane_rr + 1) % self.NLANES
        deps = self._deps(reads, writes)
        if L.count > 0:
            deps[L] = max(deps.get(L, 0), L.count)
        self._emit_waits(self.sp, deps)
        L.count += 16
        stamp = L.count
        self.sp.stream.append(("dma", out, in_, L.sem))
        for b in reads:
            b.reads[L] = stamp
        for b in writes:
            b.last_w = (L, stamp)
            b.reads = {}
        self.n_inst += 1

    def barrier(self):
        allE = list(self.E.values()) + self.lanes
        for E in list(self.E.values()) + [self.sp]:
            deps = {e: e.count for e in allE if e.count > 0 and e is not E}
            self._emit_waits(E, deps)

    def finish(self):
        deps = {e: e.count for e in list(self.E.values()) + self.lanes if e.count > 0}
        self._emit_waits(self.sp, deps)

    def replay(self, block):
        rank = {}
        for e in list(self.E.values()):
            if e.name == "pool":
                continue
            rank[e] = {c: i + 1 for i, c in enumerate(sorted(e.needed))}
        n_skipped = [0]

        def run(E, eng):
            for item in E.stream:
                if item[0] == "wait":
                    e, c = item[1], item[2]
                    eng.wait_ge(e.sem, rank[e][c] if e in rank else c)
                elif item[0] == "inst":
                    ins = item[1](eng)
                    if item[2]:
                        if E in rank and item[3] not in rank[E]:
                            n_skipped[0] += 1
                        else:
                            ins.then_inc(E.sem, 1)
                else:
                    eng.dma_start(out=item[1], in_=item[2]).then_inc(item[3], 16)

        for E in self.E.values():
            assert not E.pending, E.name

        @block.tensor
        def _(e):
            run(self.E["pe"], e)

        @block.scalar
        def _(e):
            run(self.E["act"], e)

        @block.vector
        def _(e):
            run(self.E["dve"], e)

        @block.gpsimd
        def _(e):
            run(self.E["pool"], e)

        @block.sync
        def _(e):
            run(self.sp, e)


def _const_pack(seqlen):
    nchs = seqlen // 128
    s = np.arange(128)[:, None].astype(np.float64)
    t = np.arange(128)[None, :].astype(np.float64)
    le = (s <= t)
    cols = {}
    cols["triI"] = le.astype(np.float64)
    cols["triS"] = (s > t).astype(np.float64)
    cols["ones"] = np.ones((128, 128))
    cols["triI16"] = -le.astype(np.float64) / 16.0
    cols["triS16"] = -(s > t).astype(np.float64) / 16.0
    cols["m16"] = -np.ones((128, 2)) / 16.0
    cols["causal4"] = np.tile(le.astype(np.float64), (1, 4))
    lg = np.log(1.0 - np.exp2(-5.0 - np.arange(8, dtype=np.float64)))
    dret = np.zeros((128, 8, 128))
    for h in range(8):
        dret[:, h, :] = np.where(le, np.exp(lg[h] * (t - s)), 0.0) * 0.125
    cols["dret"] = dret.reshape(128, 1024)
    qd = np.zeros((128, 4, 128))
    decs = np.zeros((128, 4))
    for j in range(4):
        for hh in range(2):
            h = 2 * j + hh
            qd[hh * 64:(hh + 1) * 64, j, :] = np.exp(lg[h] * (np.arange(128) + 1.0))[None, :] * 0.125
            decs[hh * 64:(hh + 1) * 64, j] = np.exp(lg[h] * 128.0)
    cols["qdec"] = qd.reshape(128, 512)
    cols["decs"] = decs
    wt = np.zeros((128, 8))
    for h in range(8):
        wt[:, h] = np.exp(lg[h] * (127.0 - np.arange(128)))
    cols["wtab"] = wt
    inv = 10000.0 ** (-np.arange(32, dtype=np.float32) / 32.0)
    pos = np.arange(seqlen, dtype=np.float32)
    ang = (pos[:, None] * inv[None, :]).astype(np.float32)
    cos = np.cos(ang).astype(np.float64)
    sin = np.sin(ang).astype(np.float64)
    cos2 = np.concatenate([cos, cos], axis=1).reshape(nchs, 128, 64).transpose(1, 0, 2)
    sin2 = np.concatenate([-sin, sin], axis=1).reshape(nchs, 128, 64).transpose(1, 0, 2)
    cols["cos2"] = cos2.reshape(128, nchs * 64)
    cols["sin2"] = sin2.reshape(128, nchs * 64)
    sel = np.zeros((128, 2, 4, 128))
    for g in range(2):
        for hl in range(4):
            sel[4 * g + hl, g, hl, :] = 1.0
    cols["sel"] = sel.reshape(128, 1024)
    cols["identf"] = np.eye(128)
    off = {}
    o = 0
    arrs = []
    for k, v in cols.items():
        off[k] = (o, v.shape[1])
        o += v.shape[1]
        arrs.append(v)
    cp = np.ascontiguousarray(np.concatenate(arrs, axis=1).astype(np.float32))
    cb = {}
    cb["identb"] = np.eye(128)
    cb["negmask4"] = np.tile(np.where(le, 0.0, -30000.0), (1, 4))
    cb["cmean"] = np.full((128, 128), 1.0 / 1024.0)
    offb = {}
    o = 0
    arrs = []
    for k, v in cb.items():
        offb[k] = (o, v.shape[1])
        o += v.shape[1]
        arrs.append(v)
    cbp = np.ascontiguousarray(np.concatenate(arrs, axis=1).astype(ml_dtypes.bfloat16))
    return cp, off, cbp, offb


PP_FIELDS = [("gmix", 8), ("gffn", 8), ("gbr", 12), ("cw", 24), ("cb", 6), ("bg", 24),
             ("fw", 132), ("fb", 44), ("dtb", 8), ("alog", 8), ("dsk", 8)]
PP_L = sum(n for _, n in PP_FIELDS)


def _pp_off(L):
    off = {}
    o = 0
    for k, n in PP_FIELDS:
        off[k] = o
        o += n
    return off, PP_L * L + 8


def _param_pack(L, p):
    off, tot = _pp_off(L)
    pp = np.zeros((128, tot), np.float32)

    def fm(v, nk):
        return np.asarray(v, np.float32).reshape(nk, 128).T

    for l in range(L):
        b = l * PP_L
        pp[:, b + off["gmix"]: b + off["gmix"] + 8] = fm(p["norm_mix_g"][l], 8)
        pp[:, b + off["gffn"]: b + off["gffn"] + 8] = fm(p["norm_ffn_g"][l], 8)
        for i, nm in enumerate(["ssm_norm_g", "ret_norm_g", "gla_norm_g"]):
            pp[:, b + off["gbr"] + 4 * i: b + off["gbr"] + 4 * i + 4] = fm(p[nm][l], 4)
        for j in range(4):
            pp[:, b + off["cw"] + 6 * j: b + off["cw"] + 6 * j + 6] = fm(p["ssm_conv_w"][l, j], 6)
        pp[:, b + off["cb"]: b + off["cb"] + 6] = fm(p["ssm_conv_b"][l], 6)
        for i in range(3):
            pp[:, b + off["bg"] + 8 * i: b + off["bg"] + 8 * i + 8] = fm(p["b_gate"][l, i], 8)
        for j in range(3):
            pp[:, b + off["fw"] + 44 * j: b + off["fw"] + 44 * j + 44] = fm(p["ffn_conv_w"][l, j], 44)
        pp[:, b + off["fb"]: b + off["fb"] + 44] = fm(p["ffn_conv_b"][l], 44)
        pp[:, b + off["dtb"]: b + off["dtb"] + 8] = np.broadcast_to(p["ssm_dt_bias"][l], (128, 8))
        pp[:, b + off["alog"]: b + off["alog"] + 8] = np.broadcast_to(p["ssm_a_log"][l], (128, 8))
        pp[:, b + off["dsk"]: b + off["dsk"] + 8] = np.broadcast_to(p["ssm_d"][l], (128, 8))
    pp[:, PP_L * L: PP_L * L + 8] = fm(p["norm_f_g"], 8)
    return pp


C_Z, C_XBC, C_DT, C_RQ, C_RK, C_RV, C_RG = 0, 512, 1280, 1288, 1800, 2312, 2824
C_GQ, C_GK, C_GV, C_GR, C_GLR, C_GATE = 3336, 3592, 3848, 4360, 4872, 4888


def _block_defs():
    blks = []
    blks.append(("w_in", [(C_Z, 512)], 8, "gmix"))
    blks.append(("w_in", [(C_XBC, 512)], 8, "gmix"))
    blks.append(("w_in", [(C_XBC + 512, 256), (C_DT, 8), (C_GLR, 16)], 8, "gmix"))
    blks.append(("w_in", [(C_RQ, 512)], 8, "gmix"))
    blks.append(("w_in", [(C_RK, 512)], 8, "gmix"))
    blks.append(("w_in", [(C_RV, 512)], 8, "gmix"))
    blks.append(("w_in", [(C_RG, 512)], 8, "gmix"))
    blks.append(("w_in", [(C_GQ, 512)], 8, "gmix"))
    blks.append(("w_in", [(C_GV, 512)], 8, "gmix"))
    blks.append(("w_in", [(C_GR, 512)], 8, "gmix"))
    for i in range(3):
        blks.append(("w_in", [(C_GATE + i * 1024, 512)], 8, "gmix"))
        blks.append(("w_in", [(C_GATE + i * 1024 + 512, 512)], 8, "gmix"))
        blks.append(("w_branch%d" % i, [(0, 1024)], 4, "gbr%d" % i))
    blks.append(("w_out", [(0, 512)], 8, "half"))
    blks.append(("w_out", [(512, 512)], 8, "half"))
    for b in range(11):
        blks.append(("w_up", [(2 * b * 128, 256), (DFF + 2 * b * 128, 256)], 8, "gffn"))
    for oc in range(8):
        blks.append(("w_down", [(oc * 128, 128)], 22, None))
    assert len(blks) == NBLK
    return blks


def build_nc(L, NSEQ, SEQLEN, T=256, debug=None, stage=2):
    NCH = T // 128
    NT = SEQLEN // T
    nc = bass.Bass("TRN2", target_bir_lowering=False)
    cp_np, coff, cbp_np, cboff = _const_pack(SEQLEN)
    ppoff, pptot = _pp_off(L)

    def dram(name, shape, dt=F32, kind="ExternalInput"):
        return nc.dram_tensor(name, list(shape), dt, kind=kind).ap()

    x_d = dram("x", [NSEQ, SEQLEN, D])
    w_in_d = dram("w_in", [L, D, NIN])
    w_br_d = dram("w_branch", [L, 3, 512, D])
    w_out_d = dram("w_out", [L, D, D])
    w_up_d = dram("w_up", [L, D, 2 * DFF])
    w_dn_d = dram("w_down", [L, DFF, D])
    wa2_d = dram("gla_w_alpha2", [L, 16, 256])
    ba_d = dram("gla_b_alpha", [L, 256])
    pp_d = dram("pp", [128, pptot])
    cp_d = dram("cp", list(cp_np.shape))
    cbp_d = dram("cbp", list(cbp_np.shape), BF16)
    out_d = dram("out", [NSEQ, SEQLEN, D], kind="ExternalOutput")
    ws_d = dram("wscratch", [L, NBLK, 128, 4096], BF16, kind="Internal")
    dbg_d = None
    if debug:
        dbg_d = dram("dbg", [128, debug[1]], kind="ExternalOutput")

    blkdefs = _block_defs()

    with ExitStack() as es:
        sems = [es.enter_context(nc.semaphore("s%d" % i)) for i in range(4 + Sched.NLANES)]
        S = Sched(nc, sems)
        _cnt = [0]

        def sb(shape, dt=F32):
            _cnt[0] += 1
            return es.enter_context(nc.sbuf_tensor("t%d" % _cnt[0], list(shape), dt))

        def act(out, in_, func, r, w, bias=0.0, scale=1.0, accum=None):
            if accum is None:
                S.op("act", lambda e: e.activation(out=out, in_=in_, func=func, bias=bias, scale=scale), r, w)
            else:
                S.op("act", lambda e: e.activation(out=out, in_=in_, func=func, bias=bias, scale=scale,
                                                   accum_out=accum), r, w)

        def tt(eng, out, a, b, op, r, w):
            S.op(eng, lambda e: e.tensor_tensor(out=out, in0=a, in1=b, op=op), r, w)

        def ts(eng, out, a, s1, s2, op0, op1, r, w):
            if s2 is None:
                S.op(eng, lambda e: e.tensor_scalar(out=out, in0=a, scalar1=s1, scalar2=None, op0=op0), r, w)
            else:
                S.op(eng, lambda e: e.tensor_scalar(out=out, in0=a, scalar1=s1, scalar2=s2, op0=op0, op1=op1), r, w)

        def stt(eng, out, a, s, b, op0, op1, r, w):
            S.op("dve", lambda e: e.scalar_tensor_tensor(out=out, in0=a, scalar=s, in1=b, op0=op0, op1=op1), r, w)

        def cpy(eng, out, in_, r, w):
            if eng == "act":
                S.op("act", lambda e: e.copy(out=out, in_=in_), r, w)
            else:
                S.op(eng, lambda e: e.tensor_copy(out=out, in_=in_), r, w)

        def mm(out, lhsT, rhs, start, stop, r, w, inc=None):
            if inc is None:
                inc = stop
            S.op("pe", lambda e: e.matmul(out, lhsT=lhsT, rhs=rhs, start=start, stop=stop), r, w, inc=inc)

        def trp(out, in_, ident, r, w, inc=True):
            S.op("pe", lambda e: e.transpose(out, in_, ident), r, w, inc=inc)

        def bc(ap, shape):
            return ap.to_broadcast(list(shape))

        CP = sb(cp_np.shape)
        CPb = Buf()
        CB = sb(cbp_np.shape, BF16)
        CBb = Buf()
        PP = sb([128, pptot])
        PPb = Buf()
        WA2 = sb([16, L, 256])
        BA = sb([1, L * 256])
        S.dma(CP[:], cp_d[:, :], writes=[CPb])
        S.dma(CB[:], cbp_d[:, :], writes=[CBb])
        S.dma(PP[:], pp_d[:, :], writes=[PPb])
        S.dma(WA2[:], wa2_d.rearrange("l r c -> r l c"), writes=[PPb])
        S.dma(BA[:], ba_d.rearrange("l c -> (l c)").unsqueeze(0), writes=[PPb])

        def C(name, lo=0, hi=None, p0=0, p1=128):
            o, n = coff[name]
            if hi is None:
                hi = n
            return CP[p0:p1, o + lo:o + hi]

        def CBc(name, lo=0, hi=None):
            o, n = cboff[name]
            if hi is None:
                hi = n
            return CB[:, o + lo:o + hi]

        def P(l, name, lo, hi):
            o = l * PP_L + ppoff[name]
            return PP[:, o + lo:o + hi]

        AN = sb([128, L, 8])
        BGH = sb([128, L, 24])
        for l in range(L):
            act(AN[:, l, :], P(l, "alog", 0, 8), AF.Exp, [PPb], [PPb])
            ts("dve", AN[:, l, :], AN[:, l, :], -1.0, None, ALU.mult, None, [PPb], [PPb])
            ts("dve", BGH[:, l, :], P(l, "bg", 0, 24), 0.5, None, ALU.mult, None, [PPb], [PPb])

        banks = [es.enter_context(nc.psum_tensor("pb%d" % i, [128, 512], F32)) for i in range(8)]
        bankb = [Buf() for _ in range(8)]
        _bk = [0]

        _bctx = [None]

        def bank():
            ctx = _bctx[0]
            if ctx is not None:
                i = ctx["ids"][ctx["k"] % len(ctx["ids"])]
                ctx["k"] += 1
                return banks[i], bankb[i]
            i = _bk[0]
            _bk[0] = (i + 1) % 8
            return banks[i], bankb[i]

        skAb = Buf()

        with ExitStack() as es2:
            stg = [es2.enter_context(nc.sbuf_tensor("stg%d" % i, [128, 4096], F32)) for i in range(2)]
            stgb = [Buf() for _ in range(2)]
            cvt = [es2.enter_context(nc.sbuf_tensor("cvt%d" % i, [128, 4096], BF16)) for i in range(2)]
            cvtb = [Buf() for _ in range(2)]
            wsb = Buf()
            k = 0
            engs = ["dve", "pool", "act"]
            for l in range(L):
                for bi, (mat, segs, KC, sk) in enumerate(blkdefs):
                    s_, c_ = stg[k % 2], cvt[k % 2]
                    sB, cB = stgb[k % 2], cvtb[k % 2]
                    ncols = sum(n for _, n in segs)
                    if mat == "w_in":
                        src = w_in_d[l]
                    elif mat.startswith("w_branch"):
                        src = w_br_d[l, int(mat[-1])]
                    elif mat == "w_out":
                        src = w_out_d[l]
                    elif mat == "w_up":
                        src = w_up_d[l]
                    else:
                        src = w_dn_d[l]
                    srcv = src.rearrange("(kc p) n -> p kc n", p=128)
                    sv = s_[:, 0:KC * ncols].rearrange("p (kc n) -> p kc n", kc=KC)
                    cv = c_[:, 0:KC * ncols].rearrange("p (kc n) -> p kc n", kc=KC)
                    o = 0
                    for (c0, n) in segs:
                        S.dma(sv[:, :, o:o + n], srcv[:, :, c0:c0 + n], writes=[sB])
                        o += n
                    if sk is None or sk == "half":
                        e = engs[k % 2]
                        if sk is None:
                            cpy(e, c_[:, 0:KC * ncols], s_[:, 0:KC * ncols], [sB], [cB])
                        else:
                            ts(e, c_[:, 0:KC * ncols], s_[:, 0:KC * ncols], 0.5, None, ALU.mult, None, [sB], [cB])
                    else:
                        for kc in range(KC):
                            if sk == "gmix":
                                g = P(l, "gmix", kc, kc + 1)
                            elif sk == "gffn":
                                g = P(l, "gffn", kc, kc + 1)
                            else:
                                i = int(sk[-1])
                                g = P(l, "gbr", 4 * i + kc, 4 * i + kc + 1)
                            e = engs[kc % 3]
                            if e == "act":
                                act(cv[:, kc, :], sv[:, kc, :], AF.Copy, [sB, PPb], [cB], scale=g)
                            else:
                                ts(e, cv[:, kc, :], sv[:, kc, :], g, None, ALU.mult, None, [sB, PPb], [cB])
                    S.dma(ws_d[l, bi, :, 0:KC * ncols], c_[:, 0:KC * ncols], reads=[cB], writes=[wsb])
                    k += 1
            S.barrier()

        NSLOT = 4
        ring = [sb([128, 4096], BF16) for _ in range(NSLOT)]
        ringb = [Buf() for _ in range(NSLOT)]

        units = [(s, t, l) for s in range(NSEQ) for t in range(NT) for l in range(L)]
        stream = [(l, bi) for (_, _, l) in units for bi in range([0, 21, NBLK][stage])]
        _ws = {"next_load": 0, "next_use": 0}

        def _issue_load():
            i = _ws["next_load"]
            if i >= len(stream):
                return
            l, bi = stream[i]
            _, segs, KC, _ = blkdefs[bi]
            n = KC * sum(nn for _, nn in segs)
            S.dma(ring[i % NSLOT][:, 0:n], ws_d[l, bi, :, 0:n], reads=[wsb], writes=[ringb[i % NSLOT]])
            _ws["next_load"] = i + 1

        def wnext(l, bi):
            i = _ws["next_use"]
            assert stream[i] == (l, bi), (stream[i], l, bi)
            while _ws["next_load"] < min(len(stream), i + NSLOT - 2):
                _issue_load()
            _ws["next_use"] = i + 1
            _, segs, KC, _ = blkdefs[bi]
            ncols = sum(nn for _, nn in segs)
            v = ring[i % NSLOT][:, 0:KC * ncols].rearrange("p (kc n) -> p kc n", kc=KC)
            return v, ringb[i % NSLOT]

        X = sb([128, 8, T])
        Xb = Buf()
        hT = sb([128, 8, T], BF16)
        hTb = Buf()
        sq = [sb([128, T], BF16) for _ in range(3)]
        sqb = [Buf() for _ in range(3)]
        rstd = sb([128, T])
        rstdb = Buf()
        xin = sb([128, D])
        xinb = Buf()
        xout = xin
        xoutb = xinb

        sz = sb([128, NCH, 512], BF16); szb = [Buf() for _ in range(NCH)]
        xbuf = [sb([128, 3 + T]) for _ in range(2)]; xbufb = [Buf() for _ in range(2)]
        cacc = [sb([128, T]) for _ in range(2)]; caccb = [Buf() for _ in range(2)]
        xact = sb([128, 6, T], BF16); xactb = [Buf() for _ in range(6)]
        dtraw = sb([128, NCH, 8]); dtrawb = Buf()
        glrT = sb([16, T]); glrTb = Buf()
        q_tok = sb([128, NCH, 512], BF16); q_tokb = [Buf() for _ in range(NCH)]
        k_tok = sb([128, NCH, 512], BF16); k_tokb = [Buf() for _ in range(NCH)]
        v_tok = sb([128, NCH, 512], BF16); v_tokb = [Buf() for _ in range(NCH)]
        srg = sb([128, NCH, 512], BF16); srgb = [Buf() for _ in range(NCH)]
        ropeA = [sb([128, 512])] * 2; ropeAb = [Buf()] * 2
        ropeB = [sb([128, 512])] * 2; ropeBb = [Buf()] * 2
        gqT = sb([128, 2, T], BF16); gqTb = Buf()
        gkT = sb([128, 2, T], BF16); gkTb = Buf()
        gk_tok = sb([128, NCH, 256], BF16); gk_tokb = [Buf() for _ in range(NCH)]
        gv_tok = sb([128, NCH, 512], BF16); gv_tokb = [Buf() for _ in range(NCH)]
        sgr = sb([128, NCH, 512], BF16); sgrb = [Buf() for _ in range(NCH)]
        yT = [sb([128, 4, T], BF16) for _ in range(3)]; yTb = [Buf() for _ in range(3)]
        mrg = sb([128, 8, T]); mrgb = [Buf() for _ in range(8)]
        mT = sb([128, 8, T], BF16); mTb = Buf()
        gth = [sb([128, T])] * 2; gthb = [Buf()] * 2
        gtmp = [sb([128, T])] * 2; gtmpb = [Buf()] * 2
        fa = sb([128, 22, T], BF16); fab = [Buf() for _ in range(22)]
        ub = [sb([128, 2 + T]) for _ in range(2)] * 2; ubb = [Buf() for _ in range(2)] * 2
        facc = [sb([128, T]) for _ in range(4)]; faccb = [Buf() for _ in range(4)]
        fsg = [sb([128, T]) for _ in range(2)]; fsgb = [Buf() for _ in range(2)]
        Sssd = [sb([128, 256]) for _ in range(L)]; Sssdb = [Buf() for _ in range(L)]
        Sssd16 = [sb([128, 256], BF16) for _ in range(L)]; Sssd16b = [Buf() for _ in range(L)]
        Sret = [sb([128, 256]) for _ in range(L)]; Sretb = [Buf() for _ in range(L)]
        Sret16 = [sb([128, 256], BF16) for _ in range(L)]; Sret16b = [Buf() for _ in range(L)]
        Sgla = [sb([128, 256]) for _ in range(L)]; Sglab = [Buf() for _ in range(L)]
        Sgla16 = [sb([128, 256], BF16) for _ in range(L)]; Sgla16b = [Buf() for _ in range(L)]
        halo_s = [sb([128, 6, 3]) for _ in range(L)]; halo_sb = [Buf() for _ in range(L)]
        halo_f = [sb([128, 44, 2]) for _ in range(L)]; halo_fb = [Buf() for _ in range(L)]
        dtv = sb([128, 8]); la = sb([128, 8]); smallb = Buf()
        ex3 = sb([128, 24]); ex3b = Buf()
        decsel = sb([128, 4]); decselb = Buf()
        ncr = sb([8, 128]); ncrb = Buf()
        Bm = sb([128, 8, 128]); Bmb = Buf()
        Esb = [sb([128, 512], BF16) for _ in range(2)]; Esbb = [Buf() for _ in range(2)]
        Psb = [sb([128, 512], BF16) for _ in range(2)]; Psbb = [Buf() for _ in range(2)]
        xs_tok = sb([128, 512], BF16); xs_tokb = Buf()
        vv = sb([128, 512], BF16); vvb = Buf()
        vw = sb([128, 512], BF16); vwb = Buf()
        B_tok = sb([128, 128], BF16); B_tokb = Buf()
        ytmp = [sb([128, 512]) for _ in range(2)]; ytmpb = [Buf() for _ in range(2)]
        ybf = sb([128, 512], BF16); ybfb = Buf()
        st8 = sb([128, 64]); st8b = Buf()
        kTs = sb([128, 4, 128], BF16); kTsb = Buf()
        kw = sb([128, 512], BF16); kwb = Buf()
        gL = sb([128, 256]); gLb = Buf()
        gE = sb([128, 256]); gEb = Buf()
        epos = sb([128, 2, 128]); eposb = Buf()
        eneg = sb([128, 2, 128]); enegb = Buf()
        gki = sb([128, 2, 128], BF16); gkib = Buf()
        gwst = sb([128, 256]); gwstb = Buf()
        gks = sb([128, 256], BF16); gksb = Buf()
        gdec = sb([128, 2]); gdecb = Buf()
        Cm = [sb([128, 128], BF16) for _ in range(2)]; Cmb = [Buf() for _ in range(2)]
        qm = [sb([128, 4, 128], BF16) for _ in range(2)]; qmb = [Buf() for _ in range(2)]
        qdm = [sb([128, 4, 128], BF16) for _ in range(2)]; qdmb = [Buf() for _ in range(2)]
        gqdm = [sb([128, 2, 128], BF16) for _ in range(2)]; gqdmb = [Buf() for _ in range(2)]
        for i_ in range(2):
            S.op("pool", lambda e, i_=i_: e.memset(Cm[i_][:], 0.0), [], [Cmb[i_]])
            S.op("pool", lambda e, i_=i_: e.memset(qm[i_][:], 0.0), [], [qmb[i_]])
            S.op("pool", lambda e, i_=i_: e.memset(qdm[i_][:], 0.0), [], [qdmb[i_]])
            S.op("pool", lambda e, i_=i_: e.memset(gqdm[i_][:], 0.0), [], [gqdmb[i_]])

        assert T == 256
        faflat = fa[:, :, :].rearrange("p j t -> p (j t)")
        y1r = faflat[:, 0:4 * T].bitcast(F32); y1rB = fab[0:4]
        ybf_r = faflat[:, 4 * T:6 * T]; ybf_rB = fab[4:6]
        Psb_r = [faflat[:, 6 * T:8 * T], faflat[:, 8 * T:10 * T]]; Psb_rB = [fab[6:8], fab[8:10]]
        y1g = faflat[:, 10 * T:14 * T].bitcast(F32); y1gB = fab[10:14]
        ybf_g = faflat[:, 14 * T:16 * T]; ybf_gB = fab[14:16]
        Psb_g = faflat[:, 16 * T:18 * T]; Psb_gB = fab[16:18]
        st8r = Buf(); st8g = Buf()

        dbg_list = []

        def dump(name, ap_, b, width):
            if debug and debug[0] == name:
                dbg_list.append((ap_, b, width))

        def rmsnorm():
            bk, bb = bank()
            for kc in range(8):
                s_, sB = sq[kc % 3], sqb[kc % 3]
                act(s_[:], X[:, kc, :], AF.Square, [Xb], [sB])
                mm(bk[:, 0:T], CBc("cmean"), s_[:], kc == 0, kc == 7, [sB, CBb], [bb], inc=True)
            act(rstd[:], bk[:, 0:T], AF.Ln, [bb], [rstdb], bias=RMS_EPS)
            act(rstd[:], rstd[:], AF.Exp, [rstdb], [rstdb], scale=-0.5)
            for kc in range(8):
                tt("pool" if kc % 2 else "dve", hT[:, kc, :], X[:, kc, :], rstd[:], ALU.mult, [Xb, rstdb], [hTb])

        def proj_feat(W, Wb, c0, n):
            bk, bb = bank()
            for kc in range(8):
                mm(bk[0:n, 0:T], W[:, kc, c0:c0 + n], hT[:, kc, :], kc == 0, kc == 7, [Wb, hTb], [bb])
            return bk, bb

        def proj_tok(W, Wb, c0, n, c):
            bk, bb = bank()
            for kc in range(8):
                mm(bk[:, 0:n], hT[:, kc, c * 128:(c + 1) * 128], W[:, kc, c0:c0 + n], kc == 0, kc == 7, [Wb, hTb], [bb])
            return bk, bb

        import os as _os
        SUB = int(_os.environ.get("SUB", "99"))
        SSUB = int(_os.environ.get("SSUB", "99"))
        RSUB = int(_os.environ.get("RSUB", "99"))

        def _drain(l, frm):
            for bi in range(frm, 21):
                wnext(l, bi)

        def mixer(l, first, pc0):
            rmsnorm()
            W, Wb = wnext(l, 0)
            for c in range(NCH):
                bk, bb = proj_tok(W, Wb, 0, 512, c)
                act(sz[:, c, :], bk[:, :], AF.Silu, [bb], [szb[c]])
            if SUB <= 0:
                return _drain(l, 1)
            W1, W1b = wnext(l, 1)
            W2, W2b = wnext(l, 2)
            for j in range(6):
                if j < 4:
                    bk, bb = proj_feat(W1, W1b, j * 128, 128)
                else:
                    bk, bb = proj_feat(W2, W2b, (j - 4) * 128, 128)
                xb_, xbB = xbuf[j % 2], xbufb[j % 2]
                ac_, acB = cacc[j % 2], caccb[j % 2]
                if first:
                    S.op("pool", lambda e, xb_=xb_: e.memset(xb_[:, 0:3], 0.0), [], [xbB])
                else:
                    cpy("pool", xb_[:, 0:3], halo_s[l][:, j, :], [halo_sb[l]], [xbB])
                act(xb_[:, 3:3 + T], bk[:, 0:T], AF.Copy, [bb], [xbB])
                act(ac_[:], bk[:, 0:T], AF.Identity, [bb, PPb], [acB],
                    bias=P(l, "cb", j, j + 1), scale=P(l, "cw", 18 + j, 19 + j))
                stt("dve", ac_[:], xb_[:, 2:2 + T], P(l, "cw", 12 + j, 13 + j), ac_[:], ALU.mult, ALU.add, [xbB, acB, PPb], [acB])
                stt("pool", ac_[:], xb_[:, 1:1 + T], P(l, "cw", 6 + j, 7 + j), ac_[:], ALU.mult, ALU.add, [xbB, acB, PPb], [acB])
                stt("dve", ac_[:], xb_[:, 0:T], P(l, "cw", j, j + 1), ac_[:], ALU.mult, ALU.add, [xbB, acB, PPb], [acB])
                cpy("pool", halo_s[l][:, j, :], xb_[:, T:T + 3], [xbB], [halo_sb[l]])
                if j > 0:
                    act(xact[:, j - 1, :], cacc[(j - 1) % 2][:], AF.Silu, [caccb[(j - 1) % 2]], [xactb[j - 1]])
            act(xact[:, 5, :], cacc[1][:], AF.Silu, [caccb[1]], [xactb[5]])
            for c in range(NCH):
                bk, bb = proj_tok(W2, W2b, 256, 8, c)
                tt("dve", dtraw[:, c, :], bk[:, 0:8], P(l, "dtb", 0, 8), ALU.add, [bb, PPb], [dtrawb])
            bk, bb = proj_feat(W2, W2b, 264, 16)
            cpy("act", glrT[:, :], bk[0:16, 0:T], [bb], [glrTb])
            if SUB <= 1:
                return _drain(l, 3)
            for bi, dst, dstb in ((3, q_tok, q_tokb), (4, k_tok, k_tokb)):
                W, Wb = wnext(l, bi)
                for c in range(NCH):
                    bk, bb = proj_tok(W, Wb, 0, 512, c)
                    pc = pc0 + c
                    rA, rAb = ropeA[c % 2], ropeAb[c % 2]
                    rB, rBb = ropeB[c % 2], ropeBb[c % 2]
                    bk3 = bk[:, :].rearrange("p (h d) -> p h d", d=64)
                    rA3 = rA[:, :].rearrange("p (h d) -> p h d", d=64)
                    rB3 = rB[:, :].rearrange("p (h d) -> p h d", d=64)
                    cosb = bc(C("cos2", pc * 64, pc * 64 + 64).unsqueeze(1), [128, 8, 64])
                    sn1 = bc(C("sin2", pc * 64, pc * 64 + 32).unsqueeze(1), [128, 8, 32])
                    sn2 = bc(C("sin2", pc * 64 + 32, pc * 64 + 64).unsqueeze(1), [128, 8, 32])
                    tt("dve", rA3, bk3, cosb, ALU.mult, [bb, CPb], [rAb])
                    tt("dve", rB3[:, :, 0:32], bk3[:, :, 32:64], sn1, ALU.mult, [bb, CPb], [rBb])
                    tt("dve", rB3[:, :, 32:64], bk3[:, :, 0:32], sn2, ALU.mult, [bb, CPb], [rBb])
                    tt("pool", dst[:, c, :], rA[:, :], rB[:, :], ALU.add, [rAb, rBb], [dstb[c]])
            W, Wb = wnext(l, 5)
            for c in range(NCH):
                bk, bb = proj_tok(W, Wb, 0, 512, c)
                cpy("act", v_tok[:, c, :], bk[:, :], [bb], [v_tokb[c]])
            W, Wb = wnext(l, 6)
            for c in range(NCH):
                bk, bb = proj_tok(W, Wb, 0, 512, c)
                act(srg[:, c, :], bk[:, :], AF.Silu, [bb], [srgb[c]])
            if SUB <= 2:
                return _drain(l, 7)
            W, Wb = wnext(l, 7)
            for j in range(2):
                bk, bb = proj_feat(W, Wb, j * 128, 128)
                cpy("act", gqT[:, j, :], bk[:, 0:T], [bb], [gqTb])
                bk, bb = proj_feat(W, Wb, 256 + j * 128, 128)
                cpy("act", gkT[:, j, :], bk[:, 0:T], [bb], [gkTb])
            for c in range(NCH):
                bk, bb = proj_tok(W, Wb, 256, 256, c)
                cpy("dve", gk_tok[:, c, :], bk[:, 0:256], [bb], [gk_tokb[c]])
            W, Wb = wnext(l, 8)
            for c in range(NCH):
                bk, bb = proj_tok(W, Wb, 0, 512, c)
                cpy("act", gv_tok[:, c, :], bk[:, :], [bb], [gv_tokb[c]])
            W, Wb = wnext(l, 9)
            for c in range(NCH):
                bk, bb = proj_tok(W, Wb, 0, 512, c)
                act(sgr[:, c, :], bk[:, :], AF.Silu, [bb], [sgrb[c]])
            if SUB <= 3:
                return _drain(l, 10)
            for c in range(NCH):
                gens = [(ssd_chunk(l, c, first and c == 0), None),
                        (ret_chunk(l, c, first and c == 0), {"ids": [3, 4], "k": 0}),
                        (gla_chunk(l, c, first and c == 0), {"ids": [5, 6], "k": 0})]
                while gens:
                    for item in list(gens):
                        _bctx[0] = item[1]
                        try:
                            next(item[0])
                        except StopIteration:
                            gens.remove(item)
                _bctx[0] = None
            if SUB <= 6:
                return _drain(l, 10)
            for i in range(3):
                Wg0, Wg0b = wnext(l, 10 + 3 * i)
                Wg1, Wg1b = wnext(l, 11 + 3 * i)
                Wbr, Wbrb = wnext(l, 12 + 3 * i)
                for oc in range(8):
                    Wg, Wgb = (Wg0, Wg0b) if oc < 4 else (Wg1, Wg1b)
                    bk, bb = proj_feat(Wg, Wgb, (oc % 4) * 128, 128)
                    th, thb = gth[oc % 2], gthb[oc % 2]
                    act(th[:], bk[:, 0:T], AF.Tanh, [bb, PPb], [thb], bias=BGH[:, l, 8 * i + oc:8 * i + oc + 1], scale=0.5)
                    bk2, bb2 = bank()
                    for kc in range(4):
                        mm(bk2[:, 0:T], Wbr[:, kc, oc * 128:(oc + 1) * 128], yT[i][:, kc, :], kc == 0, kc == 3,
                           [Wbrb, yTb[i]], [bb2])
                    if i == 0:
                        stt("dve", mrg[:, oc, :], th[:], 1.0, bk2[:, 0:T], ALU.add, ALU.mult, [thb, bb2], [mrgb[oc]])
                    else:
                        g_, gB = gtmp[oc % 2], gtmpb[oc % 2]
                        stt("dve", g_[:], th[:], 1.0, bk2[:, 0:T], ALU.add, ALU.mult, [thb, bb2], [gB])
                        if i == 1:
                            tt("pool", mrg[:, oc, :], mrg[:, oc, :], g_[:], ALU.add, [gB, mrgb[oc]], [mrgb[oc]])
                        else:
                            tt("pool", mT[:, oc, :], mrg[:, oc, :], g_[:], ALU.add, [gB, mrgb[oc]], [mTb])
            for half in range(2):
                W, Wb = wnext(l, 19 + half)
                for o4 in range(4):
                    oc = half * 4 + o4
                    bk, bb = bank()
                    for kc in range(8):
                        mm(bk[:, 0:T], W[:, kc, o4 * 128:(o4 + 1) * 128], mT[:, kc, :], kc == 0, kc == 7, [Wb, mTb], [bb])
                    tt("dve", X[:, oc, :], X[:, oc, :], bk[:, 0:T], ALU.add, [Xb, bb], [Xb])

        def ffn(l, first):
            rmsnorm()
            pend = None

            def fin(p):
                j_, accs_, jj_ = p
                g_, gB = fsg[jj_], fsgb[jj_]
                act(g_[:], accs_[0][0][:], AF.Silu, [accs_[0][1]], [gB])
                tt("dve", fa[:, j_, :], g_[:], accs_[1][0][:], ALU.mult, [gB, accs_[1][1]], [fab[j_]])

            for b in range(11):
                W, Wb = wnext(l, 21 + b)
                for jj in range(2):
                    j = 2 * b + jj
                    accs = []
                    for gv in range(2):
                        ci = j + 22 * gv
                        bk, bb = proj_feat(W, Wb, gv * 256 + jj * 128, 128)
                        u_, uB = ub[(2 * jj + gv) % 4], ubb[(2 * jj + gv) % 4]
                        a_, aB = facc[(2 * jj + gv) % 4], faccb[(2 * jj + gv) % 4]
                        if first:
                            S.op("pool", lambda e, u_=u_: e.memset(u_[:, 0:2], 0.0), [], [uB])
                        else:
                            cpy("pool", u_[:, 0:2], halo_f[l][:, ci, :], [halo_fb[l]], [uB])
                        if True:
                            act(u_[:, 2:2 + T], bk[:, 0:T], AF.Copy, [bb], [uB])
                        else:
                            cpy("dve", u_[:, 2:2 + T], bk[:, 0:T], [bb], [uB])
                        act(a_[:], bk[:, 0:T], AF.Identity, [bb, PPb], [aB],
                            bias=P(l, "fb", ci, ci + 1), scale=P(l, "fw", 88 + ci, 89 + ci))
                        stt("dve", a_[:], u_[:, 1:1 + T], P(l, "fw", 44 + ci, 45 + ci), a_[:], ALU.mult, ALU.add, [uB, aB, PPb], [aB])
                        stt("dve", a_[:], u_[:, 0:T], P(l, "fw", ci, ci + 1), a_[:], ALU.mult, ALU.add, [uB, aB, PPb], [aB])
                        cpy("pool", halo_f[l][:, ci, :], u_[:, T:T + 2], [uB], [halo_fb[l]])
                        accs.append((a_, aB))
                    if pend is not None:
                        fin(pend)
                    pend = (j, accs, jj)
            fin(pend)
            for oc in range(8):
                W, Wb = wnext(l, 32 + oc)
                bk, bb = bank()
                for j in range(22):
                    mm(bk[:, 0:T], W[:, j, 0:128], fa[:, j, :], j == 0, j == 21, [Wb, fab[j]], [bb])
                tt("dve", X[:, oc, :], X[:, oc, :], bk[:, 0:T], ALU.add, [Xb, bb], [Xb])

        def ssd_chunk(l, c, zero_state):
            cs = slice(c * 128, (c + 1) * 128)
            act(dtv[:], dtraw[:, c, :], AF.Exp, [dtrawb], [smallb])
            act(dtv[:], dtv[:], AF.Ln, [smallb], [smallb], bias=1.0)
            tt("dve", la[:], dtv[:], AN[:, l, :], ALU.mult, [smallb, PPb], [smallb])
            bk, bb = banks[0], bankb[0]
            mm(bk[:, 0:8], C("triI"), la[:], True, True, [CPb, smallb], [bb], inc=False)
            mm(bk[:, 8:16], C("triS"), la[:], True, True, [CPb, smallb], [bb], inc=False)
            mm(bk[:, 16:24], C("ones"), la[:], True, True, [CPb, smallb], [bb], inc=False)
            mm(bk[0:8, 128:256], la[:], C("triI"), True, True, [CPb, smallb], [bb])
            act(ex3[:], bk[:, 0:24], AF.Exp, [bb], [ex3b])
            act(decsel[0:64, :], bk[0:64, 16:20], AF.Exp, [bb], [decselb])
            act(decsel[64:128, :], bk[64:128, 20:24], AF.Exp, [bb], [decselb])
            act(ncr[:], bk[0:8, 128:256], AF.Copy, [bb], [ncrb], scale=-1.0)
            yield
            tt("pool", Bm[:], bc(C("triI").unsqueeze(1), [128, 8, 128]), bc(la[:, :].unsqueeze(2), [128, 8, 128]),
               ALU.mult, [CPb, smallb], [Bmb])
            sk, skb = banks[7][:, 0:256], bankb[7]
            for g in range(2):
                cpy("pool", Cm[g][g * 64:(g + 1) * 64, :], xact[g * 64:(g + 1) * 64, 5, cs], [xactb[5]], [Cmb[g]])
            yield
            for g in range(2):
                mm(sk[:, g * 128:(g + 1) * 128], xact[:, 4, cs], Cm[g][:, :],
                   True, True, [xactb[4], Cmb[g]], [skb], inc=(g == 1))
            tk, tkb = banks[1], bankb[1]
            tkv = tk[:, 0:256].bitcast(BF16)
            for j in range(4):
                trp(tkv[:, j * 128:(j + 1) * 128], xact[:, j, cs], CBc("identb"), [xactb[j], CBb], [tkb], inc=(j == 3))
            tb, tbb = banks[2], bankb[2]
            tbv = tb[:, 0:64].bitcast(BF16)
            trp(tbv, xact[:, 4, cs], CBc("identb"), [xactb[4], CBb], [tbb])
            yield
            cpy("act", xs_tok[:], tkv, [tkb], [xs_tokb])
            cpy("act", B_tok[:], tbv, [tbb], [B_tokb])
            yield
            tt("dve", vv[:, :].rearrange("p (h d) -> p h d", d=64), xs_tok[:, :].rearrange("p (h d) -> p h d", d=64),
               bc(dtv[:, :].unsqueeze(2), [128, 8, 64]), ALU.mult, [xs_tokb, smallb], [vvb])
            tt("pool", vw[:, :].rearrange("p (h d) -> p h d", d=64), vv[:, :].rearrange("p (h d) -> p h d", d=64),
               bc(ex3[:, 8:16].unsqueeze(2), [128, 8, 64]), ALU.mult, [vvb, ex3b], [vwb])
            yield
            for g in range(2):
                dk, dkb = banks[1 + g], bankb[1 + g]
                mm(dk[:, :], C("ones"), Bm[:, 4 * g:4 * g + 4, :].rearrange("p h t -> p (h t)"), True, False, [CPb, Bmb], [dkb], inc=False)
                mm(dk[:, :], ncr[:], C("sel", g * 512, (g + 1) * 512, 0, 8), False, False, [ncrb, CPb], [dkb], inc=False)
                mm(dk[:, :], CBc("identb"), CBc("negmask4"), False, True, [CBb], [dkb])
                yield
                act(Esb[g][:], dk[:, :], AF.Exp, [dkb], [Esbb[g]])
                tt("dve", Psb[g][:, :].rearrange("p (h t) -> p h t", h=4), Esb[g][:, :].rearrange("p (h t) -> p h t", h=4),
                   bc(sk[:, g * 128:(g + 1) * 128].unsqueeze(1), [128, 4, 128]), ALU.mult, [Esbb[g], skb], [Psbb[g]])
                yield
            ya, yab = banks[1], bankb[1]
            for h in range(8):
                g, hl = h // 4, h % 4
                mm(ya[:, h * 64:(h + 1) * 64], Psb[g][:, hl * 128:(hl + 1) * 128], vv[:, h * 64:(h + 1) * 64], True, True,
                   [Psbb[g], vvb], [yab], inc=(h == 7))
            y1, y1b = ytmp[0], ytmpb[0]
            if not zero_state:
                yb_, ybb_ = banks[2], bankb[2]
                for g in range(2):
                    mm(yb_[:, g * 256:(g + 1) * 256], Cm[g][:, :], Sssd16[l][:, :], True, True,
                       [Cmb[g], Sssd16b[l]], [ybb_], inc=(g == 1))
                yield
                tt("dve", y1[:, :].rearrange("p (h d) -> p h d", d=64), yb_[:, :].rearrange("p (h d) -> p h d", d=64),
                   bc(ex3[:, 0:8].unsqueeze(2), [128, 8, 64]), ALU.mult, [ybb_, ex3b], [y1b])
                tt("dve", y1[:], y1[:], ya[:, :], ALU.add, [y1b, yab], [y1b])
            else:
                yield
                cpy("dve", y1[:], ya[:, :], [yab], [y1b])
            yield
            y2, y2b = ytmp[1], ytmpb[1]
            tt("pool", y2[:, :].rearrange("p (h d) -> p h d", d=64), xs_tok[:, :].rearrange("p (h d) -> p h d", d=64),
               bc(P(l, "dsk", 0, 8).unsqueeze(2), [128, 8, 64]), ALU.mult, [xs_tokb, PPb], [y2b])
            tt("pool", y1[:], y1[:], y2[:], ALU.add, [y1b, y2b], [y1b])
            tt("pool", y1[:], y1[:], sz[:, c, :], ALU.mult, [y1b, szb[c]], [y1b])
            yield
            su, sub_ = banks[0], bankb[0]
            for g in range(2):
                mm(su[g * 64:(g + 1) * 64, 0:256], B_tok[:, g * 64:(g + 1) * 64], vw[:, g * 256:(g + 1) * 256], True, True,
                   [B_tokb, vwb], [sub_], inc=(g == 1))
            yield
            if zero_state:
                cpy("dve", Sssd[l][:], su[:, 0:256], [sub_], [Sssdb[l]])
            else:
                tt("pool", Sssd[l][:, :].rearrange("p (h d) -> p h d", d=64), Sssd[l][:, :].rearrange("p (h d) -> p h d", d=64),
                   bc(decsel[:, :].unsqueeze(2), [128, 4, 64]), ALU.mult, [Sssdb[l], decselb], [Sssdb[l]])
                tt("dve", Sssd[l][:], Sssd[l][:], su[:, 0:256], ALU.add, [Sssdb[l], sub_], [Sssdb[l]])
            cpy("act", Sssd16[l][:], Sssd[l][:], [Sssdb[l]], [Sssd16b[l]])
            yield
            act(y2[:, 0:256], y1[:, 0:256], AF.Square, [y1b], [y2b, st8b], accum=st8[:, 0:1])
            act(y2[:, 256:512], y1[:, 256:512], AF.Square, [y1b], [y2b, st8b], accum=st8[:, 1:2])
            act(st8[:, 2:4], st8[:, 0:2], AF.Ln, [st8b], [st8b], bias=GN_EPS, scale=1.0 / 256.0)
            act(st8[:, 2:4], st8[:, 2:4], AF.Exp, [st8b], [st8b], scale=-0.5)
            yield
            act(ybf[:, 0:256], y1[:, 0:256], AF.Copy, [y1b, st8b], [ybfb], scale=st8[:, 2:3])
            act(ybf[:, 256:512], y1[:, 256:512], AF.Copy, [y1b, st8b], [ybfb], scale=st8[:, 3:4])
            yield
            yield from finish_y(0, c, ybf, [ybfb], (banks[1], bankb[1]))

        def finish_y(i, c, yb_ap, yb_bufs, bk_=None):
            tk, tkb = bk_ if bk_ is not None else bank()
            tkv = tk[:, 0:256].bitcast(BF16)
            for j in range(4):
                trp(tkv[:, j * 128:(j + 1) * 128], yb_ap[:, j * 128:(j + 1) * 128], CBc("identb"), list(yb_bufs) + [CBb], [tkb], inc=(j == 3))
            yield
            cpy("act", yT[i][:, :, c * 128:(c + 1) * 128], tkv.rearrange("p (j t) -> p j t", j=4), [tkb], [yTb[i]])

        def ret_chunk(l, c, zero_state):
            tq, tqb = bank()
            tqv = tq[:, 0:256].bitcast(BF16)
            for j in range(4):
                trp(tqv[:, j * 128:(j + 1) * 128], q_tok[:, c, j * 128:(j + 1) * 128], CBc("identb"), [q_tokb[c], CBb], [tqb], inc=(j == 3))
            tk_, tkb_ = bank()
            tkv = tk_[:, 0:256].bitcast(BF16)
            for j in range(4):
                trp(tkv[:, j * 128:(j + 1) * 128], k_tok[:, c, j * 128:(j + 1) * 128], CBc("identb"), [k_tokb[c], CBb], [tkb_], inc=(j == 3))
            yield
            for hh in range(2):
                r0, r1 = hh * 64, hh * 64 + 64
                cpy("act", qm[hh][r0:r1, :, :].rearrange("p j t -> p (j t)"), tqv[r0:r1, :], [tqb], [qmb[hh]])
                tt("dve", qdm[hh][r0:r1, :, :].rearrange("p j t -> p (j t)"), qm[hh][r0:r1, :, :].rearrange("p j t -> p (j t)"),
                   C("qdec", 0, None, r0, r1), ALU.mult, [qmb[hh], CPb], [qdmb[hh]])
            cpy("act", kTs[:, :, :].rearrange("p j t -> p (j t)"), tkv, [tkb_], [kTsb])
            yield
            tt("pool", kw[:, :].rearrange("p (h d) -> p h d", d=64), k_tok[:, c, :].rearrange("p (h d) -> p h d", d=64),
               bc(C("wtab").unsqueeze(2), [128, 8, 64]), ALU.mult, [k_tokb[c], CPb], [kwb])
            for g in range(2):
                sk, skb = bank()
                for hl in range(4):
                    h = 4 * g + hl
                    ps = slice((h % 2) * 64, (h % 2) * 64 + 64)
                    mm(sk[:, hl * 128:(hl + 1) * 128], kTs[:, h // 2, :], qm[h % 2][:, h // 2, :], True, True, [kTsb, qmb[h % 2]], [skb], inc=(hl == 3))
                yield
                tt("dve", Psb_r[g][:, :], sk[:, :], C("dret", g * 512, (g + 1) * 512), ALU.mult, [skb, CPb], Psb_rB[g])
            yield
            ya, yab = bank()
            for h in range(8):
                g, hl = h // 4, h % 4
                ps = slice((h % 2) * 64, (h % 2) * 64 + 64)
                mm(ya[:, h * 64:(h + 1) * 64], Psb_r[g][:, hl * 128:(hl + 1) * 128], v_tok[:, c, h * 64:(h + 1) * 64], True, zero_state,
                   Psb_rB[g] + [v_tokb[c]], [yab], inc=(zero_state and h == 7))
                if not zero_state:
                    mm(ya[:, h * 64:(h + 1) * 64], qdm[h % 2][:, h // 2, :], Sret16[l][:, (h // 2) * 64:(h // 2) * 64 + 64], False, True,
                       [qdmb[h % 2], Sret16b[l]], [yab], inc=(h == 7))
            yield
            su, sub_ = bank()
            for h in range(8):
                ps = slice((h % 2) * 64, (h % 2) * 64 + 64)
                mm(su[ps, (h // 2) * 64:(h // 2) * 64 + 64], kw[:, h * 64:(h + 1) * 64], v_tok[:, c, h * 64:(h + 1) * 64], True, True,
                   [kwb, v_tokb[c]], [sub_], inc=(h == 7))
            yield
            if zero_state:
                cpy("dve", Sret[l][:], su[:, 0:256], [sub_], [Sretb[l]])
            else:
                tt("pool", Sret[l][:, :].rearrange("p (j d) -> p j d", d=64), Sret[l][:, :].rearrange("p (j d) -> p j d", d=64),
                   bc(C("decs").unsqueeze(2), [128, 4, 64]), ALU.mult, [Sretb[l], CPb], [Sretb[l]])
                tt("dve", Sret[l][:], Sret[l][:], su[:, 0:256], ALU.add, [Sretb[l], sub_], [Sretb[l]])
            cpy("act", Sret16[l][:], Sret[l][:], [Sretb[l]], [Sret16b[l]])
            yield
            y1 = y1r
            for h in range(8):
                act(y1[:, h * 64:(h + 1) * 64], ya[:, h * 64:(h + 1) * 64], AF.Identity, [yab], y1rB + [st8r], accum=st8[:, 8 + h:9 + h])
                act(y1[:, h * 64:(h + 1) * 64], ya[:, h * 64:(h + 1) * 64], AF.Square, [yab], y1rB + [st8r], accum=st8[:, 16 + h:17 + h])
                if h % 2:
                    yield
            ts("dve", st8[:, 24:32], st8[:, 8:16], 1.0 / 64.0, None, ALU.mult, None, [st8r], [st8r])
            tt("dve", st8[:, 32:40], st8[:, 24:32], st8[:, 24:32], ALU.mult, [st8r], [st8r])
            stt("dve", st8[:, 48:56], st8[:, 16:24], 1.0 / 64.0, st8[:, 32:40], ALU.mult, ALU.subtract, [st8r], [st8r])
            yield
            act(st8[:, 48:56], st8[:, 48:56], AF.Ln, [st8r], [st8r], bias=GN_EPS)
            act(st8[:, 48:56], st8[:, 48:56], AF.Exp, [st8r], [st8r], scale=-0.5)
            yield
            tt("dve", y1[:, :].rearrange("p (h d) -> p h d", d=64), ya[:, :].rearrange("p (h d) -> p h d", d=64),
               bc(st8[:, 24:32].unsqueeze(2), [128, 8, 64]), ALU.subtract, [yab, st8r], y1rB)
            yield
            tt("pool", y1[:, :].rearrange("p (h d) -> p h d", d=64), y1[:, :].rearrange("p (h d) -> p h d", d=64),
               bc(st8[:, 48:56].unsqueeze(2), [128, 8, 64]), ALU.mult, y1rB + [st8r], y1rB)
            tt("pool", ybf_r[:, :], y1[:, :], srg[:, c, :], ALU.mult, y1rB + [srgb[c]], ybf_rB)
            yield
            yield from finish_y(1, c, ybf_r, ybf_rB)

        def gla_chunk(l, c, zero_state):
            cs = slice(c * 128, (c + 1) * 128)
            al, alb = bank()
            mm(al[:, 0:256], glrT[:, cs], WA2[:, l, :], True, False, [glrTb, PPb], [alb], inc=False)
            mm(al[:, 0:256], C("ones", 0, 128, 0, 1), BA[:, l * 256:(l + 1) * 256], False, True, [CPb, PPb], [alb])
            yield
            act(gE[:], al[:, 0:256], AF.Exp, [alb], [gEb], scale=-1.0)
            act(gL[:], gE[:], AF.Ln, [gEb], [gLb], bias=1.0)
            yield
            ck, ckb = bank()
            for j in range(2):
                mm(ck[:, j * 128:(j + 1) * 128], gL[:, j * 128:(j + 1) * 128], C("triI16"), True, True, [gLb, CPb], [ckb], inc=False)
            mm(ck[:, 256:512], C("triS16"), gL[:], True, True, [gLb, CPb], [ckb])
            yield
            act(epos[:, :, :].rearrange("p j t -> p (j t)"), ck[:, 0:256], AF.Exp, [ckb], [eposb], bias=math.log(0.125))
            act(eneg[:, :, :].rearrange("p j t -> p (j t)"), ck[:, 0:256], AF.Exp, [ckb], [enegb], scale=-1.0)
            act(gwst[:], ck[:, 256:512], AF.Exp, [ckb], [gwstb])
            tk_, tkb_ = bank()
            for j in range(2):
                mm(tk_[:, 2 * j:2 * j + 2], gL[:, j * 128:(j + 1) * 128], C("m16"), True, True, [gLb, CPb], [tkb_], inc=(j == 1))
            yield
            act(gdec[:], tk_[:, 0:4].rearrange("p (j two) -> p j two", two=2)[:, :, 0], AF.Exp, [tkb_], [gdecb])
            for hh in range(2):
                r0, r1 = hh * 64, hh * 64 + 64
                tt("pool", gqdm[hh][r0:r1, :, :], gqT[r0:r1, :, cs], epos[r0:r1, :, :], ALU.mult, [gqTb, eposb], [gqdmb[hh]])
            tt("pool", gki[:], gkT[:, :, cs], eneg[:], ALU.mult, [gkTb, enegb], [gkib])
            tt("dve", gks[:], gk_tok[:, c, :], gwst[:], ALU.mult, [gk_tokb[c], gwstb], [gksb])
            yield
            sk, skb = bank()
            for h in range(4):
                ps = slice((h % 2) * 64, (h % 2) * 64 + 64)
                mm(sk[:, h * 128:(h + 1) * 128], gki[:, h // 2, :], gqdm[h % 2][:, h // 2, :], True, True, [gkib, gqdmb[h % 2]], [skb], inc=(h == 3))
            yield
            tt("dve", Psb_g[:, :], sk[:, :], C("causal4"), ALU.mult, [skb, CPb], Psb_gB)
            yield
            ya, yab = bank()
            for h in range(4):
                ps = slice((h % 2) * 64, (h % 2) * 64 + 64)
                mm(ya[:, h * 128:(h + 1) * 128], Psb_g[:, h * 128:(h + 1) * 128], gv_tok[:, c, h * 128:(h + 1) * 128], True, zero_state,
                   Psb_gB + [gv_tokb[c]], [yab], inc=(zero_state and h == 3))
                if not zero_state:
                    mm(ya[:, h * 128:(h + 1) * 128], gqdm[h % 2][:, h // 2, :], Sgla16[l][:, (h // 2) * 128:(h // 2) * 128 + 128], False, True,
                       [gqdmb[h % 2], Sgla16b[l]], [yab], inc=(h == 3))
            yield
            su, sub_ = bank()
            for h in range(4):
                ps = slice((h % 2) * 64, (h % 2) * 64 + 64)
                mm(su[ps, (h // 2) * 128:(h // 2) * 128 + 128], gks[:, h * 64:(h + 1) * 64], gv_tok[:, c, h * 128:(h + 1) * 128], True, True,
                   [gksb, gv_tokb[c]], [sub_], inc=(h == 3))
            yield
            if zero_state:
                cpy("dve", Sgla[l][:], su[:, 0:256], [sub_], [Sglab[l]])
            else:
                for j in range(2):
                    stt("dve", Sgla[l][:, j * 128:(j + 1) * 128], Sgla[l][:, j * 128:(j + 1) * 128], gdec[:, j:j + 1],
                        su[:, j * 128:(j + 1) * 128], ALU.mult, ALU.add, [Sglab[l], gdecb, sub_], [Sglab[l]])
            cpy("act", Sgla16[l][:], Sgla[l][:], [Sglab[l]], [Sgla16b[l]])
            yield
            y1 = y1g
            for g in range(4):
                act(y1[:, g * 128:(g + 1) * 128], ya[:, g * 128:(g + 1) * 128], AF.Square, [yab], y1gB + [st8g], accum=st8[:, 40 + g:41 + g])
            yield
            act(st8[:, 44:48], st8[:, 40:44], AF.Ln, [st8g], [st8g], bias=GN_EPS, scale=1.0 / 128.0)
            act(st8[:, 44:48], st8[:, 44:48], AF.Exp, [st8g], [st8g], scale=-0.5)
            yield
            tt("dve", y1[:, :].rearrange("p (h d) -> p h d", d=128), ya[:, :].rearrange("p (h d) -> p h d", d=128),
               bc(st8[:, 44:48].unsqueeze(2), [128, 4, 128]), ALU.mult, [yab, st8g], y1gB)
            yield
            tt("pool", ybf_g[:, :], y1[:, :], sgr[:, c, :], ALU.mult, y1gB + [sgrb[c]], ybf_gB)
            yield
            yield from finish_y(2, c, ybf_g, ybf_gB)

        outb = Buf()
        for s in range(NSEQ):
            for t in range(NT):
                t0 = t * T
                for c in range(NCH):
                    S.dma(xin[:], x_d[s, t0 + c * 128:t0 + (c + 1) * 128, :], writes=[xinb])
                    for half in range(2):
                        bk, bb = bank()
                        for k4 in range(4):
                            kc = half * 4 + k4
                            trp(bk[:, k4 * 128:(k4 + 1) * 128], xin[:, kc * 128:(kc + 1) * 128], C("identf"), [xinb, CPb], [bb], inc=(k4 == 3))
                        cpy("act", X[:, half * 4:half * 4 + 4, c * 128:(c + 1) * 128], bk[:, :].rearrange("p (k t) -> p k t", k=4), [bb], [Xb])
                for l in range(L):
                    if stage >= 1:
                        mixer(l, t == 0, (t0 // 128))
                    if stage >= 2:
                        ffn(l, t == 0)
                bk, bb = bank()
                for kc in range(8):
                    s_, sB = sq[kc % 3], sqb[kc % 3]
                    act(s_[:], X[:, kc, :], AF.Square, [Xb], [sB])
                    mm(bk[:, 0:T], CBc("cmean"), s_[:], kc == 0, kc == 7, [sB, CBb], [bb], inc=True)
                act(rstd[:], bk[:, 0:T], AF.Ln, [bb], [rstdb], bias=RMS_EPS)
                act(rstd[:], rstd[:], AF.Exp, [rstdb], [rstdb], scale=-0.5)
                for kc in range(8):
                    stt("dve", X[:, kc, :], X[:, kc, :], PP[:, PP_L * L + kc:PP_L * L + kc + 1], rstd[:], ALU.mult, ALU.mult,
                        [Xb, rstdb, PPb], [Xb])
                for c in range(NCH):
                    for half in range(2):
                        bk, bb = bank()
                        for k4 in range(4):
                            kc = half * 4 + k4
                            trp(bk[:, k4 * 128:(k4 + 1) * 128], X[:, kc, c * 128:(c + 1) * 128], C("identf"), [Xb, CPb], [bb], inc=(k4 == 3))
                        cpy("act", xout[:, half * 512:(half + 1) * 512], bk[:, :], [bb], [xoutb])
                    S.dma(out_d[s, t0 + c * 128:t0 + (c + 1) * 128, :], xout[:], reads=[xoutb], writes=[outb])
        if debug and dbg_list:
            o = 0
            for ap_, b, width in dbg_list:
                S.dma(dbg_d[:, o:o + width], ap_[:, 0:width], reads=[b], writes=[outb])
                o += width
        S.finish()
        with nc.Block() as block:
            S.replay(block)
    print('n_inst', S.n_inst, {k: len(v.stream) for k, v in S.E.items()})
    return nc, cp_np, cbp_np


def _run(inputs, L, NSEQ, SEQLEN, ncores, debug=None, stage=2):
    nc, cp_np, cbp_np = build_nc(L, NSEQ, SEQLEN, debug=debug, stage=stage)
    p = {k: np.asarray(v) for k, v in inputs.items()}
    pp = _param_pack(L, p)
    f = lambda a: np.ascontiguousarray(np.asarray(a, np.float32))
    shared = {
        "w_in": f(p["w_in"]), "w_branch": f(p["w_branch"]), "w_out": f(p["w_out"]), "w_up": f(p["w_up"]),
        "w_down": f(p["w_down"]), "gla_w_alpha2": f(p["gla_w_alpha2"]), "gla_b_alpha": f(p["gla_b_alpha"]),
        "pp": pp, "cp": cp_np, "cbp": cbp_np,
    }
    x = f(p["x"])
    in_maps = []
    for i in range(ncores):
        m = dict(shared)
        m["x"] = np.ascontiguousarray(x[i * NSEQ:(i + 1) * NSEQ])
        in_maps.append(m)
    res = run_bass_kernel_spmd(nc, in_maps, core_ids=list(range(ncores)))
    out = np.concatenate([r["out"] for r in res.results], axis=0)
    if debug:
        return out, [r["dbg"] for r in res.results]
    return out


def kernel(**inputs):
    x = inputs["x"]
    B, SEQLEN, _ = x.shape
    L = inputs["w_in"].shape[0]
    ncores = 8
    return _run(inputs, L, B // ncores, SEQLEN, ncores).astype(np.float32)
```

```python
import math
from contextlib import ExitStack

import numpy as np
import ml_dtypes
import concourse.bass as bass
import concourse.mybir as mybir
from concourse.bass_utils import run_bass_kernel_spmd

F32 = mybir.dt.float32
BF16 = mybir.dt.bfloat16
AF = mybir.ActivationFunctionType
ALU = mybir.AluOpType
AX = mybir.AxisListType

D = 1024
NIN = 7960
DFF = 2816
NBLK = 40
RMS_EPS = 1e-6
GN_EPS = 1e-5


class Buf:
    __slots__ = ("last_w", "reads")

    def __init__(self):
        self.last_w = None
        self.reads = {}


class Eng:
    def __init__(self, name, sem, inc):
        self.name = name
        self.sem = sem
        self.inc = inc
        self.count = 0
        self.seen = {}
        self.stream = []
        self.pending = False
        self.needed = set()


class Sched:
    NLANES = 12

    def __init__(self, nc, sems):
        self.nc = nc
        self.E = {}
        for i, n in enumerate(["pe", "act", "dve", "pool"]):
            self.E[n] = Eng(n, sems[i], 1)
        self.sp = Eng("sp", None, 0)
        self.lanes = [Eng("lane%d" % i, sems[4 + i], 16) for i in range(self.NLANES)]
        self.lane_rr = 0
        self.n_inst = 0

    def _deps(self, reads, writes):
        deps = {}
        for b in reads:
            if b.last_w is not None:
                e, c = b.last_w
                if deps.get(e, 0) < c:
                    deps[e] = c
        for b in writes:
            if b.last_w is not None:
                e, c = b.last_w
                if deps.get(e, 0) < c:
                    deps[e] = c
            for e, c in b.reads.items():
                if deps.get(e, 0) < c:
                    deps[e] = c
        return deps

    def _emit_waits(self, E, deps, skip_self=False):
        for e, c in deps.items():
            if e is E and skip_self:
                continue
            if E.seen.get(e, 0) >= c:
                continue
            E.seen[e] = c
            e.needed.add(c)
            E.stream.append(("wait", e, c))

    def op(self, eng, fn, reads=(), writes=(), inc=True):
        E = self.E[eng]
        deps = self._deps(reads, writes)
        self._emit_waits(E, deps, skip_self=(eng == "pe"))
        stamp = E.count + E.inc
        if inc:
            E.count = stamp
            E.pending = False
        else:
            E.pending = True
        E.stream.append(("inst", fn, inc, stamp))
        for b in reads:
            b.reads[E] = stamp
        for b in writes:
            b.last_w = (E, stamp)
            b.reads = {}
        self.n_inst += 1

    def dma(self, out, in_, reads=(), writes=()):
        L = self.lanes[self.lane_rr]
        self.lane_rr = (self.lane_rr + 1) % self.NLANES
        deps = self._deps(reads, writes)
        if L.count > 0:
            deps[L] = max(deps.get(L, 0), L.count)
        self._emit_waits(self.sp, deps)
        L.count += 16
        stamp = L.count
        self.sp.stream.append(("dma", out, in_, L.sem))
        for b in reads:
            b.reads[L] = stamp
        for b in writes:
            b.last_w = (L, stamp)
            b.reads = {}
        self.n_inst += 1

    def barrier(self):
        allE = list(self.E.values()) + self.lanes
        for E in list(self.E.values()) + [self.sp]:
            deps = {e: e.count for e in allE if e.count > 0 and e is not E}
            self._emit_waits(E, deps)

    def finish(self):
        deps = {e: e.count for e in list(self.E.values()) + self.lanes if e.count > 0}
        self._emit_waits(self.sp, deps)

    def replay(self, block):
        rank = {}
        for e in list(self.E.values()):
            if e.name == "pool":
                continue
            rank[e] = {c: i + 1 for i, c in enumerate(sorted(e.needed))}
        n_skipped = [0]

        def run(E, eng):
            for item in E.stream:
                if item[0] == "wait":
                    e, c = item[1], item[2]
                    eng.wait_ge(e.sem, rank[e][c] if e in rank else c)
                elif item[0] == "inst":
                    ins = item[1](eng)
                    if item[2]:
                        if E in rank and item[3] not in rank[E]:
                            n_skipped[0] += 1
                        else:
                            ins.then_inc(E.sem, 1)
                else:
                    eng.dma_start(out=item[1], in_=item[2]).then_inc(item[3], 16)

        for E in self.E.values():
            assert not E.pending, E.name

        @block.tensor
        def _(e):
            run(self.E["pe"], e)

        @block.scalar
        def _(e):
            run(self.E["act"], e)

        @block.vector
        def _(e):
            run(self.E["dve"], e)

        @block.gpsimd
        def _(e):
            run(self.E["pool"], e)

        @block.sync
        def _(e):
            run(self.sp, e)


def _const_pack(seqlen):
    nchs = seqlen // 128
    s = np.arange(128)[:, None].astype(np.float64)
    t = np.arange(128)[None, :].astype(np.float64)
    le = (s <= t)
    cols = {}
    cols["triI"] = le.astype(np.float64)
    cols["triS"] = (s > t).astype(np.float64)
    cols["ones"] = np.ones((128, 128))
    cols["triI16"] = -le.astype(np.float64) / 16.0
    cols["triS16"] = -(s > t).astype(np.float64) / 16.0
    cols["m16"] = -np.ones((128, 2)) / 16.0
    cols["causal4"] = np.tile(le.astype(np.float64), (1, 4))
    lg = np.log(1.0 - np.exp2(-5.0 - np.arange(8, dtype=np.float64)))
    dret = np.zeros((128, 8, 128))
    for h in range(8):
        dret[:, h, :] = np.where(le, np.exp(lg[h] * (t - s)), 0.0) * 0.125
    cols["dret"] = dret.reshape(128, 1024)
    qd = np.zeros((128, 4, 128))
    decs = np.zeros((128, 4))
    for j in range(4):
        for hh in range(2):
            h = 2 * j + hh
            qd[hh * 64:(hh + 1) * 64, j, :] = np.exp(lg[h] * (np.arange(128) + 1.0))[None, :] * 0.125
            decs[hh * 64:(hh + 1) * 64, j] = np.exp(lg[h] * 128.0)
    cols["qdec"] = qd.reshape(128, 512)
    cols["decs"] = decs
    wt = np.zeros((128, 8))
    for h in range(8):
        wt[:, h] = np.exp(lg[h] * (127.0 - np.arange(128)))
    cols["wtab"] = wt
    inv = 10000.0 ** (-np.arange(32, dtype=np.float32) / 32.0)
    pos = np.arange(seqlen, dtype=np.float32)
    ang = (pos[:, None] * inv[None, :]).astype(np.float32)
    cos = np.cos(ang).astype(np.float64)
    sin = np.sin(ang).astype(np.float64)
    cos2 = np.concatenate([cos, cos], axis=1).reshape(nchs, 128, 64).transpose(1, 0, 2)
    sin2 = np.concatenate([-sin, sin], axis=1).reshape(nchs, 128, 64).transpose(1, 0, 2)
    cols["cos2"] = cos2.reshape(128, nchs * 64)
    cols["sin2"] = sin2.reshape(128, nchs * 64)
    sel = np.zeros((128, 2, 4, 128))
    for g in range(2):
        for hl in range(4):
            sel[4 * g + hl, g, hl, :] = 1.0
    cols["sel"] = sel.reshape(128, 1024)
    cols["identf"] = np.eye(128)
    off = {}
    o = 0
    arrs = []
    for k, v in cols.items():
        off[k] = (o, v.shape[1])
        o += v.shape[1]
        arrs.append(v)
    cp = np.ascontiguousarray(np.concatenate(arrs, axis=1).astype(np.float32))
    cb = {}
    cb["identb"] = np.eye(128)
    cb["negmask4"] = np.tile(np.where(le, 0.0, -30000.0), (1, 4))
    cb["cmean"] = np.full((128, 128), 1.0 / 1024.0)
    offb = {}
    o = 0
    arrs = []
    for k, v in cb.items():
        offb[k] = (o, v.shape[1])
        o += v.shape[1]
        arrs.append(v)
    cbp = np.ascontiguousarray(np.concatenate(arrs, axis=1).astype(ml_dtypes.bfloat16))
    return cp, off, cbp, offb


PP_FIELDS = [("gmix", 8), ("gffn", 8), ("gbr", 12), ("cw", 24), ("cb", 6), ("bg", 24),
             ("fw", 132), ("fb", 44), ("dtb", 8), ("alog", 8), ("dsk", 8)]
PP_L = sum(n for _, n in PP_FIELDS)


def _pp_off(L):
    off = {}
    o = 0
    for k, n in PP_FIELDS:
        off[k] = o
        o += n
    return off, PP_L * L + 8


def _param_pack(L, p):
    off, tot = _pp_off(L)
    pp = np.zeros((128, tot), np.float32)

    def fm(v, nk):
        return np.asarray(v, np.float32).reshape(nk, 128).T

    for l in range(L):
        b = l * PP_L
        pp[:, b + off["gmix"]: b + off["gmix"] + 8] = fm(p["norm_mix_g"][l], 8)
        pp[:, b + off["gffn"]: b + off["gffn"] + 8] = fm(p["norm_ffn_g"][l], 8)
        for i, nm in enumerate(["ssm_norm_g", "ret_norm_g", "gla_norm_g"]):
            pp[:, b + off["gbr"] + 4 * i: b + off["gbr"] + 4 * i + 4] = fm(p[nm][l], 4)
        for j in range(4):
            pp[:, b + off["cw"] + 6 * j: b + off["cw"] + 6 * j + 6] = fm(p["ssm_conv_w"][l, j], 6)
        pp[:, b + off["cb"]: b + off["cb"] + 6] = fm(p["ssm_conv_b"][l], 6)
        for i in range(3):
            pp[:, b + off["bg"] + 8 * i: b + off["bg"] + 8 * i + 8] = fm(p["b_gate"][l, i], 8)
        for j in range(3):
            pp[:, b + off["fw"] + 44 * j: b + off["fw"] + 44 * j + 44] = fm(p["ffn_conv_w"][l, j], 44)
        pp[:, b + off["fb"]: b + off["fb"] + 44] = fm(p["ffn_conv_b"][l], 44)
        pp[:, b + off["dtb"]: b + off["dtb"] + 8] = np.broadcast_to(p["ssm_dt_bias"][l], (128, 8))
        pp[:, b + off["alog"]: b + off["alog"] + 8] = np.broadcast_to(p["ssm_a_log"][l], (128, 8))
        pp[:, b + off["dsk"]: b + off["dsk"] + 8] = np.broadcast_to(p["ssm_d"][l], (128, 8))
    pp[:, PP_L * L: PP_L * L + 8] = fm(p["norm_f_g"], 8)
    return pp


C_Z, C_XBC, C_DT, C_RQ, C_RK, C_RV, C_RG = 0, 512, 1280, 1288, 1800, 2312, 2824
C_GQ, C_GK, C_GV, C_GR, C_GLR, C_GATE = 3336, 3592, 3848, 4360, 4872, 4888


def _block_defs():
    blks = []
    blks.append(("w_in", [(C_Z, 512)], 8, "gmix"))
    blks.append(("w_in", [(C_XBC, 512)], 8, "gmix"))
    blks.append(("w_in", [(C_XBC + 512, 256), (C_DT, 8), (C_GLR, 16)], 8, "gmix"))
    blks.append(("w_in", [(C_RQ, 512)], 8, "gmix"))
    blks.append(("w_in", [(C_RK, 512)], 8, "gmix"))
    blks.append(("w_in", [(C_RV, 512)], 8, "gmix"))
    blks.append(("w_in", [(C_RG, 512)], 8, "gmix"))
    blks.append(("w_in", [(C_GQ, 512)], 8, "gmix"))
    blks.append(("w_in", [(C_GV, 512)], 8, "gmix"))
    blks.append(("w_in", [(C_GR, 512)], 8, "gmix"))
    for i in range(3):
        blks.append(("w_in", [(C_GATE + i * 1024, 512)], 8, "gmix"))
        blks.append(("w_in", [(C_GATE + i * 1024 + 512, 512)], 8, "gmix"))
        blks.append(("w_branch%d" % i, [(0, 1024)], 4, "gbr%d" % i))
    blks.append(("w_out", [(0, 512)], 8, "half"))
    blks.append(("w_out", [(512, 512)], 8, "half"))
    for b in range(11):
        blks.append(("w_up", [(2 * b * 128, 256), (DFF + 2 * b * 128, 256)], 8, "gffn"))
    for oc in range(8):
        blks.append(("w_down", [(oc * 128, 128)], 22, None))
    assert len(blks) == NBLK
    return blks


def build_nc(L, NSEQ, SEQLEN, T=256, debug=None, stage=2):
    NCH = T // 128
    NT = SEQLEN // T
    nc = bass.Bass("TRN2", target_bir_lowering=False)
    cp_np, coff, cbp_np, cboff = _const_pack(SEQLEN)
    ppoff, pptot = _pp_off(L)

    def dram(name, shape, dt=F32, kind="ExternalInput"):
        return nc.dram_tensor(name, list(shape), dt, kind=kind).ap()

    x_d = dram("x", [NSEQ, SEQLEN, D])
    w_in_d = dram("w_in", [L, D, NIN])
    w_br_d = dram("w_branch", [L, 3, 512, D])
    w_out_d = dram("w_out", [L, D, D])
    w_up_d = dram("w_up", [L, D, 2 * DFF])
    w_dn_d = dram("w_down", [L, DFF, D])
    wa2_d = dram("gla_w_alpha2", [L, 16, 256])
    ba_d = dram("gla_b_alpha", [L, 256])
    pp_d = dram("pp", [128, pptot])
    cp_d = dram("cp", list(cp_np.shape))
    cbp_d = dram("cbp", list(cbp_np.shape), BF16)
    out_d = dram("out", [NSEQ, SEQLEN, D], kind="ExternalOutput")
    ws_d = dram("wscratch", [L, NBLK, 128, 4096], BF16, kind="Internal")
    dbg_d = None
    if debug:
        dbg_d = dram("dbg", [128, debug[1]], kind="ExternalOutput")

    blkdefs = _block_defs()

    with ExitStack() as es:
        sems = [es.enter_context(nc.semaphore("s%d" % i)) for i in range(4 + Sched.NLANES)]
        S = Sched(nc, sems)
        _cnt = [0]

        def sb(shape, dt=F32):
            _cnt[0] += 1
            return es.enter_context(nc.sbuf_tensor("t%d" % _cnt[0], list(shape), dt))

        def act(out, in_, func, r, w, bias=0.0, scale=1.0, accum=None):
            if accum is None:
                S.op("act", lambda e: e.activation(out=out, in_=in_, func=func, bias=bias, scale=scale), r, w)
            else:
                S.op("act", lambda e: e.activation(out=out, in_=in_, func=func, bias=bias, scale=scale,
                                                   accum_out=accum), r, w)

        def tt(eng, out, a, b, op, r, w):
            S.op(eng, lambda e: e.tensor_tensor(out=out, in0=a, in1=b, op=op), r, w)

        def ts(eng, out, a, s1, s2, op0, op1, r, w):
            if s2 is None:
                S.op(eng, lambda e: e.tensor_scalar(out=out, in0=a, scalar1=s1, scalar2=None, op0=op0), r, w)
            else:
                S.op(eng, lambda e: e.tensor_scalar(out=out, in0=a, scalar1=s1, scalar2=s2, op0=op0, op1=op1), r, w)

        def stt(eng, out, a, s, b, op0, op1, r, w):
            S.op("dve", lambda e: e.scalar_tensor_tensor(out=out, in0=a, scalar=s, in1=b, op0=op0, op1=op1), r, w)

        def cpy(eng, out, in_, r, w):
            if eng == "act":
                S.op("act", lambda e: e.copy(out=out, in_=in_), r, w)
            else:
                S.op(eng, lambda e: e.tensor_copy(out=out, in_=in_), r, w)

        def mm(out, lhsT, rhs, start, stop, r, w, inc=None):
            if inc is None:
                inc = stop
            S.op("pe", lambda e: e.matmul(out, lhsT=lhsT, rhs=rhs, start=start, stop=stop), r, w, inc=inc)

        def trp(out, in_, ident, r, w, inc=True):
            S.op("pe", lambda e: e.transpose(out, in_, ident), r, w, inc=inc)

        def bc(ap, shape):
            return ap.to_broadcast(list(shape))

        CP = sb(cp_np.shape)
        CPb = Buf()
        CB = sb(cbp_np.shape, BF16)
        CBb = Buf()
        PP = sb([128, pptot])
        PPb = Buf()
        WA2 = sb([16, L, 256])
        BA = sb([1, L * 256])
        S.dma(CP[:], cp_d[:, :], writes=[CPb])
        S.dma(CB[:], cbp_d[:, :], writes=[CBb])
        S.dma(PP[:], pp_d[:, :], writes=[PPb])
        S.dma(WA2[:], wa2_d.rearrange("l r c -> r l c"), writes=[PPb])
        S.dma(BA[:], ba_d.rearrange("l c -> (l c)").unsqueeze(0), writes=[PPb])

        def C(name, lo=0, hi=None, p0=0, p1=128):
            o, n = coff[name]
            if hi is None:
                hi = n
            return CP[p0:p1, o + lo:o + hi]

        def CBc(name, lo=0, hi=None):
            o, n = cboff[name]
            if hi is None:
                hi = n
            return CB[:, o + lo:o + hi]

        def P(l, name, lo, hi):
            o = l * PP_L + ppoff[name]
            return PP[:, o + lo:o + hi]

        AN = sb([128, L, 8])
        BGH = sb([128, L, 24])
        for l in range(L):
            act(AN[:, l, :], P(l, "alog", 0, 8), AF.Exp, [PPb], [PPb])
            ts("dve", AN[:, l, :], AN[:, l, :], -1.0, None, ALU.mult, None, [PPb], [PPb])
            ts("dve", BGH[:, l, :], P(l, "bg", 0, 24), 0.5, None, ALU.mult, None, [PPb], [PPb])

        banks = [es.enter_context(nc.psum_tensor("pb%d" % i, [128, 512], F32)) for i in range(8)]
        bankb = [Buf() for _ in range(8)]
        _bk = [0]

        _bctx = [None]

        def bank():
            ctx = _bctx[0]
            if ctx is not None:
                i = ctx["ids"][ctx["k"] % len(ctx["ids"])]
                ctx["k"] += 1
                return banks[i], bankb[i]
            i = _bk[0]
            _bk[0] = (i + 1) % 8
            return banks[i], bankb[i]

        skAb = Buf()

        with ExitStack() as es2:
            stg = [es2.enter_context(nc.sbuf_tensor("stg%d" % i, [128, 4096], F32)) for i in range(2)]
            stgb = [Buf() for _ in range(2)]
            cvt = [es2.enter_context(nc.sbuf_tensor("cvt%d" % i, [128, 4096], BF16)) for i in range(2)]
            cvtb = [Buf() for _ in range(2)]
            wsb = Buf()
            k = 0
            engs = ["dve", "pool", "act"]
            for l in range(L):
                for bi, (mat, segs, KC, sk) in enumerate(blkdefs):
                    s_, c_ = stg[k % 2], cvt[k % 2]
                    sB, cB = stgb[k % 2], cvtb[k % 2]
                    ncols = sum(n for _, n in segs)
                    if mat == "w_in":
                        src = w_in_d[l]
                    elif mat.startswith("w_branch"):
                        src = w_br_d[l, int(mat[-1])]
                    elif mat == "w_out":
                        src = w_out_d[l]
                    elif mat == "w_up":
                        src = w_up_d[l]
                    else:
                        src = w_dn_d[l]
                    srcv = src.rearrange("(kc p) n -> p kc n", p=128)
                    sv = s_[:, 0:KC * ncols].rearrange("p (kc n) -> p kc n", kc=KC)
                    cv = c_[:, 0:KC * ncols].rearrange("p (kc n) -> p kc n", kc=KC)
                    o = 0
                    for (c0, n) in segs:
                        S.dma(sv[:, :, o:o + n], srcv[:, :, c0:c0 + n], writes=[sB])
                        o += n
                    if sk is None or sk == "half":
                        e = engs[k % 2]
                        if sk is None:
                            cpy(e, c_[:, 0:KC * ncols], s_[:, 0:KC * ncols], [sB], [cB])
                        else:
                            ts(e, c_[:, 0:KC * ncols], s_[:, 0:KC * ncols], 0.5, None, ALU.mult, None, [sB], [cB])
                    else:
                        for kc in range(KC):
                            if sk == "gmix":
                                g = P(l, "gmix", kc, kc + 1)
                            elif sk == "gffn":
                                g = P(l, "gffn", kc, kc + 1)
                            else:
                                i = int(sk[-1])
                                g = P(l, "gbr", 4 * i + kc, 4 * i + kc + 1)
                            e = engs[kc % 3]
                            if e == "act":
                                act(cv[:, kc, :], sv[:, kc, :], AF.Copy, [sB, PPb], [cB], scale=g)
                            else:
                                ts(e, cv[:, kc, :], sv[:, kc, :], g, None, ALU.mult, None, [sB, PPb], [cB])
                    S.dma(ws_d[l, bi, :, 0:KC * ncols], c_[:, 0:KC * ncols], reads=[cB], writes=[wsb])
                    k += 1
            S.barrier()

        NSLOT = 5
        ring = [sb([128, 4096], BF16) for _ in range(NSLOT)]
        ringb = [Buf() for _ in range(NSLOT)]

        units = [(s, t, l) for s in range(NSEQ) for t in range(NT) for l in range(L)]
        stream = [(l, bi) for (_, _, l) in units for bi in range([0, 21, NBLK][stage])]
        _ws = {"next_load": 0, "next_use": 0}

        def _issue_load():
            i = _ws["next_load"]
            if i >= len(stream):
                return
            l, bi = stream[i]
            _, segs, KC, _ = blkdefs[bi]
            n = KC * sum(nn for _, nn in segs)
            S.dma(ring[i % NSLOT][:, 0:n], ws_d[l, bi, :, 0:n], reads=[wsb], writes=[ringb[i % NSLOT]])
            _ws["next_load"] = i + 1

        def wnext(l, bi):
            i = _ws["next_use"]
            assert stream[i] == (l, bi), (stream[i], l, bi)
            while _ws["next_load"] < min(len(stream), i + NSLOT - 2):
                _issue_load()
            _ws["next_use"] = i + 1
            _, segs, KC, _ = blkdefs[bi]
            ncols = sum(nn for _, nn in segs)
            v = ring[i % NSLOT][:, 0:KC * ncols].rearrange("p (kc n) -> p kc n", kc=KC)
            return v, ringb[i % NSLOT]

        X = sb([128, 8, T])
        Xb = Buf()
        hT = sb([128, 8, T], BF16)
        hTb = Buf()
        sq = [sb([128, T], BF16) for _ in range(3)]
        sqb = [Buf() for _ in range(3)]
        rstd = sb([128, T])
        rstdb = Buf()
        xin = sb([128, D])
        xinb = Buf()
        xout = xin
        xoutb = xinb

        sz = sb([128, NCH, 512], BF16); szb = [Buf() for _ in range(NCH)]
        xbuf = [sb([128, 3 + T]) for _ in range(2)]; xbufb = [Buf() for _ in range(2)]
        cacc = [sb([128, T]) for _ in range(2)]; caccb = [Buf() for _ in range(2)]
        xact = sb([128, 6, T], BF16); xactb = [Buf() for _ in range(6)]
        dtraw = sb([128, NCH, 8]); dtrawb = Buf()
        glrT = sb([16, T]); glrTb = Buf()
        q_tok = sb([128, NCH, 512], BF16); q_tokb = [Buf() for _ in range(NCH)]
        k_tok = sb([128, NCH, 512], BF16); k_tokb = [Buf() for _ in range(NCH)]
        v_tok = sb([128, NCH, 512], BF16); v_tokb = [Buf() for _ in range(NCH)]
        srg = sb([128, NCH, 512], BF16); srgb = [Buf() for _ in range(NCH)]
        ropeA = [sb([128, 512])] * 2; ropeAb = [Buf()] * 2
        ropeB = [sb([128, 512])] * 2; ropeBb = [Buf()] * 2
        gqT = sb([128, 2, T], BF16); gqTb = Buf()
        gkT = sb([128, 2, T], BF16); gkTb = Buf()
        gk_tok = sb([128, NCH, 256], BF16); gk_tokb = [Buf() for _ in range(NCH)]
        gv_tok = sb([128, NCH, 512], BF16); gv_tokb = [Buf() for _ in range(NCH)]
        sgr = sb([128, NCH, 512], BF16); sgrb = [Buf() for _ in range(NCH)]
        yT = [sb([128, 4, T], BF16) for _ in range(3)]; yTb = [Buf() for _ in range(3)]
        mT = sb([128, 8, T], BF16); mTb = Buf()
        gth = [sb([128, T])] * 2; gthb = [Buf()] * 2
        gtmp = [sb([128, T])] * 2; gtmpb = [Buf()] * 2
        fa = sb([128, 22, T], BF16); fab = [Buf() for _ in range(22)]
        ub = [sb([128, 2 + T]) for _ in range(2)] * 2; ubb = [Buf() for _ in range(2)] * 2
        facc = [sb([128, T]) for _ in range(4)]; faccb = [Buf() for _ in range(4)]
        fsg = [sb([128, T]) for _ in range(2)]; fsgb = [Buf() for _ in range(2)]
        Sssd = [sb([128, 256]) for _ in range(L)]; Sssdb = [Buf() for _ in range(L)]
        Sssd16 = [sb([128, 256], BF16) for _ in range(L)]; Sssd16b = [Buf() for _ in range(L)]
        Sret = [sb([128, 256]) for _ in range(L)]; Sretb = [Buf() for _ in range(L)]
        Sret16 = [sb([128, 256], BF16) for _ in range(L)]; Sret16b = [Buf() for _ in range(L)]
        Sgla = [sb([128, 256]) for _ in range(L)]; Sglab = [Buf() for _ in range(L)]
        Sgla16 = [sb([128, 256], BF16) for _ in range(L)]; Sgla16b = [Buf() for _ in range(L)]
        halo_s = [sb([128, 6, 3]) for _ in range(L)]; halo_sb = [Buf() for _ in range(L)]
        halo_f = [sb([128, 44, 2]) for _ in range(L)]; halo_fb = [Buf() for _ in range(L)]
        dtv = sb([128, 8]); la = sb([128, 8]); smallb = Buf()
        ex3 = sb([128, 24]); ex3b = Buf()
        decsel = sb([128, 4]); decselb = Buf()
        ncr = sb([8, 128]); ncrb = Buf()
        Bm = sb([128, 8, 128]); Bmb = Buf()
        Esb = [sb([128, 512], BF16) for _ in range(2)]; Esbb = [Buf() for _ in range(2)]
        Psb = [sb([128, 512], BF16) for _ in range(2)]; Psbb = [Buf() for _ in range(2)]
        xs_tok = sb([128, 512], BF16); xs_tokb = Buf()
        vv = sb([128, 512], BF16); vvb = Buf()
        vw = sb([128, 512], BF16); vwb = Buf()
        B_tok = sb([128, 128], BF16); B_tokb = Buf()
        ytmp = [sb([128, 512]) for _ in range(2)]; ytmpb = [Buf() for _ in range(2)]
        ybf = sb([128, 512], BF16); ybfb = Buf()
        st8 = sb([128, 64]); st8b = Buf()
        kTs = sb([128, 4, 128], BF16); kTsb = Buf()
        kw = sb([128, 512], BF16); kwb = Buf()
        gL = sb([128, 256]); gLb = Buf()
        gE = sb([128, 256]); gEb = Buf()
        epos = sb([128, 2, 128]); eposb = Buf()
        eneg = sb([128, 2, 128]); enegb = Buf()
        gki = sb([128, 2, 128], BF16); gkib = Buf()
        gwst = sb([128, 256]); gwstb = Buf()
        gks = sb([128, 256], BF16); gksb = Buf()
        gdec = sb([128, 2]); gdecb = Buf()
        Cm = [sb([128, 128], BF16) for _ in range(2)]; Cmb = [Buf() for _ in range(2)]
        qm = [sb([128, 4, 128], BF16) for _ in range(2)]; qmb = [Buf() for _ in range(2)]
        qdm = [sb([128, 4, 128], BF16) for _ in range(2)]; qdmb = [Buf() for _ in range(2)]
        gqdm = [sb([128, 2, 128], BF16) for _ in range(2)]; gqdmb = [Buf() for _ in range(2)]
        for i_ in range(2):
            S.op("pool", lambda e, i_=i_: e.memset(Cm[i_][:], 0.0), [], [Cmb[i_]])
            S.op("pool", lambda e, i_=i_: e.memset(qm[i_][:], 0.0), [], [qmb[i_]])
            S.op("pool", lambda e, i_=i_: e.memset(qdm[i_][:], 0.0), [], [qdmb[i_]])
            S.op("pool", lambda e, i_=i_: e.memset(gqdm[i_][:], 0.0), [], [gqdmb[i_]])

        assert T == 256
        faflat = fa[:, :, :].rearrange("p j t -> p (j t)")
        y1r = faflat[:, 0:4 * T].bitcast(F32); y1rB = fab[0:4]
        ybf_r = faflat[:, 4 * T:6 * T]; ybf_rB = fab[4:6]
        Psb_r = [faflat[:, 6 * T:8 * T], faflat[:, 8 * T:10 * T]]; Psb_rB = [fab[6:8], fab[8:10]]
        y1g = faflat[:, 10 * T:14 * T].bitcast(F32); y1gB = fab[10:14]
        ybf_g = faflat[:, 14 * T:16 * T]; ybf_gB = fab[14:16]
        Psb_g = faflat[:, 16 * T:18 * T]; Psb_gB = fab[16:18]
        st8r = Buf(); st8g = Buf()
        mrg = faflat[:, 0:16 * T].bitcast(F32).rearrange("p (o t) -> p o t", o=8)
        mrgB = lambda oc: fab[2 * oc:2 * oc + 2]

        dbg_list = []

        def dump(name, ap_, b, width):
            if debug and debug[0] == name:
                dbg_list.append((ap_, b, width))

        def rmsnorm():
            bk, bb = bank()
            for kc in range(8):
                s_, sB = sq[kc % 3], sqb[kc % 3]
                if kc % 2 == 0:
                    act(s_[:], X[:, kc, :], AF.Square, [Xb], [sB])
                else:
                    tt("dve", s_[:], X[:, kc, :], X[:, kc, :], ALU.mult, [Xb], [sB])
                mm(bk[:, 0:T], CBc("cmean"), s_[:], kc == 0, kc == 7, [sB, CBb], [bb], inc=True)
            act(rstd[:], bk[:, 0:T], AF.Ln, [bb], [rstdb], bias=RMS_EPS)
            act(rstd[:], rstd[:], AF.Exp, [rstdb], [rstdb], scale=-0.5)
            for kc in range(8):
                tt("pool" if kc % 2 else "dve", hT[:, kc, :], X[:, kc, :], rstd[:], ALU.mult, [Xb, rstdb], [hTb])

        def proj_feat(W, Wb, c0, n):
            bk, bb = bank()
            for kc in range(8):
                mm(bk[0:n, 0:T], W[:, kc, c0:c0 + n], hT[:, kc, :], kc == 0, kc == 7, [Wb, hTb], [bb])
            return bk, bb

        def proj_tok(W, Wb, c0, n, c):
            bk, bb = bank()
            for kc in range(8):
                mm(bk[:, 0:n], hT[:, kc, c * 128:(c + 1) * 128], W[:, kc, c0:c0 + n], kc == 0, kc == 7, [Wb, hTb], [bb])
            return bk, bb

        import os as _os
        SUB = int(_os.environ.get("SUB", "99"))
        SSUB = int(_os.environ.get("SSUB", "99"))
        RSUB = int(_os.environ.get("RSUB", "99"))

        def _drain(l, frm):
            for bi in range(frm, 21):
                wnext(l, bi)

        def mixer(l, first, pc0):
            rmsnorm()
            W, Wb = wnext(l, 0)
            for c in range(NCH):
                bk, bb = proj_tok(W, Wb, 0, 512, c)
                act(sz[:, c, :], bk[:, :], AF.Silu, [bb], [szb[c]])
            if SUB <= 0:
                return _drain(l, 1)
            W1, W1b = wnext(l, 1)
            W2, W2b = wnext(l, 2)
            for j in range(6):
                if j < 4:
                    bk, bb = proj_feat(W1, W1b, j * 128, 128)
                else:
                    bk, bb = proj_feat(W2, W2b, (j - 4) * 128, 128)
                xb_, xbB = xbuf[j % 2], xbufb[j % 2]
                ac_, acB = cacc[j % 2], caccb[j % 2]
                if first:
                    S.op("pool", lambda e, xb_=xb_: e.memset(xb_[:, 0:3], 0.0), [], [xbB])
                else:
                    cpy("pool", xb_[:, 0:3], halo_s[l][:, j, :], [halo_sb[l]], [xbB])
                act(xb_[:, 3:3 + T], bk[:, 0:T], AF.Copy, [bb], [xbB])
                act(ac_[:], bk[:, 0:T], AF.Identity, [bb, PPb], [acB],
                    bias=P(l, "cb", j, j + 1), scale=P(l, "cw", 18 + j, 19 + j))
                stt("dve", ac_[:], xb_[:, 2:2 + T], P(l, "cw", 12 + j, 13 + j), ac_[:], ALU.mult, ALU.add, [xbB, acB, PPb], [acB])
                stt("pool", ac_[:], xb_[:, 1:1 + T], P(l, "cw", 6 + j, 7 + j), ac_[:], ALU.mult, ALU.add, [xbB, acB, PPb], [acB])
                stt("dve", ac_[:], xb_[:, 0:T], P(l, "cw", j, j + 1), ac_[:], ALU.mult, ALU.add, [xbB, acB, PPb], [acB])
                cpy("pool", halo_s[l][:, j, :], xb_[:, T:T + 3], [xbB], [halo_sb[l]])
                if j > 0:
                    act(xact[:, j - 1, :], cacc[(j - 1) % 2][:], AF.Silu, [caccb[(j - 1) % 2]], [xactb[j - 1]])
            act(xact[:, 5, :], cacc[1][:], AF.Silu, [caccb[1]], [xactb[5]])
            for c in range(NCH):
                bk, bb = proj_tok(W2, W2b, 256, 8, c)
                tt("dve", dtraw[:, c, :], bk[:, 0:8], P(l, "dtb", 0, 8), ALU.add, [bb, PPb], [dtrawb])
            bk, bb = proj_feat(W2, W2b, 264, 16)
            cpy("act", glrT[:, :], bk[0:16, 0:T], [bb], [glrTb])
            if SUB <= 1:
                return _drain(l, 3)
            for bi, dst, dstb in ((3, q_tok, q_tokb), (4, k_tok, k_tokb)):
                W, Wb = wnext(l, bi)
                for c in range(NCH):
                    bk, bb = proj_tok(W, Wb, 0, 512, c)
                    pc = pc0 + c
                    rA, rAb = ropeA[c % 2], ropeAb[c % 2]
                    rB, rBb = ropeB[c % 2], ropeBb[c % 2]
                    bk3 = bk[:, :].rearrange("p (h d) -> p h d", d=64)
                    rA3 = rA[:, :].rearrange("p (h d) -> p h d", d=64)
                    rB3 = rB[:, :].rearrange("p (h d) -> p h d", d=64)
                    cosb = bc(C("cos2", pc * 64, pc * 64 + 64).unsqueeze(1), [128, 8, 64])
                    sn1 = bc(C("sin2", pc * 64, pc * 64 + 32).unsqueeze(1), [128, 8, 32])
                    sn2 = bc(C("sin2", pc * 64 + 32, pc * 64 + 64).unsqueeze(1), [128, 8, 32])
                    tt("dve", rA3, bk3, cosb, ALU.mult, [bb, CPb], [rAb])
                    tt("dve", rB3[:, :, 0:32], bk3[:, :, 32:64], sn1, ALU.mult, [bb, CPb], [rBb])
                    tt("dve", rB3[:, :, 32:64], bk3[:, :, 0:32], sn2, ALU.mult, [bb, CPb], [rBb])
                    tt("pool", dst[:, c, :], rA[:, :], rB[:, :], ALU.add, [rAb, rBb], [dstb[c]])
            W, Wb = wnext(l, 5)
            for c in range(NCH):
                bk, bb = proj_tok(W, Wb, 0, 512, c)
                cpy("act", v_tok[:, c, :], bk[:, :], [bb], [v_tokb[c]])
            W, Wb = wnext(l, 6)
            for c in range(NCH):
                bk, bb = proj_tok(W, Wb, 0, 512, c)
                act(srg[:, c, :], bk[:, :], AF.Silu, [bb], [srgb[c]])
            if SUB <= 2:
                return _drain(l, 7)
            W, Wb = wnext(l, 7)
            for j in range(2):
                bk, bb = proj_feat(W, Wb, j * 128, 128)
                cpy("act", gqT[:, j, :], bk[:, 0:T], [bb], [gqTb])
                bk, bb = proj_feat(W, Wb, 256 + j * 128, 128)
                cpy("act", gkT[:, j, :], bk[:, 0:T], [bb], [gkTb])
            for c in range(NCH):
                bk, bb = proj_tok(W, Wb, 256, 256, c)
                cpy("dve", gk_tok[:, c, :], bk[:, 0:256], [bb], [gk_tokb[c]])
            W, Wb = wnext(l, 8)
            for c in range(NCH):
                bk, bb = proj_tok(W, Wb, 0, 512, c)
                cpy("act", gv_tok[:, c, :], bk[:, :], [bb], [gv_tokb[c]])
            W, Wb = wnext(l, 9)
            for c in range(NCH):
                bk, bb = proj_tok(W, Wb, 0, 512, c)
                act(sgr[:, c, :], bk[:, :], AF.Silu, [bb], [sgrb[c]])
            if SUB <= 3:
                return _drain(l, 10)
            for c in range(NCH):
                gens = [(ssd_chunk(l, c, first and c == 0), None),
                        (ret_chunk(l, c, first and c == 0), {"ids": [3, 4], "k": 0}),
                        (gla_chunk(l, c, first and c == 0), {"ids": [5, 6], "k": 0})]
                while gens:
                    for item in list(gens):
                        _bctx[0] = item[1]
                        try:
                            next(item[0])
                        except StopIteration:
                            gens.remove(item)
                _bctx[0] = None
            if SUB <= 6:
                return _drain(l, 10)
            for i in range(3):
                Wg0, Wg0b = wnext(l, 10 + 3 * i)
                Wg1, Wg1b = wnext(l, 11 + 3 * i)
                Wbr, Wbrb = wnext(l, 12 + 3 * i)
                for oc in range(8):
                    Wg, Wgb = (Wg0, Wg0b) if oc < 4 else (Wg1, Wg1b)
                    bk, bb = proj_feat(Wg, Wgb, (oc % 4) * 128, 128)
                    th, thb = gth[oc % 2], gthb[oc % 2]
                    act(th[:], bk[:, 0:T], AF.Tanh, [bb, PPb], [thb], bias=BGH[:, l, 8 * i + oc:8 * i + oc + 1], scale=0.5)
                    bk2, bb2 = bank()
                    for kc in range(4):
                        mm(bk2[:, 0:T], Wbr[:, kc, oc * 128:(oc + 1) * 128], yT[i][:, kc, :], kc == 0, kc == 3,
                           [Wbrb, yTb[i]], [bb2])
                    if i == 0:
                        stt("dve", mrg[:, oc, :], th[:], 1.0, bk2[:, 0:T], ALU.add, ALU.mult, [thb, bb2], mrgB(oc))
                    else:
                        g_, gB = gtmp[oc % 2], gtmpb[oc % 2]
                        stt("dve", g_[:], th[:], 1.0, bk2[:, 0:T], ALU.add, ALU.mult, [thb, bb2], [gB])
                        if i == 1:
                            tt("pool", mrg[:, oc, :], mrg[:, oc, :], g_[:], ALU.add, [gB] + mrgB(oc), mrgB(oc))
                        else:
                            tt("pool", mT[:, oc, :], mrg[:, oc, :], g_[:], ALU.add, [gB] + mrgB(oc), [mTb])
            for half in range(2):
                W, Wb = wnext(l, 19 + half)
                for o4 in range(4):
                    oc = half * 4 + o4
                    bk, bb = bank()
                    for kc in range(8):
                        mm(bk[:, 0:T], W[:, kc, o4 * 128:(o4 + 1) * 128], mT[:, kc, :], kc == 0, kc == 7, [Wb, mTb], [bb])
                    tt("dve", X[:, oc, :], X[:, oc, :], bk[:, 0:T], ALU.add, [Xb, bb], [Xb])

        def ffn(l, first):
            rmsnorm()
            pend = None

            def fin(p):
                j_, accs_, jj_ = p
                g_, gB = fsg[jj_], fsgb[jj_]
                act(g_[:], accs_[0][0][:], AF.Silu, [accs_[0][1]], [gB])
                tt("dve", fa[:, j_, :], g_[:], accs_[1][0][:], ALU.mult, [gB, accs_[1][1]], [fab[j_]])

            for b in range(11):
                W, Wb = wnext(l, 21 + b)
                for jj in range(2):
                    j = 2 * b + jj
                    accs = []
                    for gv in range(2):
                        ci = j + 22 * gv
                        bk, bb = proj_feat(W, Wb, gv * 256 + jj * 128, 128)
                        u_, uB = ub[(2 * jj + gv) % 4], ubb[(2 * jj + gv) % 4]
                        a_, aB = facc[(2 * jj + gv) % 4], faccb[(2 * jj + gv) % 4]
                        if first:
                            S.op("pool", lambda e, u_=u_: e.memset(u_[:, 0:2], 0.0), [], [uB])
                        else:
                            cpy("pool", u_[:, 0:2], halo_f[l][:, ci, :], [halo_fb[l]], [uB])
                        if True:
                            act(u_[:, 2:2 + T], bk[:, 0:T], AF.Copy, [bb], [uB])
                        else:
                            cpy("dve", u_[:, 2:2 + T], bk[:, 0:T], [bb], [uB])
                        act(a_[:], bk[:, 0:T], AF.Identity, [bb, PPb], [aB],
                            bias=P(l, "fb", ci, ci + 1), scale=P(l, "fw", 88 + ci, 89 + ci))
                        stt("dve", a_[:], u_[:, 1:1 + T], P(l, "fw", 44 + ci, 45 + ci), a_[:], ALU.mult, ALU.add, [uB, aB, PPb], [aB])
                        stt("dve", a_[:], u_[:, 0:T], P(l, "fw", ci, ci + 1), a_[:], ALU.mult, ALU.add, [uB, aB, PPb], [aB])
                        cpy("pool", halo_f[l][:, ci, :], u_[:, T:T + 2], [uB], [halo_fb[l]])
                        accs.append((a_, aB))
                    if pend is not None:
                        fin(pend)
                    pend = (j, accs, jj)
            fin(pend)
            for oc in range(8):
                W, Wb = wnext(l, 32 + oc)
                bk, bb = bank()
                for j in range(22):
                    mm(bk[:, 0:T], W[:, j, 0:128], fa[:, j, :], j == 0, j == 21, [Wb, fab[j]], [bb])
                tt("dve", X[:, oc, :], X[:, oc, :], bk[:, 0:T], ALU.add, [Xb, bb], [Xb])

        def ssd_chunk(l, c, zero_state):
            cs = slice(c * 128, (c + 1) * 128)
            act(dtv[:], dtraw[:, c, :], AF.Exp, [dtrawb], [smallb])
            act(dtv[:], dtv[:], AF.Ln, [smallb], [smallb], bias=1.0)
            tt("dve", la[:], dtv[:], AN[:, l, :], ALU.mult, [smallb, PPb], [smallb])
            bk, bb = banks[0], bankb[0]
            mm(bk[:, 0:8], C("triI"), la[:], True, True, [CPb, smallb], [bb], inc=False)
            mm(bk[:, 8:16], C("triS"), la[:], True, True, [CPb, smallb], [bb], inc=False)
            mm(bk[:, 16:24], C("ones"), la[:], True, True, [CPb, smallb], [bb], inc=False)
            mm(bk[0:8, 128:256], la[:], C("triI"), True, True, [CPb, smallb], [bb])
            act(ex3[:], bk[:, 0:24], AF.Exp, [bb], [ex3b])
            act(decsel[0:64, :], bk[0:64, 16:20], AF.Exp, [bb], [decselb])
            act(decsel[64:128, :], bk[64:128, 20:24], AF.Exp, [bb], [decselb])
            act(ncr[:], bk[0:8, 128:256], AF.Copy, [bb], [ncrb], scale=-1.0)
            yield
            tt("pool", Bm[:], bc(C("triI").unsqueeze(1), [128, 8, 128]), bc(la[:, :].unsqueeze(2), [128, 8, 128]),
               ALU.mult, [CPb, smallb], [Bmb])
            sk, skb = banks[7][:, 0:256], bankb[7]
            for g in range(2):
                cpy("pool", Cm[g][g * 64:(g + 1) * 64, :], xact[g * 64:(g + 1) * 64, 5, cs], [xactb[5]], [Cmb[g]])
            yield
            for g in range(2):
                mm(sk[:, g * 128:(g + 1) * 128], xact[:, 4, cs], Cm[g][:, :],
                   True, True, [xactb[4], Cmb[g]], [skb], inc=(g == 1))
            tk, tkb = banks[1], bankb[1]
            tkv = tk[:, 0:256].bitcast(BF16)
            for j in range(4):
                trp(tkv[:, j * 128:(j + 1) * 128], xact[:, j, cs], CBc("identb"), [xactb[j], CBb], [tkb], inc=(j == 3))
            tb, tbb = banks[2], bankb[2]
            tbv = tb[:, 0:64].bitcast(BF16)
            trp(tbv, xact[:, 4, cs], CBc("identb"), [xactb[4], CBb], [tbb])
            yield
            cpy("act", xs_tok[:], tkv, [tkb], [xs_tokb])
            cpy("act", B_tok[:], tbv, [tbb], [B_tokb])
            yield
            tt("dve", vv[:, :].rearrange("p (h d) -> p h d", d=64), xs_tok[:, :].rearrange("p (h d) -> p h d", d=64),
               bc(dtv[:, :].unsqueeze(2), [128, 8, 64]), ALU.mult, [xs_tokb, smallb], [vvb])
            tt("pool", vw[:, :].rearrange("p (h d) -> p h d", d=64), vv[:, :].rearrange("p (h d) -> p h d", d=64),
               bc(ex3[:, 8:16].unsqueeze(2), [128, 8, 64]), ALU.mult, [vvb, ex3b], [vwb])
            yield
            for g in range(2):
                dk, dkb = banks[1 + g], bankb[1 + g]
                mm(dk[:, :], C("ones"), Bm[:, 4 * g:4 * g + 4, :].rearrange("p h t -> p (h t)"), True, False, [CPb, Bmb], [dkb], inc=False)
                mm(dk[:, :], ncr[:], C("sel", g * 512, (g + 1) * 512, 0, 8), False, False, [ncrb, CPb], [dkb], inc=False)
                mm(dk[:, :], CBc("identb"), CBc("negmask4"), False, True, [CBb], [dkb])
                yield
                act(Esb[g][:], dk[:, :], AF.Exp, [dkb], [Esbb[g]])
                tt("dve", Psb[g][:, :].rearrange("p (h t) -> p h t", h=4), Esb[g][:, :].rearrange("p (h t) -> p h t", h=4),
                   bc(sk[:, g * 128:(g + 1) * 128].unsqueeze(1), [128, 4, 128]), ALU.mult, [Esbb[g], skb], [Psbb[g]])
                yield
            ya, yab = banks[1], bankb[1]
            for h in range(8):
                g, hl = h // 4, h % 4
                mm(ya[:, h * 64:(h + 1) * 64], Psb[g][:, hl * 128:(hl + 1) * 128], vv[:, h * 64:(h + 1) * 64], True, True,
                   [Psbb[g], vvb], [yab], inc=(h == 7))
            y1, y1b = ytmp[0], ytmpb[0]
            if not zero_state:
                yb_, ybb_ = banks[2], bankb[2]
                for g in range(2):
                    mm(yb_[:, g * 256:(g + 1) * 256], Cm[g][:, :], Sssd16[l][:, :], True, True,
                       [Cmb[g], Sssd16b[l]], [ybb_], inc=(g == 1))
                yield
                tt("dve", y1[:, :].rearrange("p (h d) -> p h d", d=64), yb_[:, :].rearrange("p (h d) -> p h d", d=64),
                   bc(ex3[:, 0:8].unsqueeze(2), [128, 8, 64]), ALU.mult, [ybb_, ex3b], [y1b])
                tt("dve", y1[:], y1[:], ya[:, :], ALU.add, [y1b, yab], [y1b])
            else:
                yield
                cpy("dve", y1[:], ya[:, :], [yab], [y1b])
            yield
            y2, y2b = ytmp[1], ytmpb[1]
            tt("pool", y2[:, :].rearrange("p (h d) -> p h d", d=64), xs_tok[:, :].rearrange("p (h d) -> p h d", d=64),
               bc(P(l, "dsk", 0, 8).unsqueeze(2), [128, 8, 64]), ALU.mult, [xs_tokb, PPb], [y2b])
            tt("pool", y1[:], y1[:], y2[:], ALU.add, [y1b, y2b], [y1b])
            tt("pool", y1[:], y1[:], sz[:, c, :], ALU.mult, [y1b, szb[c]], [y1b])
            yield
            su, sub_ = banks[0], bankb[0]
            for g in range(2):
                mm(su[g * 64:(g + 1) * 64, 0:256], B_tok[:, g * 64:(g + 1) * 64], vw[:, g * 256:(g + 1) * 256], True, True,
                   [B_tokb, vwb], [sub_], inc=(g == 1))
            yield
            if zero_state:
                cpy("dve", Sssd[l][:], su[:, 0:256], [sub_], [Sssdb[l]])
            else:
                tt("pool", Sssd[l][:, :].rearrange("p (h d) -> p h d", d=64), Sssd[l][:, :].rearrange("p (h d) -> p h d", d=64),
                   bc(decsel[:, :].unsqueeze(2), [128, 4, 64]), ALU.mult, [Sssdb[l], decselb], [Sssdb[l]])
                tt("dve", Sssd[l][:], Sssd[l][:], su[:, 0:256], ALU.add, [Sssdb[l], sub_], [Sssdb[l]])
            cpy("act", Sssd16[l][:], Sssd[l][:], [Sssdb[l]], [Sssd16b[l]])
            yield
            act(y2[:, 0:256], y1[:, 0:256], AF.Square, [y1b], [y2b, st8b], accum=st8[:, 0:1])
            act(y2[:, 256:512], y1[:, 256:512], AF.Square, [y1b], [y2b, st8b], accum=st8[:, 1:2])
            act(st8[:, 2:4], st8[:, 0:2], AF.Ln, [st8b], [st8b], bias=GN_EPS, scale=1.0 / 256.0)
            act(st8[:, 2:4], st8[:, 2:4], AF.Exp, [st8b], [st8b], scale=-0.5)
            yield
            act(ybf[:, 0:256], y1[:, 0:256], AF.Copy, [y1b, st8b], [ybfb], scale=st8[:, 2:3])
            act(ybf[:, 256:512], y1[:, 256:512], AF.Copy, [y1b, st8b], [ybfb], scale=st8[:, 3:4])
            yield
            yield from finish_y(0, c, ybf, [ybfb], (banks[1], bankb[1]))

        def finish_y(i, c, yb_ap, yb_bufs, bk_=None):
            tk, tkb = bk_ if bk_ is not None else bank()
            tkv = tk[:, 0:256].bitcast(BF16)
            for j in range(4):
                trp(tkv[:, j * 128:(j + 1) * 128], yb_ap[:, j * 128:(j + 1) * 128], CBc("identb"), list(yb_bufs) + [CBb], [tkb], inc=(j == 3))
            yield
            cpy("act", yT[i][:, :, c * 128:(c + 1) * 128], tkv.rearrange("p (j t) -> p j t", j=4), [tkb], [yTb[i]])

        def ret_chunk(l, c, zero_state):
            tq, tqb = bank()
            tqv = tq[:, 0:256].bitcast(BF16)
            for j in range(4):
                trp(tqv[:, j * 128:(j + 1) * 128], q_tok[:, c, j * 128:(j + 1) * 128], CBc("identb"), [q_tokb[c], CBb], [tqb], inc=(j == 3))
            tk_, tkb_ = bank()
            tkv = tk_[:, 0:256].bitcast(BF16)
            for j in range(4):
                trp(tkv[:, j * 128:(j + 1) * 128], k_tok[:, c, j * 128:(j + 1) * 128], CBc("identb"), [k_tokb[c], CBb], [tkb_], inc=(j == 3))
            yield
            for hh in range(2):
                r0, r1 = hh * 64, hh * 64 + 64
                cpy("act", qm[hh][r0:r1, :, :].rearrange("p j t -> p (j t)"), tqv[r0:r1, :], [tqb], [qmb[hh]])
                tt("dve", qdm[hh][r0:r1, :, :].rearrange("p j t -> p (j t)"), qm[hh][r0:r1, :, :].rearrange("p j t -> p (j t)"),
                   C("qdec", 0, None, r0, r1), ALU.mult, [qmb[hh], CPb], [qdmb[hh]])
            cpy("act", kTs[:, :, :].rearrange("p j t -> p (j t)"), tkv, [tkb_], [kTsb])
            yield
            tt("pool", kw[:, :].rearrange("p (h d) -> p h d", d=64), k_tok[:, c, :].rearrange("p (h d) -> p h d", d=64),
               bc(C("wtab").unsqueeze(2), [128, 8, 64]), ALU.mult, [k_tokb[c], CPb], [kwb])
            for g in range(2):
                sk, skb = bank()
                for hl in range(4):
                    h = 4 * g + hl
                    ps = slice((h % 2) * 64, (h % 2) * 64 + 64)
                    mm(sk[:, hl * 128:(hl + 1) * 128], kTs[:, h // 2, :], qm[h % 2][:, h // 2, :], True, True, [kTsb, qmb[h % 2]], [skb], inc=(hl == 3))
                yield
                tt("dve", Psb_r[g][:, :], sk[:, :], C("dret", g * 512, (g + 1) * 512), ALU.mult, [skb, CPb], Psb_rB[g])
            yield
            ya, yab = bank()
            for h in range(8):
                g, hl = h // 4, h % 4
                ps = slice((h % 2) * 64, (h % 2) * 64 + 64)
                mm(ya[:, h * 64:(h + 1) * 64], Psb_r[g][:, hl * 128:(hl + 1) * 128], v_tok[:, c, h * 64:(h + 1) * 64], True, zero_state,
                   Psb_rB[g] + [v_tokb[c]], [yab], inc=(zero_state and h == 7))
                if not zero_state:
                    mm(ya[:, h * 64:(h + 1) * 64], qdm[h % 2][:, h // 2, :], Sret16[l][:, (h // 2) * 64:(h // 2) * 64 + 64], False, True,
                       [qdmb[h % 2], Sret16b[l]], [yab], inc=(h == 7))
            yield
            su, sub_ = bank()
            for h in range(8):
                ps = slice((h % 2) * 64, (h % 2) * 64 + 64)
                mm(su[ps, (h // 2) * 64:(h // 2) * 64 + 64], kw[:, h * 64:(h + 1) * 64], v_tok[:, c, h * 64:(h + 1) * 64], True, True,
                   [kwb, v_tokb[c]], [sub_], inc=(h == 7))
            yield
            if zero_state:
                cpy("dve", Sret[l][:], su[:, 0:256], [sub_], [Sretb[l]])
            else:
                tt("pool", Sret[l][:, :].rearrange("p (j d) -> p j d", d=64), Sret[l][:, :].rearrange("p (j d) -> p j d", d=64),
                   bc(C("decs").unsqueeze(2), [128, 4, 64]), ALU.mult, [Sretb[l], CPb], [Sretb[l]])
                tt("dve", Sret[l][:], Sret[l][:], su[:, 0:256], ALU.add, [Sretb[l], sub_], [Sretb[l]])
            cpy("act", Sret16[l][:], Sret[l][:], [Sretb[l]], [Sret16b[l]])
            yield
            y1 = y1r
            for h in range(8):
                act(y1[:, h * 64:(h + 1) * 64], ya[:, h * 64:(h + 1) * 64], AF.Identity, [yab], y1rB + [st8r], accum=st8[:, 8 + h:9 + h])
                act(y1[:, h * 64:(h + 1) * 64], ya[:, h * 64:(h + 1) * 64], AF.Square, [yab], y1rB + [st8r], accum=st8[:, 16 + h:17 + h])
                if h % 2:
                    yield
            ts("dve", st8[:, 24:32], st8[:, 8:16], 1.0 / 64.0, None, ALU.mult, None, [st8r], [st8r])
            tt("dve", st8[:, 32:40], st8[:, 24:32], st8[:, 24:32], ALU.mult, [st8r], [st8r])
            stt("dve", st8[:, 48:56], st8[:, 16:24], 1.0 / 64.0, st8[:, 32:40], ALU.mult, ALU.subtract, [st8r], [st8r])
            yield
            act(st8[:, 48:56], st8[:, 48:56], AF.Ln, [st8r], [st8r], bias=GN_EPS)
            act(st8[:, 48:56], st8[:, 48:56], AF.Exp, [st8r], [st8r], scale=-0.5)
            yield
            tt("dve", y1[:, :].rearrange("p (h d) -> p h d", d=64), ya[:, :].rearrange("p (h d) -> p h d", d=64),
               bc(st8[:, 24:32].unsqueeze(2), [128, 8, 64]), ALU.subtract, [yab, st8r], y1rB)
            yield
            tt("pool", y1[:, :].rearrange("p (h d) -> p h d", d=64), y1[:, :].rearrange("p (h d) -> p h d", d=64),
               bc(st8[:, 48:56].unsqueeze(2), [128, 8, 64]), ALU.mult, y1rB + [st8r], y1rB)
            tt("pool", ybf_r[:, :], y1[:, :], srg[:, c, :], ALU.mult, y1rB + [srgb[c]], ybf_rB)
            yield
            yield from finish_y(1, c, ybf_r, ybf_rB)

        def gla_chunk(l, c, zero_state):
            cs = slice(c * 128, (c + 1) * 128)
            al, alb = bank()
            mm(al[:, 0:256], glrT[:, cs], WA2[:, l, :], True, False, [glrTb, PPb], [alb], inc=False)
            mm(al[:, 0:256], C("ones", 0, 128, 0, 1), BA[:, l * 256:(l + 1) * 256], False, True, [CPb, PPb], [alb])
            yield
            act(gE[:], al[:, 0:256], AF.Exp, [alb], [gEb], scale=-1.0)
            act(gL[:], gE[:], AF.Ln, [gEb], [gLb], bias=1.0)
            yield
            ck, ckb = bank()
            for j in range(2):
                mm(ck[:, j * 128:(j + 1) * 128], gL[:, j * 128:(j + 1) * 128], C("triI16"), True, True, [gLb, CPb], [ckb], inc=False)
            mm(ck[:, 256:512], C("triS16"), gL[:], True, True, [gLb, CPb], [ckb])
            yield
            act(epos[:, :, :].rearrange("p j t -> p (j t)"), ck[:, 0:256], AF.Exp, [ckb], [eposb], bias=math.log(0.125))
            act(eneg[:, :, :].rearrange("p j t -> p (j t)"), ck[:, 0:256], AF.Exp, [ckb], [enegb], scale=-1.0)
            act(gwst[:], ck[:, 256:512], AF.Exp, [ckb], [gwstb])
            tk_, tkb_ = bank()
            for j in range(2):
                mm(tk_[:, 2 * j:2 * j + 2], gL[:, j * 128:(j + 1) * 128], C("m16"), True, True, [gLb, CPb], [tkb_], inc=(j == 1))
            yield
            act(gdec[:], tk_[:, 0:4].rearrange("p (j two) -> p j two", two=2)[:, :, 0], AF.Exp, [tkb_], [gdecb])
            for hh in range(2):
                r0, r1 = hh * 64, hh * 64 + 64
                tt("pool", gqdm[hh][r0:r1, :, :], gqT[r0:r1, :, cs], epos[r0:r1, :, :], ALU.mult, [gqTb, eposb], [gqdmb[hh]])
            tt("pool", gki[:], gkT[:, :, cs], eneg[:], ALU.mult, [gkTb, enegb], [gkib])
            tt("dve", gks[:], gk_tok[:, c, :], gwst[:], ALU.mult, [gk_tokb[c], gwstb], [gksb])
            yield
            sk, skb = bank()
            for h in range(4):
                ps = slice((h % 2) * 64, (h % 2) * 64 + 64)
                mm(sk[:, h * 128:(h + 1) * 128], gki[:, h // 2, :], gqdm[h % 2][:, h // 2, :], True, True, [gkib, gqdmb[h % 2]], [skb], inc=(h == 3))
            yield
            tt("dve", Psb_g[:, :], sk[:, :], C("causal4"), ALU.mult, [skb, CPb], Psb_gB)
            yield
            ya, yab = bank()
            for h in range(4):
                ps = slice((h % 2) * 64, (h % 2) * 64 + 64)
                mm(ya[:, h * 128:(h + 1) * 128], Psb_g[:, h * 128:(h + 1) * 128], gv_tok[:, c, h * 128:(h + 1) * 128], True, zero_state,
                   Psb_gB + [gv_tokb[c]], [yab], inc=(zero_state and h == 3))
                if not zero_state:
                    mm(ya[:, h * 128:(h + 1) * 128], gqdm[h % 2][:, h // 2, :], Sgla16[l][:, (h // 2) * 128:(h // 2) * 128 + 128], False, True,
                       [gqdmb[h % 2], Sgla16b[l]], [yab], inc=(h == 3))
            yield
            su, sub_ = bank()
            for h in range(4):
                ps = slice((h % 2) * 64, (h % 2) * 64 + 64)
                mm(su[ps, (h // 2) * 128:(h // 2) * 128 + 128], gks[:, h * 64:(h + 1) * 64], gv_tok[:, c, h * 128:(h + 1) * 128], True, True,
                   [gksb, gv_tokb[c]], [sub_], inc=(h == 3))
            yield
            if zero_state:
                cpy("dve", Sgla[l][:], su[:, 0:256], [sub_], [Sglab[l]])
            else:
                for j in range(2):
                    stt("dve", Sgla[l][:, j * 128:(j + 1) * 128], Sgla[l][:, j * 128:(j + 1) * 128], gdec[:, j:j + 1],
                        su[:, j * 128:(j + 1) * 128], ALU.mult, ALU.add, [Sglab[l], gdecb, sub_], [Sglab[l]])
            cpy("act", Sgla16[l][:], Sgla[l][:], [Sglab[l]], [Sgla16b[l]])
            yield
            y1 = y1g
            for g in range(4):
                act(y1[:, g * 128:(g + 1) * 128], ya[:, g * 128:(g + 1) * 128], AF.Square, [yab], y1gB + [st8g], accum=st8[:, 40 + g:41 + g])
            yield
            act(st8[:, 44:48], st8[:, 40:44], AF.Ln, [st8g], [st8g], bias=GN_EPS, scale=1.0 / 128.0)
            act(st8[:, 44:48], st8[:, 44:48], AF.Exp, [st8g], [st8g], scale=-0.5)
            yield
            tt("dve", y1[:, :].rearrange("p (h d) -> p h d", d=128), ya[:, :].rearrange("p (h d) -> p h d", d=128),
               bc(st8[:, 44:48].unsqueeze(2), [128, 4, 128]), ALU.mult, [yab, st8g], y1gB)
            yield
            tt("pool", ybf_g[:, :], y1[:, :], sgr[:, c, :], ALU.mult, y1gB + [sgrb[c]], ybf_gB)
            yield
            yield from finish_y(2, c, ybf_g, ybf_gB)

        outb = Buf()
        for s in range(NSEQ):
            for t in range(NT):
                t0 = t * T
                for c in range(NCH):
                    S.dma(xin[:], x_d[s, t0 + c * 128:t0 + (c + 1) * 128, :], writes=[xinb])
                    for half in range(2):
                        bk, bb = bank()
                        for k4 in range(4):
                            kc = half * 4 + k4
                            trp(bk[:, k4 * 128:(k4 + 1) * 128], xin[:, kc * 128:(kc + 1) * 128], C("identf"), [xinb, CPb], [bb], inc=(k4 == 3))
                        cpy("act", X[:, half * 4:half * 4 + 4, c * 128:(c + 1) * 128], bk[:, :].rearrange("p (k t) -> p k t", k=4), [bb], [Xb])
                for l in range(L):
                    if stage >= 1:
                        mixer(l, t == 0, (t0 // 128))
                    if stage >= 2:
                        ffn(l, t == 0)
                bk, bb = bank()
                for kc in range(8):
                    s_, sB = sq[kc % 3], sqb[kc % 3]
                    act(s_[:], X[:, kc, :], AF.Square, [Xb], [sB])
                    mm(bk[:, 0:T], CBc("cmean"), s_[:], kc == 0, kc == 7, [sB, CBb], [bb], inc=True)
                act(rstd[:], bk[:, 0:T], AF.Ln, [bb], [rstdb], bias=RMS_EPS)
                act(rstd[:], rstd[:], AF.Exp, [rstdb], [rstdb], scale=-0.5)
                for kc in range(8):
                    stt("dve", X[:, kc, :], X[:, kc, :], PP[:, PP_L * L + kc:PP_L * L + kc + 1], rstd[:], ALU.mult, ALU.mult,
                        [Xb, rstdb, PPb], [Xb])
                for c in range(NCH):
                    for half in range(2):
                        bk, bb = bank()
                        for k4 in range(4):
                            kc = half * 4 + k4
                            trp(bk[:, k4 * 128:(k4 + 1) * 128], X[:, kc, c * 128:(c + 1) * 128], C("identf"), [Xb, CPb], [bb], inc=(k4 == 3))
                        cpy("act", xout[:, half * 512:(half + 1) * 512], bk[:, :], [bb], [xoutb])
                    S.dma(out_d[s, t0 + c * 128:t0 + (c + 1) * 128, :], xout[:], reads=[xoutb], writes=[outb])
        if debug and dbg_list:
            o = 0
            for ap_, b, width in dbg_list:
                S.dma(dbg_d[:, o:o + width], ap_[:, 0:width], reads=[b], writes=[outb])
                o += width
        S.finish()
        with nc.Block() as block:
            S.replay(block)
    print('n_inst', S.n_inst, {k: len(v.stream) for k, v in S.E.items()})
    return nc, cp_np, cbp_np


def _run(inputs, L, NSEQ, SEQLEN, ncores, debug=None, stage=2):
    nc, cp_np, cbp_np = build_nc(L, NSEQ, SEQLEN, debug=debug, stage=stage)
    p = {k: np.asarray(v) for k, v in inputs.items()}
    pp = _param_pack(L, p)
    f = lambda a: np.ascontiguousarray(np.asarray(a, np.float32))
    shared = {
        "w_in": f(p["w_in"]), "w_branch": f(p["w_branch"]), "w_out": f(p["w_out"]), "w_up": f(p["w_up"]),
        "w_down": f(p["w_down"]), "gla_w_alpha2": f(p["gla_w_alpha2"]), "gla_b_alpha": f(p["gla_b_alpha"]),
        "pp": pp, "cp": cp_np, "cbp": cbp_np,
    }
    x = f(p["x"])
    in_maps = []
    for i in range(ncores):
        m = dict(shared)
        m["x"] = np.ascontiguousarray(x[i * NSEQ:(i + 1) * NSEQ])
        in_maps.append(m)
    res = run_bass_kernel_spmd(nc, in_maps, core_ids=list(range(ncores)))
    out = np.concatenate([r["out"] for r in res.results], axis=0)
    if debug:
        return out, [r["dbg"] for r in res.results]
    return out


def kernel(**inputs):
    x = inputs["x"]
    B, SEQLEN, _ = x.shape
    L = inputs["w_in"].shape[0]
    ncores = 8
    return _run(inputs, L, B // ncores, SEQLEN, ncores).astype(np.float32)
```
